# Optimizing a Trainium2 kernel written in Bass

```python
import math
import jax
import jax.numpy as jnp
from jax import lax
import numpy as np

D_MODEL = 1024
BATCH = 8
SEQ = 4096
DEPTH = 1
DEC_BATCH = 128
DEC_SEQ = 4
PAST_LEN = 8192
PAGE_SIZE = 128

ATT_HEAD_DIM = 64
ATT_HEADS = (D_MODEL // 2) // ATT_HEAD_DIM
ATT_KV_HEADS = ATT_HEADS // 2
ATT_GROUP = ATT_HEADS // ATT_KV_HEADS
MOBA_BLOCK = 256
MOBA_TOPK = 3
Q_CHUNK = 128
REL_BUCKETS = 32
REL_MAX_DIST = 4096
GDN_DK = 128
GDN_DV = 128
GDN_HEADS = (D_MODEL // 2) // GDN_DV
GDN_CONV = 4
GDN_CHUNK = 64
ATT_Q_W = ATT_HEADS * ATT_HEAD_DIM
ATT_KV_W = ATT_KV_HEADS * ATT_HEAD_DIM
GDN_QK_W = GDN_HEADS * GDN_DK
GDN_V_W = GDN_HEADS * GDN_DV
GDN_CONV_W = 2 * GDN_QK_W + GDN_V_W
MIX_W = ATT_Q_W + GDN_V_W
IN_W = ATT_Q_W + 2 * ATT_KV_W + GDN_CONV_W + GDN_V_W + 2 * GDN_HEADS
N_EXPERTS = 32
TOP_K = 4
D_FF = D_MODEL
SWIGLU_LIMIT = 7.0
SWIGLU_ALPHA = 1.702
EPS = 1e-6
NEG = -1e30

kernel_name = 'hymba_moba_gdn_moe_adaln_step'


def rms_norm(x, w):
    xf = x.astype(jnp.float32)
    y = xf * lax.rsqrt(jnp.mean(xf * xf, axis=-1, keepdims=True) + EPS)
    return (y * w).astype(x.dtype)


def l2_normalize(x):
    return x * lax.rsqrt(jnp.sum(x * x, axis=-1, keepdims=True) + EPS)


def ada_modulation(c, w_ada, b_ada):
    m = jax.nn.silu(c) @ w_ada + b_ada
    return jnp.split(m[:, None, :], 6, axis=-1)


def rel_bucket(dist):
    n = jnp.maximum(dist, 0)
    max_exact = REL_BUCKETS // 2
    nf = jnp.maximum(n, max_exact).astype(jnp.float32)
    large = max_exact + (jnp.log(nf / max_exact) / math.log(REL_MAX_DIST / max_exact)
                         * (REL_BUCKETS - max_exact)).astype(jnp.int32)
    return jnp.where(n < max_exact, n, jnp.minimum(large, REL_BUCKETS - 1))


def moba_attend(q, qpos, k, v, rel_bias):
    nq = q.shape[0]
    n_blk = k.shape[0] // MOBA_BLOCK
    kk = min(MOBA_TOPK, n_blk)
    scale = ATT_HEAD_DIM ** -0.5
    qg = q.reshape(nq, ATT_KV_HEADS, ATT_GROUP, ATT_HEAD_DIM).astype(jnp.float32)
    kb = k.reshape(n_blk, MOBA_BLOCK, ATT_KV_HEADS, ATT_HEAD_DIM).transpose(2, 0, 1, 3)
    vb = v.reshape(n_blk, MOBA_BLOCK, ATT_KV_HEADS, ATT_HEAD_DIM).transpose(2, 0, 1, 3)
    k_mean = jnp.mean(kb.astype(jnp.float32), axis=2)
    own_blk = qpos // MOBA_BLOCK
    gate = jnp.einsum('qhgd,hnd->qhgn', qg, k_mean)
    past = jnp.arange(n_blk)[None, None, None, :] < own_blk[:, None, None, None]
    _, sel = lax.top_k(jnp.where(past, gate, NEG), kk)
    sel_ok = jnp.arange(kk)[None, None, None, :] < own_blk[:, None, None, None]
    hh = jnp.arange(ATT_KV_HEADS)[None, :, None, None]
    k_sel = kb[hh, sel].astype(jnp.float32)
    v_sel = vb[hh, sel].astype(jnp.float32)
    tab = rel_bias.T.reshape(ATT_KV_HEADS, ATT_GROUP, REL_BUCKETS).astype(jnp.float32)
    kpos_sel = sel[..., None] * MOBA_BLOCK + jnp.arange(MOBA_BLOCK)
    b_sel = rel_bucket(qpos[:, None, None, None, None] - kpos_sel)
    bias_sel = tab[hh[..., None], jnp.arange(ATT_GROUP)[None, None, :, None, None], b_sel]
    s_sel = jnp.einsum('qhgd,qhgjtd->qhgjt', qg, k_sel) * scale + bias_sel
    s_sel = jnp.where(sel_ok[..., None], s_sel, NEG).reshape(nq, ATT_KV_HEADS, ATT_GROUP, kk * MOBA_BLOCK)
    k_own = kb[:, own_blk].astype(jnp.float32)
    v_own = vb[:, own_blk].astype(jnp.float32)
    kpos_own = (own_blk * MOBA_BLOCK)[:, None] + jnp.arange(MOBA_BLOCK)
    bias_own = tab[:, :, rel_bucket(qpos[:, None] - kpos_own)].transpose(2, 0, 1, 3)
    s_own = jnp.einsum('qhgd,hqtd->qhgt', qg, k_own) * scale + bias_own
    s_own = jnp.where((kpos_own <= qpos[:, None])[:, None, None, :], s_own, NEG)
    p = jax.nn.softmax(jnp.concatenate([s_sel, s_own], axis=-1), axis=-1)
    p_sel = p[..., :kk * MOBA_BLOCK].reshape(nq, ATT_KV_HEADS, ATT_GROUP, kk, MOBA_BLOCK)
    p_own = p[..., kk * MOBA_BLOCK:]
    out = (jnp.einsum('qhgjt,qhgjtd->qhgd', p_sel, v_sel)
           + jnp.einsum('qhgt,hqtd->qhgd', p_own, v_own))
    return out.reshape(nq, ATT_Q_W).astype(q.dtype)


def moba_prompt(q, k, v, rel_bias):
    b, seq = q.shape[0], q.shape[1]
    l_pad = -(-seq // MOBA_BLOCK) * MOBA_BLOCK
    pad = ((0, 0), (0, l_pad - seq), (0, 0), (0, 0))
    kp, vp = jnp.pad(k, pad), jnp.pad(v, pad)
    n_chunk = seq // Q_CHUNK
    qc = q.reshape(b, n_chunk, Q_CHUNK, ATT_HEADS, ATT_HEAD_DIM)

    def per_seq(args):
        q_s, k_s, v_s = args

        def per_chunk(args2):
            q_c, c_idx = args2
            qpos = c_idx * Q_CHUNK + jnp.arange(Q_CHUNK, dtype=jnp.int32)
            return moba_attend(q_c, qpos, k_s, v_s, rel_bias)
        return lax.map(per_chunk, (q_s, jnp.arange(n_chunk, dtype=jnp.int32)))

    out = lax.map(per_seq, (qc, kp, vp))
    return out.reshape(b, seq, ATT_Q_W)


def moba_sample(q, k_new, v_new, cache_k, cache_v, layer, page_table, rel_bias):
    dec_seq = q.shape[1]
    total = PAST_LEN + dec_seq
    l_pad = -(-total // MOBA_BLOCK) * MOBA_BLOCK
    qpos = PAST_LEN + jnp.arange(dec_seq, dtype=jnp.int32)

    def per_seq(args):
        q_s, kn, vn, pages = args
        pad = jnp.zeros((l_pad - total, ATT_KV_HEADS, ATT_HEAD_DIM), kn.dtype)
        k_past = cache_k[layer, pages].reshape(PAST_LEN, ATT_KV_HEADS, ATT_HEAD_DIM).astype(kn.dtype)
        v_past = cache_v[layer, pages].reshape(PAST_LEN, ATT_KV_HEADS, ATT_HEAD_DIM).astype(vn.dtype)
        k_s = jnp.concatenate([k_past, kn, pad], axis=0)
        v_s = jnp.concatenate([v_past, vn, pad], axis=0)
        return moba_attend(q_s, qpos, k_s, v_s, rel_bias)

    return lax.map(per_seq, (q, k_new, v_new, page_table))


def causal_conv(x_ext, w, t_out):
    return sum(x_ext[:, i:i + t_out] * w[i] for i in range(GDN_CONV))


def gdn_features(conv_out, b_raw, a_raw, a_log, dt_bias):
    n, t, _ = conv_out.shape
    act = jax.nn.silu(conv_out.astype(jnp.float32))
    q = act[..., :GDN_QK_W].reshape(n, t, GDN_HEADS, GDN_DK)
    k = act[..., GDN_QK_W:2 * GDN_QK_W].reshape(n, t, GDN_HEADS, GDN_DK)
    v = act[..., 2 * GDN_QK_W:].reshape(n, t, GDN_HEADS, GDN_DV)
    q = l2_normalize(q) * GDN_DK ** -0.5
    k = l2_normalize(k)
    beta = jax.nn.sigmoid(b_raw.astype(jnp.float32))
    g = -jnp.exp(a_log.astype(jnp.float32)) * jax.nn.softplus(a_raw.astype(jnp.float32) + dt_bias.astype(jnp.float32))
    return q, k, v, beta, g


def gdn_chunked(q, k, v, beta, g):
    b, t = q.shape[0], q.shape[1]
    n = t // GDN_CHUNK
    cs = GDN_CHUNK

    def to_chunks(x):
        return x.reshape((b, n, cs) + x.shape[2:]).swapaxes(2, 3)

    qc, kc, vc, bc, gc = (to_chunks(a) for a in (q, k, v, beta, g))
    gcum = jnp.cumsum(gc, axis=-1)
    tril = jnp.tril(jnp.ones((cs, cs), bool))
    strict = jnp.tril(jnp.ones((cs, cs), bool), -1)
    diff = gcum[..., :, None] - gcum[..., None, :]
    decay = jnp.where(tril, jnp.exp(jnp.where(tril, diff, 0.0)), 0.0)
    kbeta = kc * bc[..., None]
    lower = jnp.where(strict, jnp.einsum('bnhid,bnhjd->bnhij', kbeta, kc) * decay, 0.0)
    eye = jnp.eye(cs, dtype=lower.dtype)
    tmat = lax.linalg.triangular_solve(lower + eye, jnp.broadcast_to(eye, lower.shape),
                                       left_side=True, lower=True, unit_diagonal=True)
    u = tmat @ (vc * bc[..., None])
    w = tmat @ (kbeta * jnp.exp(gcum)[..., None])
    intra = jnp.where(tril, jnp.einsum('bnhid,bnhjd->bnhij', qc, kc) * decay, 0.0)

    def step(s, xs):
        q_i, k_i, u_i, w_i, g_i, a_i = xs
        v_new = u_i - jnp.einsum('bhck,bhkv->bhcv', w_i, s)
        o_i = (jnp.einsum('bhck,bhkv->bhcv', q_i * jnp.exp(g_i)[..., None], s)
               + jnp.einsum('bhcj,bhjv->bhcv', a_i, v_new))
        g_last = g_i[..., -1:]
        s = s * jnp.exp(g_last)[..., None] + jnp.einsum(
            'bhck,bhcv->bhkv', k_i * jnp.exp(g_last - g_i)[..., None], v_new)
        return s, o_i

    s0 = jnp.zeros((b, GDN_HEADS, GDN_DK, GDN_DV), jnp.float32)
    xs = tuple(jnp.moveaxis(a, 1, 0) for a in (qc, kc, u, w, gcum, intra))
    s_fin, o = lax.scan(step, s0, xs)
    o = jnp.moveaxis(o, 0, 1).swapaxes(2, 3).reshape(b, t, GDN_HEADS, GDN_DV)
    return o, s_fin


def gdn_recurrent(q, k, v, beta, g, s0):
    def step(s, xs):
        q_t, k_t, v_t, b_t, g_t = xs
        s = s * jnp.exp(g_t)[..., None, None]
        delta = (v_t - jnp.einsum('bhk,bhkv->bhv', k_t, s)) * b_t[..., None]
        s = s + jnp.einsum('bhk,bhv->bhkv', k_t, delta)
        return s, jnp.einsum('bhk,bhkv->bhv', q_t, s)

    s_fin, o = lax.scan(step, s0, tuple(a.swapaxes(0, 1) for a in (q, k, v, beta, g)))
    return o.swapaxes(0, 1), s_fin


def gdn_prompt(qkv_pre, b_raw, a_raw, conv_w, a_log, dt_bias):
    t = qkv_pre.shape[1]
    ext = jnp.pad(qkv_pre, ((0, 0), (GDN_CONV - 1, 0), (0, 0)))
    q, k, v, beta, g = gdn_features(causal_conv(ext, conv_w, t), b_raw, a_raw, a_log, dt_bias)
    o, s = gdn_chunked(q, k, v, beta, g)
    return o, qkv_pre[:, t - (GDN_CONV - 1):], s


def gdn_sample(qkv_pre, b_raw, a_raw, conv_w, a_log, dt_bias, conv_state, ssm_state):
    t = qkv_pre.shape[1]
    ext = jnp.concatenate([conv_state.astype(qkv_pre.dtype), qkv_pre], axis=1)
    q, k, v, beta, g = gdn_features(causal_conv(ext, conv_w, t), b_raw, a_raw, a_log, dt_bias)
    o, s = gdn_recurrent(q, k, v, beta, g, ssm_state.astype(jnp.float32))
    return o, ext[:, t:], s


def gated_rms_norm(o, z, w):
    y = o * lax.rsqrt(jnp.mean(o * o, axis=-1, keepdims=True) + EPS) * w
    return y * jax.nn.silu(z.astype(jnp.float32))


def moe_ffn(h, w_router, b_router, w_up, b_up, w_down, b_down):
    shp = h.shape
    tok = h.reshape(-1, shp[-1])
    logits = (tok @ w_router + b_router).astype(jnp.float32)
    top_val, top_idx = lax.top_k(logits, TOP_K)
    top_w = jax.nn.softmax(top_val, axis=-1)
    gates = jnp.sum(jax.nn.one_hot(top_idx, N_EXPERTS, dtype=jnp.float32) * top_w[..., None], axis=-2)

    def expert_step(acc, ew):
        w1, b1, w2, b2, g_e = ew
        hu = tok @ w1 + b1
        x_glu = jnp.minimum(hu[:, :D_FF], SWIGLU_LIMIT)
        x_lin = jnp.clip(hu[:, D_FF:], -SWIGLU_LIMIT, SWIGLU_LIMIT)
        act = x_glu * jax.nn.sigmoid(SWIGLU_ALPHA * x_glu) * (x_lin + 1.0)
        return acc + g_e[:, None] * (act @ w2 + b2).astype(jnp.float32), None

    acc, _ = lax.scan(expert_step, jnp.zeros(tok.shape, jnp.float32), (w_up, b_up, w_down, b_down, gates.T))
    return acc.reshape(shp).astype(h.dtype)


def layer_forward(x, c, attn_fn, gdn_fn, w_ada, b_ada, norm_attn_w, norm_ffn_w, w_in, w_out,
                  gdn_norm_w, w_router, b_router, w_up, b_up, w_down, b_down):
    n, t, _ = x.shape
    sh_a, sc_a, gt_a, sh_f, sc_f, gt_f = ada_modulation(c, w_ada, b_ada)
    h = rms_norm(x, norm_attn_w) * (1.0 + sc_a) + sh_a
    proj = h @ w_in
    o1 = ATT_Q_W
    o2 = o1 + ATT_KV_W
    o3 = o2 + ATT_KV_W
    o4 = o3 + GDN_CONV_W
    o5 = o4 + GDN_V_W
    o6 = o5 + GDN_HEADS
    q_a = proj[..., :o1].reshape(n, t, ATT_HEADS, ATT_HEAD_DIM)
    k_a = proj[..., o1:o2].reshape(n, t, ATT_KV_HEADS, ATT_HEAD_DIM)
    v_a = proj[..., o2:o3].reshape(n, t, ATT_KV_HEADS, ATT_HEAD_DIM)
    z_g = proj[..., o4:o5].reshape(n, t, GDN_HEADS, GDN_DV)
    attn_out = attn_fn(q_a, k_a, v_a)
    o_g, conv_new, ssm_new = gdn_fn(proj[..., o3:o4], proj[..., o5:o6], proj[..., o6:])
    gdn_out = gated_rms_norm(o_g, z_g, gdn_norm_w).reshape(n, t, GDN_V_W).astype(x.dtype)
    x = x + gt_a * (jnp.concatenate([attn_out, gdn_out], axis=-1) @ w_out)
    h2 = rms_norm(x, norm_ffn_w) * (1.0 + sc_f) + sh_f
    x = x + gt_f * moe_ffn(h2, w_router, b_router, w_up, b_up, w_down, b_down)
    return x, k_a, v_a, conv_new, ssm_new


def setup_inputs(seed: int = 0) -> dict:
    key = jax.random.key(seed)
    ks = jax.random.split(key, 32)
    n_pages = PAST_LEN // PAGE_SIZE
    n_pool = (DEC_BATCH * n_pages * 5) // 4

    def nrm(k, shape, s):
        return jax.random.normal(k, shape, jnp.float32) * s

    perm = jax.random.permutation(ks[6], n_pool)
    return {
        'x_prompt': nrm(ks[0], (BATCH, SEQ, D_MODEL), 1.0),
        'x_sample': nrm(ks[1], (DEC_BATCH, DEC_SEQ, D_MODEL), 1.0),
        'cache_k': nrm(ks[2], (DEPTH, n_pool, PAGE_SIZE, ATT_KV_HEADS, ATT_HEAD_DIM), 1.0),
        'cache_v': nrm(ks[3], (DEPTH, n_pool, PAGE_SIZE, ATT_KV_HEADS, ATT_HEAD_DIM), 1.0),
        'state_conv': nrm(ks[4], (DEPTH, DEC_BATCH, GDN_CONV - 1, GDN_CONV_W), 1.0),
        'state_ssm': nrm(ks[5], (DEPTH, DEC_BATCH, GDN_HEADS, GDN_DK, GDN_DV), 0.05),
        'page_table': perm[:DEC_BATCH * n_pages].reshape(DEC_BATCH, n_pages).astype(jnp.int32),
        'c_prompt': nrm(ks[7], (BATCH, D_MODEL), 1.0),
        'c_sample': nrm(ks[8], (DEC_BATCH, D_MODEL), 1.0),
        'w_ada': nrm(ks[9], (DEPTH, D_MODEL, 6 * D_MODEL), 0.5 * D_MODEL ** -0.5),
        'b_ada': nrm(ks[10], (DEPTH, 6 * D_MODEL), 0.01),
        'norm_attn_w': 1.0 + nrm(ks[11], (DEPTH, D_MODEL), 0.05),
        'norm_ffn_w': 1.0 + nrm(ks[12], (DEPTH, D_MODEL), 0.05),
        'norm_final_w': 1.0 + nrm(ks[13], (D_MODEL,), 0.05),
        'w_in': nrm(ks[14], (DEPTH, D_MODEL, IN_W), D_MODEL ** -0.5),
        'rel_bias': nrm(ks[15], (REL_BUCKETS, ATT_HEADS), 0.5),
        'conv_w': nrm(ks[16], (DEPTH, GDN_CONV, GDN_CONV_W), GDN_CONV ** -0.5),
        'a_log': jnp.log(jax.random.uniform(ks[17], (DEPTH, GDN_HEADS), jnp.float32, 1.0, 16.0)),
        'dt_bias': nrm(ks[18], (DEPTH, GDN_HEADS), 0.5),
        'gdn_norm_w': 1.0 + nrm(ks[19], (DEPTH, GDN_DV), 0.05),
        'w_out': nrm(ks[20], (DEPTH, MIX_W, D_MODEL), MIX_W ** -0.5),
        'w_router': nrm(ks[21], (DEPTH, D_MODEL, N_EXPERTS), D_MODEL ** -0.5),
        'b_router': nrm(ks[22], (DEPTH, N_EXPERTS), 0.01),
        'w_up': nrm(ks[23], (DEPTH, N_EXPERTS, D_MODEL, 2 * D_FF), D_MODEL ** -0.5),
        'b_up': nrm(ks[24], (DEPTH, N_EXPERTS, 2 * D_FF), 0.01),
        'w_down': nrm(ks[25], (DEPTH, N_EXPERTS, D_FF, D_MODEL), D_FF ** -0.5),
        'b_down': nrm(ks[26], (DEPTH, N_EXPERTS, D_MODEL), 0.01),
    }


def reference(x_prompt, x_sample, cache_k, cache_v, state_conv, state_ssm, page_table, c_prompt, c_sample,
              w_ada, b_ada, norm_attn_w, norm_ffn_w, norm_final_w, w_in, rel_bias, conv_w, a_log, dt_bias,
              gdn_norm_w, w_out, w_router, b_router, w_up, b_up, w_down, b_down):
    xp, xs = x_prompt, x_sample
    kp_l, vp_l, cp_l, sp_l, ks_l, vs_l, cs_l, ss_l = [], [], [], [], [], [], [], []
    for l in range(DEPTH):
        lw = (w_ada[l], b_ada[l], norm_attn_w[l], norm_ffn_w[l], w_in[l], w_out[l], gdn_norm_w[l],
              w_router[l], b_router[l], w_up[l], b_up[l], w_down[l], b_down[l])

        def attn_p(q, k, v):
            return moba_prompt(q, k, v, rel_bias)

        def attn_s(q, k, v, l=l):
            return moba_sample(q, k, v, cache_k, cache_v, l, page_table, rel_bias)

        def gdn_p(qkv, b_raw, a_raw, l=l):
            return gdn_prompt(qkv, b_raw, a_raw, conv_w[l], a_log[l], dt_bias[l])

        def gdn_s(qkv, b_raw, a_raw, l=l):
            return gdn_sample(qkv, b_raw, a_raw, conv_w[l], a_log[l], dt_bias[l], state_conv[l], state_ssm[l])

        xp, kp, vp, cp, sp = layer_forward(xp, c_prompt, attn_p, gdn_p, *lw)
        xs, ks_, vs_, cs_, ss_ = layer_forward(xs, c_sample, attn_s, gdn_s, *lw)
        kp_l.append(kp)
        vp_l.append(vp)
        cp_l.append(cp)
        sp_l.append(sp)
        ks_l.append(ks_)
        vs_l.append(vs_)
        cs_l.append(cs_)
        ss_l.append(ss_)
    y_prompt = rms_norm(xp, norm_final_w)
    y_sample = rms_norm(xs, norm_final_w)
    return (y_prompt, y_sample, jnp.stack(kp_l), jnp.stack(vp_l), jnp.stack(cp_l), jnp.stack(sp_l),
            jnp.stack(ks_l), jnp.stack(vs_l), jnp.stack(cs_l), jnp.stack(ss_l))
```

```python
import numpy as np
import concourse.bass as bass
import concourse.mybir as mybir
from concourse.bass_utils import run_bass_kernel_spmd

F32 = mybir.dt.float32
BF16 = mybir.dt.bfloat16
I32 = mybir.dt.int32
AF = mybir.ActivationFunctionType
ALU = mybir.AluOpType
AX = mybir.AxisListType

NCORES = 8
D = 1024
SEQ = 4096
NT = SEQ // 128
SB = 16
ST = SB * 4
IN_W = 3080
NEG = -30000.0
EPS = 1e-6
NE = 32
SEM_LIMIT = 20000


class Buf:
    __slots__ = ("t", "lw", "rd", "name")

    def __init__(self, t, name=""):
        self.t = t
        self.lw = None
        self.rd = []
        self.name = name

    def __getitem__(self, idx):
        return self.t[idx]


class Sub(Buf):
    __slots__ = ("pre",)

    def __init__(self, t, pre, name=""):
        super().__init__(t, name)
        self.pre = pre

    def __getitem__(self, idx):
        if not isinstance(idx, tuple):
            idx = (idx,)
        return self.t[self.pre(idx) if callable(self.pre) else tuple(self.pre) + idx]


class Prog:
    COMPUTE = ("pe", "act", "dve", "pool")

    def __init__(self, nc):
        self.nc = nc
        self.eng = {"pe": nc.tensor, "act": nc.scalar, "dve": nc.vector, "pool": nc.gpsimd, "sp": nc.sync}
        self.rec = {k: [] for k in self.eng}
        self.sems = {}
        self.cur = {}
        self.seen = {k: {} for k in self.eng}
        self.nsem = 0
        for k in self.eng:
            self._new_eng_sem(k)
        self.dq = {}
        self.dq_next = {}
        for q in ("sp", "pool", "act"):
            lst = []
            for i in range(12):
                key = self._alloc_sem("d%s%d" % (q, i))
                lst.append([key, 0])
            self.dq[q] = lst
            self.dq_next[q] = 0

    def _alloc_sem(self, name):
        key = "%s_%d" % (name, self.nsem)
        self.nsem += 1
        self.sems[key] = self.nc.alloc_semaphore(key)
        return key

    def _new_eng_sem(self, e):
        self.cur[e] = [self._alloc_sem("e" + e), 0]

    def _collect(self, e, reads, writes, is_dma):
        need = {}

        def add(dep, raw):
            if dep is None:
                return
            key, val, prod = dep
            if (not is_dma) and (not raw) and prod == e and e == "pe":
                return
            if self.seen[e].get(key, 0) >= val:
                return
            if need.get(key, 0) < val:
                need[key] = val

        for b in reads:
            add(b.lw, True)
        for b in writes:
            add(b.lw, False)
            for r in b.rd:
                add(r, False)
        return need

    def _emit_waits(self, e, need):
        for key, val in need.items():
            self.seen[e][key] = val
            sem = self.sems[key]
            self.rec[e].append(lambda eng, sem=sem, val=val: eng.wait_ge(sem, val))

    def op(self, e, fn, reads=(), writes=()):
        need = self._collect(e, reads, writes, False)
        self._emit_waits(e, need)
        if self.cur[e][1] >= SEM_LIMIT:
            self._new_eng_sem(e)
        cur = self.cur[e]
        cur[1] += 1
        key, val = cur[0], cur[1]
        sem = self.sems[key]
        self.rec[e].append(lambda eng, fn=fn, sem=sem: fn(eng).then_inc(sem, 1))
        dep = (key, val, e)
        for b in writes:
            b.lw = dep
            b.rd = []
        for b in reads:
            b.rd.append(dep)

    def dma(self, q, out_ap, in_ap, reads=(), writes=(), **kw):
        need = self._collect(q, reads, writes, True)
        lst = self.dq[q]
        i = self.dq_next[q]
        self.dq_next[q] = (i + 1) % len(lst)
        slot = lst[i]
        if slot[1] > 0 and self.seen[q].get(slot[0], 0) < slot[1]:
            if need.get(slot[0], 0) < slot[1]:
                need[slot[0]] = slot[1]
        self._emit_waits(q, need)
        slot[1] += 16
        key, val = slot[0], slot[1]
        sem = self.sems[key]
        self.rec[q].append(lambda eng, o=out_ap, i_=in_ap, sem=sem, kw=kw: eng.dma_start(out=o, in_=i_, **kw).then_inc(sem, 16))
        dep = (key, val, "dma")
        for b in writes:
            b.lw = dep
            b.rd = []
        for b in reads:
            b.rd.append(dep)

    def dma_custom(self, q, issue, reads=(), writes=(), extra=0):
        need = self._collect(q, reads, writes, True)
        lst = self.dq[q]
        slots = []
        for _ in range(1 + extra):
            i = self.dq_next[q]
            self.dq_next[q] = (i + 1) % len(lst)
            slot = lst[i]
            if slot[1] > 0 and self.seen[q].get(slot[0], 0) < slot[1]:
                if need.get(slot[0], 0) < slot[1]:
                    need[slot[0]] = slot[1]
            slots.append(slot)
        self._emit_waits(q, need)
        deps = []
        for slot in slots:
            slot[1] += 16
            deps.append((slot[0], slot[1], "dma"))
        sems = [self.sems[sl[0]] for sl in slots]

        def run(eng, issue=issue, sems=sems):
            self._k_sem = sems[0]
            issue(eng).then_inc(sems[-1], 16)
        self.rec[q].append(run)
        for b, dep in zip(writes, deps):
            b.lw = dep
            b.rd = []
        for b in reads:
            b.rd.extend(deps)

    def wait_all(self, e, bufs):
        need = {}
        for b in bufs:
            if b.lw is not None:
                key, val, _ = b.lw
                if self.seen[e].get(key, 0) < val and need.get(key, 0) < val:
                    need[key] = val
        self._emit_waits(e, need)

    def barrier(self):
        for e in self.eng:
            need = {}
            for e2, (key, val) in self.cur.items():
                if val > 0 and self.seen[e].get(key, 0) < val:
                    need[key] = val
            for q, lst in self.dq.items():
                for key, val in lst:
                    if val > 0 and self.seen[e].get(key, 0) < val:
                        need[key] = val
            self._emit_waits(e, need)

    def emit(self):
        nc = self.nc
        rec = self.rec
        self.rec = {k: [] for k in self.eng}
        self._emit(rec)

    def _emit(self, rec):
        nc = self.nc
        with nc.Block() as block:
            @block.sync
            def _(eng):
                for f in rec["sp"]:
                    f(eng)

            @block.scalar
            def _(eng):
                for f in rec["act"]:
                    f(eng)

            @block.vector
            def _(eng):
                for f in rec["dve"]:
                    f(eng)

            @block.gpsimd
            def _(eng):
                for f in rec["pool"]:
                    f(eng)

            @block.tensor
            def _(eng):
                for f in rec["pe"]:
                    f(eng)


def make_consts():
    c = {}
    c["ident"] = np.eye(128, dtype=np.float32)
    j = np.arange(128)[:, None]
    i = np.arange(128)[None, :]
    c["tri"] = (j <= i).astype(np.float32)
    c["negus"] = np.where(i < j, 0.0, NEG).astype(np.float32)
    c["negut"] = np.where(i >= j, 0.0, NEG).astype(np.float32)
    c["jrev"] = np.eye(128, dtype=np.float32)[::-1].copy()
    return np.concatenate([c["ident"], c["tri"], c["negus"], c["negut"], c["jrev"]], axis=1)


C_IDENT, C_TRI, C_NEGUS, C_NEGUT, C_JREV = 0, 128, 256, 384, 512
NCST = 640
RLEN = 4224


def rel_bucket_np(dist):
    n = np.maximum(dist, 0)
    nf = np.maximum(n, 16).astype(np.float32)
    large = 16 + (np.log(nf / np.float32(16)) / np.float32(np.log(4096 / 16)) * np.float32(16)).astype(np.int32)
    return np.where(n < 16, n, np.minimum(large, 31))


def make_bucket_onehot(dists):
    oh = np.zeros((33, len(dists)), np.float32)
    b = rel_bucket_np(dists)
    for i, d in enumerate(dists):
        if d < 0:
            oh[32, i] = 1.0
        else:
            oh[b[i], i] = 1.0
    return oh


class Ctx:
    pass


def allocators(nc, es, prefix):
    def sb(name, shape, dt=F32):
        return Buf(es.enter_context(nc.sbuf_tensor("%s_%s" % (prefix, name), list(shape), dt)), name)

    def ps(name, shape, dt=F32):
        return Buf(es.enter_context(nc.psum_tensor("%s_%s" % (prefix, name), list(shape), dt)), name)
    return sb, ps


def declare_io(nc, npool=10240, ne_store=NE, dbg=False):
    T = {}

    def inp(name, shape, dt=F32):
        T[name] = nc.dram_tensor(name, list(shape), dt, kind="ExternalInput").ap()

    def outp(name, shape, dt=F32):
        T[name] = nc.dram_tensor(name, list(shape), dt, kind="ExternalOutput").ap()

    def scr(name, shape, dt=F32):
        T[name] = nc.dram_tensor(name, list(shape), dt, kind="Internal").ap()

    inp("xp", [SEQ, D]); inp("xs", [ST, D]); inp("cc", [1 + SB, D])
    inp("w_ada", [D, 6 * D]); inp("b_ada", [1, 6 * D]); inp("nw", [3, D])
    inp("w_in", [D, IN_W]); inp("rel_bias", [32, 8]); inp("conv_w", [4, 1536])
    inp("a_log", [1, 4]); inp("dt_bias", [1, 4]); inp("gnw", [1, 128]); inp("w_out", [D, D])
    inp("w_router", [D, NE]); inp("b_router", [1, NE])
    inp("w_up", [ne_store, D, 2 * D]); inp("b_up", [NE, 2 * D]); inp("w_down", [ne_store, D, D]); inp("b_down", [NE, D])
    inp("cache_k", [npool * 128, 256]); inp("cache_v", [npool * 128, 256])
    inp("state_conv", [SB * 3, 1536]); inp("state_ssm", [SB * 4 * 128, 128])
    inp("page_table", [SB, 64], I32)
    inp("cst", [128, NCST]); inp("ohp", [33, RLEN]); inp("ohs", [4 * 33, NKS]); inp("sel01", [32, 4]); inp("pcol", [128, 1])
    outp("y_p", [SEQ, D]); outp("y_s", [ST, D])
    outp("nk_p", [SEQ, 256]); outp("nv_p", [SEQ, 256]); outp("nc_p", [3, 1536]); outp("ns_p", [4 * 128, 128])
    outp("nk_s", [ST, 256]); outp("nv_s", [ST, 256]); outp("nc_s", [SB * 3, 1536]); outp("ns_s", [SB * 4 * 128, 128])
    if dbg:
        outp("dbg", [128, 8192])
    scr("m_d", [1 + SB, 6 * D])
    scr("rd", [8, RLEN])
    scr("q_s", [ST, 512])
    scr("mix_d", [SEQ + ST, D])
    scr("x1_d", [SEQ + ST, D])
    return T


def phase0_ada(P, nc, es, T):
    R = 1 + SB
    sb, ps = allocators(nc, es, "p0")
    cst = sb("cst0", [128, NCST])
    cc = sb("cc", [R, D])
    ccT = sb("ccT", [128, 8, R])
    ones = sb("ones", [1, R])
    bada = sb("bada", [1, 6 * D])
    wch = [sb("wch%d" % i, [128, 8, 512]) for i in range(2)]
    mo = [sb("mo%d" % i, [R, 512]) for i in range(2)]
    pT = ps("pT", [128, 8, R])
    pm = [ps("pm%d" % i, [R, 512]) for i in range(2)]
    m_d = Buf(T["m_d"], "m_d")
    P.dma("sp", cst[:], T["cst"], writes=[cst])
    P.dma("sp", cc[:], T["cc"], writes=[cc])
    P.dma("sp", bada[:], T["b_ada"], writes=[bada])
    P.op("dve", lambda e: e.memset(ones[:], 1.0), writes=[ones])
    P.op("act", lambda e: e.activation(out=cc[:], in_=cc[:], func=AF.Silu), reads=[cc], writes=[cc])
    for kc in range(8):
        P.op("pe", lambda e, kc=kc: e.transpose(pT[:, kc, :], cc[:, kc * 128:(kc + 1) * 128], cst[0:R, 0:R]),
             reads=[cc, cst], writes=[pT])
    P.op("dve", lambda e: e.tensor_copy(ccT[:], pT[:]), reads=[pT], writes=[ccT])
    wv = T["w_ada"].rearrange("(kc p) n -> p kc n", p=128)
    for j in range(12):
        w = wch[j % 2]
        P.dma("sp", w[:], wv[:, :, j * 512:(j + 1) * 512], writes=[w])
        pp = pm[j % 2]
        for kc in range(8):
            P.op("pe", lambda e, kc=kc, w=w, pp=pp: e.matmul(pp[:], lhsT=ccT[:, kc, :], rhs=w[:, kc, :], start=(kc == 0), stop=False),
                 reads=[ccT, w], writes=[pp])
        P.op("pe", lambda e, pp=pp, j=j: e.matmul(pp[:], lhsT=ones[:], rhs=bada[:, j * 512:(j + 1) * 512], start=False, stop=True),
             reads=[ones, bada], writes=[pp])
        o = mo[j % 2]
        P.op("act", lambda e, o=o, pp=pp: e.copy(out=o[:], in_=pp[:]), reads=[pp], writes=[o])
        P.dma("sp", T["m_d"][:, j * 512:(j + 1) * 512], o[:], reads=[o], writes=[m_d])
    return m_d


def build(stages=("p0",), npool=10240, ne_store=NE, dbg=False, **xkw):
    from contextlib import ExitStack
    nc = bass.Bass("TRN2", target_bir_lowering=False)
    T = declare_io(nc, npool, ne_store, dbg)
    P = Prog(nc)
    X = Ctx()
    X.T = T
    for k_, v_ in xkw.items():
        setattr(X, k_, v_)
    X.outs = []
    X.mix_d = Buf(T["mix_d"], "mix_d")
    X.x1_d = Buf(T["x1_d"], "x1_d")
    X.q_sd = Buf(T["q_s"], "q_s")
    X.ncs = Buf(T["nc_s"], "nc_s")
    if "p0" in stages:
        with ExitStack() as es:
            X.m_d = phase0_ada(P, nc, es, T)
            P.barrier()
            if dbg and stages == ("p0",):
                P.dma("sp", T["dbg"][0:17, 0:6144], T["m_d"], reads=[X.m_d])
                P.barrier()
            P.emit()
    if "a1" in stages:
        with ExitStack() as esA:
            alloc_attn_persist(nc, esA, X)
            with ExitStack() as es:
                alloc_a1_extra(nc, es, X)
                phase_a1(P, nc, es, T, X, sample=False)
                if dbg and "a2" not in stages:
                    P.dma("sp", T["dbg"][:, 0:4096], X.mix_d[0:128, :].rearrange("p (a b) -> p a b", a=1)[:, 0, :] if False else T["mix_d"][0:128, :].rearrange("p d -> p d")[:, :], reads=[X.mix_d]) if False else None
                P.barrier()
                P.emit()
            if "a2" in stages:
                with ExitStack() as es:
                    X.rdb = build_rel_table(P, nc, es, T, X, 8, "ohp", RLEN, "rd", "rt")
                    P.barrier()
                    P.emit()
                with ExitStack() as es:
                    phase_a2(P, nc, es, T, X)
                    P.barrier()
                    P.emit()
    if "smp" in stages:
        with ExitStack() as es:
            X.nks_buf = Buf(T["nk_s"], "nk_s"); X.nvs_buf = Buf(T["nv_s"], "nv_s")
            alloc_a1_extra(nc, es, X, "a1xs")
            phase_a1(P, nc, es, T, X, sample=True)
            P.barrier()
            P.emit()
        with ExitStack() as es:
            phase_a2s(P, nc, es, T, X)
            P.barrier()
            P.emit()
    if "a3" in stages:
        with ExitStack() as es:
            def tiles(gta_p, gta_s):
                for t in range(getattr(X, "ntile", NT)):
                    yield (t * 128, 128, T["xp"][t * 128:(t + 1) * 128, :], gta_p)
                if "smp" in stages:
                    yield (SEQ, 4 * getattr(X, "nseq", SB), T["xs"][0:4 * getattr(X, "nseq", SB), :], gta_s)
            phase_a3(P, nc, es, T, X, tiles)
            P.barrier()
            if dbg:
                nt_ = getattr(X, "ntile", NT)
                for t in range(min(nt_, 16)):
                    P.dma("sp", T["dbg"][:, t * 512:(t + 1) * 512], T["mix_d"][t * 128:(t + 1) * 128, 0:512], reads=[X.mix_d])
            P.barrier()
            P.emit()
    if "b" in stages:
        with ExitStack() as es:
            nt_ = getattr(X, "ntile", NT)
            alltiles = [(t * 128, 128, False, T["y_p"][t * 128:(t + 1) * 128, :]) for t in range(nt_)]
            per = getattr(X, "per_pass", 7)
            passes = [alltiles[i:i + per] for i in range(0, len(alltiles), per)]
            if "smp" in stages:
                ns_ = 4 * getattr(X, "nseq", SB)
                stile = (SEQ, ns_, True, T["y_s"][0:ns_, :])
                if passes and len(passes[-1]) < per:
                    passes[-1].append(stile)
                else:
                    passes.append([stile])
            phase_b(P, nc, es, T, X, passes, ne=getattr(X, "ne", NE))
            P.barrier()
            P.emit()
    return nc


def shard_inputs(inp, c):
    f = lambda a: np.ascontiguousarray(a)
    m = {}
    m["xp"] = f(inp["x_prompt"][c])
    m["xs"] = f(inp["x_sample"][c * SB:(c + 1) * SB].reshape(ST, D))
    m["cc"] = f(np.concatenate([inp["c_prompt"][c:c + 1], inp["c_sample"][c * SB:(c + 1) * SB]], axis=0))
    m["w_ada"] = f(inp["w_ada"][0]); m["b_ada"] = f(inp["b_ada"])
    m["nw"] = f(np.stack([inp["norm_attn_w"][0], inp["norm_ffn_w"][0], inp["norm_final_w"]], axis=0))
    m["w_in"] = f(inp["w_in"][0]); m["rel_bias"] = f(inp["rel_bias"]); m["conv_w"] = f(inp["conv_w"][0])
    m["a_log"] = f(inp["a_log"]); m["dt_bias"] = f(inp["dt_bias"]); m["gnw"] = f(inp["gdn_norm_w"])
    m["w_out"] = f(inp["w_out"][0]); m["w_router"] = f(inp["w_router"][0]); m["b_router"] = f(inp["b_router"])
    m["w_up"] = f(inp["w_up"][0]); m["b_up"] = f(inp["b_up"][0]); m["w_down"] = f(inp["w_down"][0]); m["b_down"] = f(inp["b_down"][0])
    m["cache_k"] = inp["cache_k"].reshape(-1, 256); m["cache_v"] = inp["cache_v"].reshape(-1, 256)
    m["state_conv"] = f(inp["state_conv"][0, c * SB:(c + 1) * SB].reshape(SB * 3, 1536))
    m["state_ssm"] = f(inp["state_ssm"][0, c * SB:(c + 1) * SB].reshape(SB * 4 * 128, 128))
    m["page_table"] = f(inp["page_table"][c * SB:(c + 1) * SB])
    m["cst"] = make_consts()
    m["ohp"] = make_bucket_onehot(4095 - np.arange(RLEN))
    m["ohs"] = make_sample_onehots()
    m["pcol"] = np.arange(128, dtype=np.float32).reshape(128, 1)
    m["sel01"] = (np.arange(4)[None, :] == ((np.arange(32) % 8) // 2)[:, None]).astype(np.float32)
    return m


def mm(P, ob, oap, lhsT, rhs, rd, start=True, stop=True):
    P.op("pe", lambda e: e.matmul(oap, lhsT=lhsT, rhs=rhs, start=start, stop=stop), reads=rd, writes=[ob])


def tr(P, ob, oap, in_ap, ident_ap, rd):
    P.op("pe", lambda e: e.transpose(oap, in_ap, ident_ap), reads=rd, writes=[ob])


def tr32(P, ob, oap, in_ap, ident_ap, rd):
    P.op("pe", lambda e: e.matmul(oap, lhsT=in_ap, rhs=ident_ap, start=True, stop=True), reads=rd, writes=[ob])


def actf(P, ob, oap, in_ap, func, rd, bias=None, scale=None, accum=None, wr_extra=(), eng="act"):
    kw = {}
    if bias is not None:
        kw["bias"] = bias
    if scale is not None:
        kw["scale"] = scale
    if accum is not None:
        kw["accum_out"] = accum
    P.op(eng, lambda e: e.activation(out=oap, in_=in_ap, func=func, **kw), reads=rd, writes=[ob] + list(wr_extra))


def ts(P, eng, ob, oap, in0, s1, s2, op0, op1=None, rd=(), accum=None, wr_extra=()):
    kw = {}
    if op1 is not None:
        kw["op1"] = op1
    if accum is not None:
        kw["accum_out"] = accum
    P.op(eng, lambda e: e.tensor_scalar(oap, in0, s1, s2, op0, **kw), reads=rd, writes=[ob] + list(wr_extra))


def tt(P, eng, ob, oap, in0, in1, op, rd=()):
    P.op(eng, lambda e: e.tensor_tensor(oap, in0, in1, op), reads=rd, writes=[ob])


def stt(P, eng, ob, oap, in0, scalar, in1, op0, op1, rd=()):
    P.op(eng, lambda e: e.scalar_tensor_tensor(oap, in0, scalar, in1, op0, op1), reads=rd, writes=[ob])


def cp(P, eng, ob, oap, in_ap, rd=()):
    if eng == "act":
        P.op(eng, lambda e: e.copy(out=oap, in_=in_ap), reads=rd, writes=[ob])
    else:
        P.op(eng, lambda e: e.tensor_copy(oap, in_ap), reads=rd, writes=[ob])


def rsqrt_col(P, ob, oap, in_ap, scale, eps, rd, post=1.0):
    actf(P, ob, oap, in_ap, AF.Ln, rd=rd, scale=scale, bias=eps)
    if post != 1.0:
        actf(P, ob, oap, oap, AF.Exp, rd=[ob], scale=-0.5, bias=float(np.log(post)))
    else:
        actf(P, ob, oap, oap, AF.Exp, rd=[ob], scale=-0.5)


class Rot:
    def __init__(self, bufs):
        self.b = bufs
        self.i = 0

    def get(self):
        b = self.b[self.i % len(self.b)]
        self.i += 1
        return b


def phase_a1(P, nc, es, T, X, sample=False):
    pre = "a1s" if sample else "a1"
    sb, ps = allocators(nc, es, pre)
    ntile = getattr(X, 'nseq', SB) if sample else getattr(X, 'ntile', NT)
    TP = 4 if sample else 128
    TS = TP
    x_d = T["xs"] if sample else T["xp"]
    row0 = SEQ if sample else 0
    cst = sb("cst", [128, NCST])
    P.dma("sp", cst[:], T["cst"], writes=[cst])
    ident = cst[:, C_IDENT:C_IDENT + 128]
    tri = cst[:, C_TRI:C_TRI + 128]
    negus = cst[:, C_NEGUS:C_NEGUS + 128]
    negut = cst[:, C_NEGUT:C_NEGUT + 128]
    identb = sb("identb", [128, 128], BF16)
    cp(P, "dve", identb, identb[:], ident, rd=[cst])
    jrevb = sb("jrevb", [128, 128], BF16)
    cp(P, "dve", jrevb, jrevb[:], cst[:, C_JREV:C_JREV + 128], rd=[cst])
    wmod = sb("wmod", [128, D]); shb = sb("shb", [128, D]); tmp = sb("tmp", [128, D]); nwb = tmp; junk = tmp
    m_d = X.m_d
    if not sample:
        P.dma("sp", shb[:], T["m_d"][0, 0:D].partition_broadcast(128), reads=[m_d], writes=[shb])
        P.dma("sp", wmod[:], T["m_d"][0, D:2 * D].partition_broadcast(128), reads=[m_d], writes=[wmod])
    if sample:
        nwb2 = sb("nwb2", [4, D])
        P.dma("sp", nwb2[:], T["nw"][0, :].partition_broadcast(4), writes=[nwb2])
    if not sample:
        P.dma("sp", nwb[:], T["nw"][0, :].partition_broadcast(128), writes=[nwb])
        stt(P, "dve", wmod, wmod[0:TP, :], wmod[0:TP, :], 1.0, nwb[0:TP, :], ALU.add, ALU.mult, rd=[wmod, nwb])
    w_in = sb("w_in", [128, 8, IN_W], BF16)
    wiv = T["w_in"].rearrange("(kc p) n -> p kc n", p=128)
    for kc in range(8):
        for hh in range(2):
            P.dma("pool", w_in[:, kc, hh * 1540:(hh + 1) * 1540], wiv[:, kc, hh * 1540:(hh + 1) * 1540], writes=[w_in])
    cw = sb("cw", [128, 12, 4])
    with nc.allow_non_contiguous_dma(reason="tiny conv weight transpose"):
        pass
    for i in range(4):
        P.dma("sp", cw[:, :, i], T["conv_w"][i, :].rearrange("(j p) -> p j", p=128), writes=[cw], allow_slow_non_contiguous=True)
    dtb = sb("dtb", [128, 4]); negA = sb("negA", [128, 4]); gnw = sb("gnw", [128, 128])
    P.dma("sp", dtb[:], T["dt_bias"][0, :].partition_broadcast(128), writes=[dtb])
    P.dma("sp", negA[:], T["a_log"][0, :].partition_broadcast(128), writes=[negA])
    P.dma("sp", gnw[:], T["gnw"][0, :].partition_broadcast(128), writes=[gnw])
    actf(P, negA, negA[:], negA[:], AF.Exp, rd=[negA])
    ts(P, "dve", negA, negA[:], negA[:], -1.0, None, ALU.mult, rd=[negA])
    if getattr(X, 'stopat', 99) == 1:
        return

    xt = Rot([sb("x%d" % i, [128, D]) for i in range(1)])
    hb = sb("hb", [128, D], BF16); hT = sb("hT", [128, 8, 128], BF16)
    ss = sb("ss", [128, 8])
    q_tm = sb("q_tm", [128, 512]); kv_tm = Rot([sb("kv%d" % i, [128, 512]) for i in range(1)])
    sz = sb("sz", [128, 512]); ba = sb("ba", [128, 8])
    ext = sb("ext", [128, 12, 131]); cacc = sb("cacc", [128, 12, 128]); ctmp = sb("ctmp", [128, 12, 128])
    afm = sb("afm", [128, 12, 128])
    pre_tm = ctmp
    beta = sb("beta", [128, 4]); nbeta = sb("nbeta", [128, 4]); g = sb("g", [128, 4]); gc = sb("gc", [128, 4]); ngc = sb("ngc", [128, 4])
    egc = sb("egc", [128, 4]); bege = sb("bege", [128, 4])
    S = [sb("S%d" % h, [128, 128]) for h in range(4)]
    mixg = sb("mixg", [128, 512])
    pTr = ps("pTr", [128, 8, 128], BF16)
    pA = Rot([ps("pA%d" % i, [128, 512]) for i in range(2)])
    gbanks = [ps("pG%d" % i, [128, 4, 128]) for i in range(4)]
    gps = Rot([bv2(b, j, "g%d" % j) for b in gbanks for j in range(4)])
    names = ["kh", "qh", "vb", "kbe", "kt", "qt", "qtt", "Dm", "DTm", "EG", "Nm", "NTm", "Pa", "PTa", "Pb", "PTb", "XTa", "XTb",
             "wT", "IT", "usb", "vnew", "ktil", "tmpg"]
    hsets = [{n: sb("%s_%d" % (n, i), [128, 128]) for n in names} for i in range(1)] * 2
    cols = [{n: sb("%s_%d" % (n, i), [128, 1]) for n in ["rq", "rk", "glast", "eglast", "ekl", "rso", "ssq", "ssk", "sso"]} for i in range(2)]
    for h in range(4):
        P.op("dve", lambda e, h=h: e.memset(S[h][:], 0.0), writes=[S[h]])
    P.op("dve", lambda e: e.memset(ext[:], 0.0), writes=[ext])
    nk_d = X.nks_buf if sample else Buf(T["nk_p"]); nv_d = X.nvs_buf if sample else Buf(T["nv_p"])
    mix_d = X.mix_d
    n = TP

    if sample:
        stc = sb("stc", [SB * 3, 1536])
        P.dma("sp", stc[:], T["state_conv"], writes=[stc])
        Ssm = Rot([[sb("Ss%d_%d" % (i, h), [128, 128]) for h in range(4)] for i in range(2)])
        ns_s = Buf(T["ns_s"], "ns_s")
        q_sd = X.q_sd
    for t in range(ntile):
        x = xt.get()
        P.dma("sp", x[0:n, :], x_d[t * TS:t * TS + n, :], writes=[x])
        if sample:
            P.dma("sp", shb[0:4, :], T["m_d"][1 + t, 0:D].partition_broadcast(4), reads=[m_d], writes=[shb])
            P.dma("sp", wmod[0:4, :], T["m_d"][1 + t, D:2 * D].partition_broadcast(4), reads=[m_d], writes=[wmod])
            stt(P, "dve", wmod, wmod[0:4, :], wmod[0:4, :], 1.0, nwb2[0:4, :], ALU.add, ALU.mult, rd=[wmod, nwb2])
            for jb in range(3):
                pp = pA.get()
                for jj in range(4):
                    j = jb * 4 + jj
                    P.op("pe", lambda e, pp=pp, jj=jj, j=j, t=t: e.matmul(pp[:, jj * 128:jj * 128 + 3], lhsT=stc[:, j * 128:(j + 1) * 128],
                                                                         rhs=ident[0:48, 3 * t:3 * t + 3], start=True, stop=True),
                         reads=[stc, cst], writes=[pp])
                cp(P, "dve", ext, ext[:, jb * 4:(jb + 1) * 4, 0:3], pp[:, :].rearrange("p (j t) -> p j t", j=4)[:, :, 0:3], rd=[pp])
            Scur = Ssm.get()
            for h in range(4):
                r = (t * 4 + h) * 128
                P.dma("sp", Scur[h][:, :], T["state_ssm"][r:r + 128, :], writes=[Scur[h]])
            X.S_for = lambda h, Scur=Scur: Scur[h]
        actf(P, junk, junk[0:n, :], x[0:n, :], AF.Square, rd=[x], accum=ss[0:n, 0:1], wr_extra=[ss])
        rsqrt_col(P, ss, ss[0:n, 2:3], ss[0:n, 0:1], 1.0 / D, EPS, [ss])
        stt(P, "dve", tmp, tmp[0:n, :], x[0:n, :], ss[0:n, 2:3], wmod[0:n, :], ALU.mult, ALU.mult, rd=[x, ss, wmod])
        tt(P, "pool", hb, hb[0:n, :], tmp[0:n, :], shb[0:n, :], ALU.add, rd=[tmp, shb])
        if getattr(X, 'stopat', 99) == 2:
            return

        for kc in range(8):
            tr(P, pTr, pTr[:, kc, 0:n], hb[0:n, kc * 128:(kc + 1) * 128], identb[0:n, 0:n], rd=[hb, identb])
        cp(P, "act", hT, hT[:, :, 0:n], pTr[:, :, 0:n], rd=[pTr])
        if getattr(X, 'stopat', 99) == 3:
            return

        for gi, (c0, wdt) in enumerate([(0, 512), (512, 512), (2560, 512), (3072, 8)]):
            pp = pA.get()
            for kc in range(8):
                mm(P, pp, pp[0:n, 0:wdt], hT[:, kc, 0:n], w_in[:, kc, c0:c0 + wdt], [hT, w_in], start=(kc == 0), stop=(kc == 7))
            if gi == 0:
                cp(P, "act", q_tm, q_tm[0:n, :], pp[0:n, :], rd=[pp])
            elif gi == 1:
                kv = kv_tm.get()
                cp(P, "dve", kv, kv[0:n, :], pp[0:n, :], rd=[pp])
                P.dma("sp", nk_d[t * TS:t * TS + n, :], kv[0:n, 0:256], reads=[kv], writes=[nk_d])
                P.dma("sp", nv_d[t * TS:t * TS + n, :], kv[0:n, 256:512], reads=[kv], writes=[nv_d])
                kv_hook(P, t, kv, n, locals(), X)
            elif gi == 2:
                actf(P, sz, sz[0:n, :], pp[0:n, :], AF.Silu, rd=[pp])
            else:
                cp(P, "dve", ba, ba[0:n, :], pp[0:n, 0:8], rd=[pp])
        q_hook(P, t, q_tm, n, locals(), X)
        if getattr(X, 'stopat', 99) == 5:
            return

        for jb in range(3):
            pp = pA.get()
            for jj in range(4):
                j = jb * 4 + jj
                for kc in range(8):
                    mm(P, pp, pp[:, jj * 128:jj * 128 + n], w_in[:, kc, 1024 + j * 128:1024 + (j + 1) * 128], hT[:, kc, 0:n], [hT, w_in],
                       start=(kc == 0), stop=(kc == 7))
            cp(P, "act" if jb % 2 == 0 else "dve", ext, ext[:, jb * 4:(jb + 1) * 4, 3:3 + n],
               pp[:, :].rearrange("p (j t) -> p j t", j=4)[:, :, 0:n], rd=[pp])
        conv_hook(P, t, n, locals(), X)
        for i in range(4):
            src = ext[:, :, i:i + n]
            wb = cw[:, :, i:i + 1].to_broadcast([128, 12, n])
            if i == 0:
                tt(P, "dve", cacc, cacc[:, :, 0:n], src, wb, ALU.mult, rd=[ext, cw])
            else:
                tt(P, "pool", ctmp, ctmp[:, :, 0:n], src, wb, ALU.mult, rd=[ext, cw])
                tt(P, "dve", cacc, cacc[:, :, 0:n], cacc[:, :, 0:n], ctmp[:, :, 0:n], ALU.add, rd=[cacc, ctmp])
        actf(P, afm, afm[:, :, 0:n], cacc[:, :, 0:n], AF.Silu, rd=[cacc])
        if getattr(X, 'stopat', 99) == 6:
            return

        if not sample:
            cp(P, "pool", ext, ext[:, :, 0:3], ext[:, :, 128:131], rd=[ext])
        if getattr(X, 'stopat', 99) == 7:
            return
        actf(P, beta, beta[0:n, :], ba[0:n, 0:4], AF.Sigmoid, rd=[ba])
        if getattr(X, 'stopat', 99) == 8:
            return
        ts(P, "dve", nbeta, nbeta[0:n, :], beta[0:n, :], -1.0, None, ALU.mult, rd=[beta])
        tt(P, "dve", g, g[0:n, :], ba[0:n, 4:8], dtb[0:n, :], ALU.add, rd=[ba, dtb])
        actf(P, g, g[0:n, :], g[0:n, :], AF.Exp, rd=[g])
        if getattr(X, 'stopat', 99) == 9:
            return
        actf(P, g, g[0:n, :], g[0:n, :], AF.Ln, rd=[g], bias=1.0)
        if getattr(X, 'stopat', 99) == 10:
            return
        tt(P, "dve", g, g[0:n, :], g[0:n, :], negA[0:n, :], ALU.mult, rd=[g, negA])
        if not getattr(X, 'nogdn', False):
            gdn_tile(P, t, n, locals(), X)
        if sample:
            for h in range(4):
                r = (t * 4 + h) * 128
                P.dma("sp", ns_s[r:r + 128, :], Scur[h][:, :], reads=[Scur[h]], writes=[ns_s])
    a1_end(P, locals(), X)


def sub2(t, j, name=""):
    return Sub(t, lambda idx, j=j: (idx[0], j) + tuple(idx[1:]), name)


class BV:
    def __init__(self, parent, pre, name=""):
        self.parent = parent
        self.pre = pre
        self.name = name

    @property
    def lw(self):
        return self.parent.lw

    @lw.setter
    def lw(self, v):
        self.parent.lw = v

    @property
    def rd(self):
        return self.parent.rd

    @rd.setter
    def rd(self, v):
        self.parent.rd = v

    def __getitem__(self, idx):
        if not isinstance(idx, tuple):
            idx = (idx,)
        return self.parent.t[self.pre(idx)]


def bv2(parent, j, name=""):
    return BV(parent, lambda idx, j=j: (idx[0], j) + tuple(idx[1:]), name)


def gdn_tile(P, t, n, L, X):
    cst, tri, ident, negus, negut = L["cst"], L["tri"], L["ident"], L["negus"], L["negut"]
    g, gc, ngc, egc, bege, beta, nbeta = L["g"], L["gc"], L["ngc"], L["egc"], L["bege"], L["beta"], L["nbeta"]
    gps, afm, S, sz, gnw, mixg, hsets, cols = L["gps"], L["afm"], L["S"], L["sz"], L["gnw"], L["mixg"], L["hsets"], L["cols"]
    gp = gps.get()
    mm(P, gp, gp[0:n, 0:4], tri[0:n, 0:n], g[0:n, :], [cst, g])
    cp(P, "dve", gc, gc[0:n, :], gp[0:n, 0:4], rd=[gp])
    ts(P, "dve", ngc, ngc[0:n, :], gc[0:n, :], -1.0, None, ALU.mult, rd=[gc])
    actf(P, egc, egc[0:n, :], gc[0:n, :], AF.Exp, rd=[gc])
    tt(P, "dve", bege, bege[0:n, :], beta[0:n, :], egc[0:n, :], ALU.mult, rd=[beta, egc])
    if getattr(X, 'gstop', 99) == 1:
        return
    for h in range(4):
        hs = hsets[h % 2]
        cl = cols[h % 2]
        kh, qh, vb, kbe, kt, qt, qtt = hs["kh"], hs["qh"], hs["vb"], hs["kbe"], hs["kt"], hs["qt"], hs["qtt"]
        Dm, DTm, EG, Nm, NTm, wT, IT, usb, vnew, ktil, tmpg = (hs[k] for k in
                                                                ["Dm", "DTm", "EG", "Nm", "NTm", "wT", "IT", "usb", "vnew", "ktil", "tmpg"])
        pq = gps.get(); tr32(P, pq, pq[0:n, :], afm[:, h, 0:n], ident, [afm, cst])
        pk = gps.get(); tr32(P, pk, pk[0:n, :], afm[:, 4 + h, 0:n], ident, [afm, cst])
        pv = gps.get(); tr32(P, pv, pv[0:n, :], afm[:, 8 + h, 0:n], ident, [afm, cst])
        if getattr(X, 'gstop', 99) == 11:
            return
        actf(P, tmpg, tmpg[0:n, :], pq[0:n, :], AF.Square, rd=[pq], accum=cl["ssq"][0:n, :], wr_extra=[cl["ssq"]])
        actf(P, tmpg, tmpg[0:n, :], pk[0:n, :], AF.Square, rd=[pk], accum=cl["ssk"][0:n, :], wr_extra=[cl["ssk"]])
        if getattr(X, 'gstop', 99) == 12:
            return
        rsqrt_col(P, cl["rq"], cl["rq"][0:n, :], cl["ssq"][0:n, :], 1.0, EPS, [cl["ssq"]])
        rsqrt_col(P, cl["rk"], cl["rk"][0:n, :], cl["ssk"][0:n, :], 1.0, EPS, [cl["ssk"]])
        if getattr(X, 'gstop', 99) == 13:
            return
        ts(P, "dve", qh, qh[0:n, :], pq[0:n, :], cl["rq"][0:n, :], 128.0 ** -0.5, ALU.mult, ALU.mult, rd=[pq, cl["rq"]])
        ts(P, "dve", kh, kh[0:n, :], pk[0:n, :], cl["rk"][0:n, :], None, ALU.mult, rd=[pk, cl["rk"]])
        ts(P, "dve", vb, vb[0:n, :], pv[0:n, :], beta[0:n, h:h + 1], None, ALU.mult, rd=[pv, beta])
        if getattr(X, 'gstop', 99) == 14:
            return
        ts(P, "pool", kbe, kbe[0:n, :], kh[0:n, :], bege[0:n, h:h + 1], None, ALU.mult, rd=[kh, bege])
        if getattr(X, 'gstop', 99) == 2:
            return
        pg = gps.get()
        mm(P, pg, pg[:, 0:n], g[0:n, h:h + 1].to_broadcast([n, 128]), tri[0:n, 0:n], [g, cst])
        cp(P, "dve", cl["glast"], cl["glast"][:, :], pg[:, n - 1:n], rd=[pg])
        actf(P, cl["eglast"], cl["eglast"][:, :], cl["glast"][:, :], AF.Exp, rd=[cl["glast"]])
        actf(P, cl["ekl"], cl["ekl"][0:n, :], gc[0:n, h:h + 1], AF.Exp, rd=[gc, cl["glast"]], scale=-1.0, bias=cl["glast"][0:n, :])
        ts(P, "pool", ktil, ktil[0:n, :], kh[0:n, :], cl["ekl"][0:n, :], None, ALU.mult, rd=[kh, cl["ekl"]])
        if getattr(X, 'gstop', 99) == 3:
            return
        stt(P, "dve", tmpg, tmpg[0:n, 0:n], pg[0:n, 0:n], -1.0, negus[0:n, 0:n], ALU.mult, ALU.add, rd=[pg, cst])
        actf(P, Dm, Dm[0:n, 0:n], tmpg[0:n, 0:n], AF.Exp, rd=[tmpg, gc], bias=gc[0:n, h:h + 1])
        tt(P, "dve", IT, IT[0:n, 0:n], pg[0:n, 0:n], negut[0:n, 0:n], ALU.add, rd=[pg, cst])
        actf(P, DTm, DTm[0:n, 0:n], IT[0:n, 0:n], AF.Exp, rd=[IT, ngc], bias=ngc[0:n, h:h + 1])
        actf(P, EG, EG[:, 0:n], pg[:, 0:n], AF.Exp, rd=[pg])
        if getattr(X, 'gstop', 99) == 4:
            return
        pkt = gps.get(); tr32(P, pkt, pkt[:, 0:n], kh[0:n, :], ident[0:n, 0:n], [kh, cst])
        cp(P, "act", kt, kt[:, 0:n], pkt[:, 0:n], rd=[pkt])
        pqt = gps.get(); tr32(P, pqt, pqt[:, 0:n], qh[0:n, :], ident[0:n, 0:n], [qh, cst])
        cp(P, "dve", qt, qt[:, 0:n], pqt[:, 0:n], rd=[pqt])
        tt(P, "dve", qtt, qtt[:, 0:n], pqt[:, 0:n], EG[:, 0:n], ALU.mult, rd=[pqt, EG])
        if getattr(X, 'gstop', 99) == 5:
            return
        pkk = gps.get(); mm(P, pkk, pkk[0:n, 0:n], kt[:, 0:n], kt[:, 0:n], [kt])
        stt(P, "dve", Nm, Nm[0:n, 0:n], pkk[0:n, 0:n], nbeta[0:n, h:h + 1], Dm[0:n, 0:n], ALU.mult, ALU.mult, rd=[pkk, nbeta, Dm])
        pnt = gps.get(); tr32(P, pnt, pnt[0:n, 0:n], Nm[0:n, 0:n], ident[0:n, 0:n], [Nm, cst])
        cp(P, "act", NTm, NTm[0:n, 0:n], pnt[0:n, 0:n], rd=[pnt])
        XT = hs["XTa"]
        tt(P, "pool", XT, XT[0:n, 0:n], NTm[0:n, 0:n], ident[0:n, 0:n], ALU.add, rd=[NTm, cst])
        if getattr(X, 'gstop', 99) == 6:
            return
        Pc, PTc = Nm, NTm
        nsteps = 0
        while (1 << (nsteps + 1)) < n:
            nsteps += 1
        for s in range(1, nsteps + 1):
            Pn, PTn = (hs["Pa"], hs["PTa"]) if s % 2 == 1 else (hs["Pb"], hs["PTb"])
            p1 = gps.get(); mm(P, p1, p1[0:n, 0:n], PTc[0:n, 0:n], Pc[0:n, 0:n], [PTc, Pc])
            cp(P, "act", Pn, Pn[0:n, 0:n], p1[0:n, 0:n], rd=[p1])
            if s < nsteps:
                p2 = gps.get(); mm(P, p2, p2[0:n, 0:n], Pc[0:n, 0:n], PTc[0:n, 0:n], [PTc, Pc])
                cp(P, "dve", PTn, PTn[0:n, 0:n], p2[0:n, 0:n], rd=[p2])
            p3 = gps.get(); mm(P, p3, p3[0:n, 0:n], Pn[0:n, 0:n], XT[0:n, 0:n], [Pn, XT])
            XTn = hs["XTb"] if XT is hs["XTa"] else hs["XTa"]
            tt(P, "dve", XTn, XTn[0:n, 0:n], XT[0:n, 0:n], p3[0:n, 0:n], ALU.add, rd=[XT, p3])
            XT = XTn
            Pc, PTc = Pn, PTn
        pu = gps.get(); mm(P, pu, pu[0:n, :], XT[0:n, 0:n], vb[0:n, :], [XT, vb])
        if getattr(X, 'gstop', 99) == 7:
            return
        cp(P, "act", usb, usb[0:n, :], pu[0:n, :], rd=[pu])
        pw = gps.get(); mm(P, pw, pw[:, 0:n], kbe[0:n, :], XT[0:n, 0:n], [XT, kbe])
        cp(P, "dve", wT, wT[:, 0:n], pw[:, 0:n], rd=[pw])
        pit = gps.get(); mm(P, pit, pit[0:n, 0:n], kt[:, 0:n], qt[:, 0:n], [kt, qt])
        tt(P, "dve", IT, IT[0:n, 0:n], pit[0:n, 0:n], DTm[0:n, 0:n], ALU.mult, rd=[pit, DTm])
        if getattr(X, 'gstop', 99) == 8:
            return
        Sh = X.S_for(h) if hasattr(X, "S_for") else S[h]
        pws = gps.get(); mm(P, pws, pws[0:n, :], wT[:, 0:n], Sh[:, :], [wT, Sh])
        tt(P, "dve", vnew, vnew[0:n, :], usb[0:n, :], pws[0:n, :], ALU.subtract, rd=[usb, pws])
        po = gps.get()
        mm(P, po, po[0:n, :], qtt[:, 0:n], Sh[:, :], [qtt, Sh], start=True, stop=False)
        mm(P, po, po[0:n, :], IT[0:n, 0:n], vnew[0:n, :], [IT, vnew], start=False, stop=True)
        psu = gps.get(); mm(P, psu, psu[:, :], ktil[0:n, :], vnew[0:n, :], [ktil, vnew])
        stt(P, "dve", Sh, Sh[:, :], Sh[:, :], cl["eglast"][:, 0:1], psu[:, :], ALU.mult, ALU.add, rd=[Sh, cl["eglast"], psu])
        if getattr(X, 'gstop', 99) == 9:
            return
        osb = hs["usb"]
        cp(P, "dve", osb, osb[0:n, :], po[0:n, :], rd=[po])
        actf(P, tmpg, tmpg[0:n, :], osb[0:n, :], AF.Square, rd=[osb], accum=cl["sso"][0:n, :], wr_extra=[cl["sso"]])
        if getattr(X, 'gstop', 99) == 17:
            return
        rsqrt_col(P, cl["rso"], cl["rso"][0:n, :], cl["sso"][0:n, :], 1.0 / 128, EPS, [cl["sso"]])
        if getattr(X, 'gstop', 99) == 18:
            return
        stt(P, "dve", tmpg, tmpg[0:n, :], osb[0:n, :], cl["rso"][0:n, :], gnw[0:n, :], ALU.mult, ALU.mult, rd=[osb, cl["rso"], gnw])
        if getattr(X, 'gstop', 99) == 19:
            return
        tt(P, "pool", mixg, mixg[0:n, h * 128:(h + 1) * 128], tmpg[0:n, :], sz[0:n, h * 128:(h + 1) * 128], ALU.mult, rd=[tmpg, sz])
        if getattr(X, 'gstop', 99) == 15:
            return
    if getattr(X, 'gstop', 99) == 16:
        return
    r0 = L["row0"] + t * L["TS"]
    P.dma("sp", X.T["mix_d"][r0:r0 + n, 512:1024], mixg[0:n, :], reads=[mixg], writes=[X.mix_d])


def kv_hook(P, t, kv, n, L, X):
    if L["sample"]:
        return
    identb = L["identb"]
    kd = X.kdup.get()
    Vs = X.Vbs[t]
    cp(P, "pool", Vs, Vs[:, :, 0:64], kv[:, 256:512].rearrange("p (h d) -> p h d", h=4), rd=[kv])
    P.op("pool", lambda e: e.memset(Vs[:, :, 64:65], 1.0), writes=[Vs])
    src = kv[:, 0:256].rearrange("p (h d) -> p h d", h=4).unsqueeze(2).to_broadcast([128, 4, 2, 64])
    cp(P, "pool", kd, kd[:, :, :, :], src, rd=[kv])
    pk = X.pKT
    for h in range(4):
        tr(P, pk, pk[:, h, :], kd[:, h, :, :].rearrange("p r d -> p (r d)"), identb[:, :], [kd, identb])
    Ks = X.KTs[t]
    cp(P, "act", Ks, Ks[:, :, :], pk[:, :, :], rd=[pk])


def stcT_src(stc, t, j):
    return stc[:, j * 128:(j + 1) * 128]


def q_hook(P, t, q_tm, n, L, X):
    if L["sample"]:
        P.dma("sp", X.T["q_s"][t * 4:t * 4 + 4, :], q_tm[0:4, :], reads=[q_tm], writes=[X.q_sd])
        return
    identb = L["jrevb"]
    qb = X.qb.get()
    cp(P, "pool", qb, qb[:, :], q_tm[:, :], rd=[q_tm])
    pq = X.pQT
    for p in range(4):
        tr(P, pq, pq[:, p, :], qb[:, p * 128:(p + 1) * 128], identb[:, :], [qb, identb])
    Qs = X.QTs[t]
    cp(P, "dve", Qs, Qs[:, :, :], pq[:, :, :], rd=[pq])


def conv_hook(P, t, n, L, X):
    if (not L["sample"]) and t != NT - 1:
        return
    hT, w_in, pA, pre_tm = L["hT"], L["w_in"], L["pA"], L["pre_tm"]
    for gi in range(3):
        pp = pA.get()
        for kc in range(8):
            mm(P, pp, pp[0:n, :], hT[:, kc, 0:n], w_in[:, kc, 1024 + gi * 512:1024 + (gi + 1) * 512], [hT, w_in], start=(kc == 0), stop=(kc == 7))
        cp(P, "act", pre_tm, pre_tm[0:n, gi * 4:(gi + 1) * 4, :], pp[0:n, :].rearrange("p (j t) -> p j t", j=4), rd=[pp])
    if L["sample"]:
        ncs = X.ncs
        P.dma("sp", ncs[3 * t:3 * t + 3, :].rearrange("r (j t) -> r j t", j=12), pre_tm[1:4, :, :], reads=[pre_tm], writes=[ncs])
        return
    ncp = Buf(X.T["nc_p"])
    P.dma("sp", ncp[:, :].rearrange("r (j t) -> r j t", j=12), pre_tm[125:128, :, :], reads=[pre_tm], writes=[ncp])
    X.outs.append(ncp)


def a1_end(P, L, X):
    if L["sample"] or getattr(X, 'noend', False):
        return
    nsp = Buf(X.T["ns_p"])
    v = getattr(X, 'endvar', 0)
    for h in range(4 if v != 1 else 1):
        src = L["S"][h] if v != 2 else L["tmp"]
        P.dma("sp", nsp[h * 128:(h + 1) * 128, :], src[:, 0:128], reads=[src], writes=[nsp])
    X.outs += [nsp, L["nk_d"], L["nv_d"]]


def alloc_attn_persist(nc, es, X):
    sb, ps = allocators(nc, es, "pers")
    QT = sb("QT", [128, 4, SEQ], BF16)
    KT = sb("KT", [128, 4, SEQ], BF16)
    Vb = sb("Vb", [128, NT, 4, 65], BF16)
    X.QT, X.KT, X.Vb = QT, KT, Vb
    X.QTs = [Sub(QT.t, lambda idx, t=t: (idx[0], idx[1], slice(t * 128, (t + 1) * 128)) if True else None, "QT%d" % t) for t in range(NT)]
    X.KTs = [Sub(KT.t, lambda idx, t=t: (idx[0], idx[1], slice(t * 128, (t + 1) * 128)), "KT%d" % t) for t in range(NT)]
    X.Vbs = [sub2(Vb.t, t, "Vb%d" % t) for t in range(NT)]


def alloc_a1_extra(nc, es, X, pre="a1x"):
    sb, ps = allocators(nc, es, pre)
    X.kdup = Rot([sb("kdup%d" % i, [128, 4, 2, 64], BF16) for i in range(2)])
    X.qb = Rot([sb("qb%d" % i, [128, 512], BF16) for i in range(2)])
    pb = Buf(es.enter_context(nc.psum_tensor(pre + "_pKQ", [128, 8, 128], BF16)), "pKQ")
    X.pKT = BV(pb, lambda idx: (idx[0], (slice(0, 4) if isinstance(idx[1], slice) else idx[1])) + tuple(idx[2:]), "pKT")
    X.pQT = BV(pb, lambda idx: (idx[0], (slice(4, 8) if isinstance(idx[1], slice) else idx[1] + 4)) + tuple(idx[2:]), "pQT")


def build_rel_table(P, nc, es, T, X, relb_rows, oh_name, ncols, rd_name, pre):
    sb, ps = allocators(nc, es, pre)
    relb = sb("relb", [33, 8])
    P.op("dve", lambda e: e.memset(relb[:], NEG), writes=[relb])
    P.dma("sp", relb[0:32, :], T["rel_bias"], writes=[relb])
    oh = sb("oh", [33, ncols])
    P.dma("sp", oh[:], T[oh_name], writes=[oh])
    rsb = sb("rsb", [relb_rows, ncols])
    pp = ps("pp", [relb_rows, 512])
    lhs = X.rel_lhs(relb) if hasattr(X, "rel_lhs") else relb[:, :]
    for j in range(0, ncols, 512):
        w = min(512, ncols - j)
        mm(P, pp, pp[:, 0:w], lhs, oh[:, j:j + w], [relb, oh])
        actf(P, rsb, rsb[:, j:j + w], pp[:, 0:w], AF.Exp, rd=[pp], eng="act")
    rdb = Buf(T[rd_name], rd_name)
    P.dma("sp", rdb[:, :], rsb[:, :], reads=[rsb], writes=[rdb])
    return rdb


def phase_a2(P, nc, es, T, X):
    sb, ps = allocators(nc, es, "a2")
    scale = 64.0 ** -0.5
    nch = getattr(X, "ntile", NT)
    cst = sb("cst", [128, NCST])
    P.dma("sp", cst[:], T["cst"], writes=[cst])
    jrevb = sb("jrevb", [128, 128], BF16)
    cp(P, "dve", jrevb, jrevb[:], cst[:, C_JREV:C_JREV + 128], rd=[cst])
    rdb = X.rdb
    tabs = Rot([sb("tab%d" % i, [128, 4096]) for i in range(2)])
    S_all = Rot([sb("S_all%d" % i, [128, 4096]) for i in range(2)])
    E = sb("E", [128, 4096])
    Pb = Rot([sb("Pb%d" % i, [128, 4096], BF16) for i in range(2)])
    PTs = Rot([sb("PTs%d" % i, [128, 4, 128], BF16) for i in range(3)])
    gt = sb("gt", [128, 16]); mx8 = sb("mx8", [128, 8]); mb = sb("mb", [128, 16]); bcol = sb("bcol", [128, 16])
    nm = sb("nm", [128, 1]); rm = sb("rm", [128, 1]); rcp = sb("rcp", [128, 1])
    ao = Rot([sb("ao%d" % i, [128, 64]) for i in range(2)])
    pS = Rot([ps("pS%d" % i, [128, 512]) for i in range(3)])
    pT = Rot([ps("pT%d" % i, [128, 8, 128], BF16) for i in range(2)])
    pO = Rot([ps("pO%d" % i, [128, 512]) for i in range(2)])
    mix_d = X.mix_d
    for hq in range(8):
        hk = hq // 2
        r0 = (hq % 2) * 64
        tab = tabs.get()
        src = bass.AP(tensor=T["rd"].tensor, offset=hq * RLEN, ap=[[1, 128], [1, 4096]])
        P.dma("sp", tab[:, :], src, reads=[rdb], writes=[tab])
        for c in range(nch):
            nk = 128 * (c + 1)
            ob = c // 2
            q0 = 128 * c
            Sa = S_all.get()
            for pi, k0 in enumerate(range(0, nk, 512)):
                w = min(512, nk - k0)
                pp = pS.get()
                rdq = [X.QTs[c]] + [X.KTs[t] for t in range(k0 // 128, (k0 + w) // 128)]
                mm(P, pp, pp[:, 0:w], X.QT[r0:r0 + 64, hk, q0:q0 + 128], X.KT[r0:r0 + 64, hk, k0:k0 + w], rdq)
                cp(P, "dve" if pi % 2 == 0 else "act", Sa, Sa[:, k0:k0 + w], pp[:, 0:w], rd=[pp])
            P.op("pool", lambda e: e.memset(gt[:], -1e30), writes=[gt])
            if ob > 0:
                P.op("dve", lambda e, Sa=Sa, ob=ob: e.tensor_reduce(gt[:, 0:ob], Sa[:, 0:256 * ob].rearrange("p (n k) -> p n k", k=256), AX.X, ALU.add),
                     reads=[Sa], writes=[gt])
            P.op("dve", lambda e: e.max(mx8[:], gt[:]), reads=[gt], writes=[mx8])
            ts(P, "dve", mb, mb[:], gt[:], mx8[:, 2:3], 1.0, ALU.is_ge, ALU.subtract, rd=[gt, mx8])
            P.op("dve", lambda e, Sa=Sa, nk=nk: e.reduce_max(rm[:], Sa[:, 0:nk], AX.X), reads=[Sa], writes=[rm])
            ts(P, "dve", nm, nm[:], rm[:], -scale, None, ALU.mult, rd=[rm])
            ts(P, "dve", bcol, bcol[:], mb[:], -NEG, nm[:, 0:1], ALU.mult, ALU.add, rd=[mb, nm])
            for n in range(ob):
                actf(P, E, E[:, 256 * n:256 * (n + 1)], Sa[:, 256 * n:256 * (n + 1)], AF.Exp, rd=[Sa, bcol], scale=scale, bias=bcol[:, n:n + 1])
            actf(P, E, E[:, 256 * ob:nk], Sa[:, 256 * ob:nk], AF.Exp, rd=[Sa, nm], scale=scale, bias=nm[:, 0:1])
            Pc = Pb.get()
            tt(P, "dve" if c % 2 == 0 else "pool", Pc, Pc[:, 0:nk], E[:, 0:nk], tab[:, 3968 - q0:4096], ALU.mult, rd=[E, tab])
            po = pO.get()
            for g0 in range(0, c + 1, 4):
                g1 = min(g0 + 4, c + 1)
                pt = pT.get()
                for kt in range(g0, g1):
                    tr(P, pt, pt[:, kt - g0, :], Pc[:, kt * 128:(kt + 1) * 128], jrevb[:, :], [Pc, jrevb])
                pts = PTs.get()
                cp(P, "act" if (g0 // 4) % 2 == 0 else "dve", pts, pts[:, 0:g1 - g0, :], pt[:, 0:g1 - g0, :], rd=[pt])
                for kt in range(g0, g1):
                    mm(P, po, po[:, 0:65], pts[:, kt - g0, :], X.Vb[:, kt, hk, :], [pts, X.Vbs[kt]], start=(kt == 0), stop=(kt == c))
            P.op("dve", lambda e, po=po: e.reciprocal(rcp[:], po[:, 64:65]), reads=[po], writes=[rcp])
            a = ao.get()
            ts(P, "dve", a, a[:, :], po[:, 0:64], rcp[:, 0:1], None, ALU.mult, rd=[po, rcp])
            P.dma("sp", T["mix_d"][q0:q0 + 128, hq * 64:(hq + 1) * 64], a[:, :], reads=[a], writes=[mix_d])


def phase_a3(P, nc, es, T, X, tiles):
    sb, ps = allocators(nc, es, "a3")
    cst = sb("cst", [128, NCST])
    P.dma("sp", cst[:], T["cst"], writes=[cst])
    identb = sb("identb", [128, 128], BF16)
    cp(P, "dve", identb, identb[:], cst[:, C_IDENT:C_IDENT + 128], rd=[cst])
    w_out = sb("w_out", [128, 8, D], BF16)
    wov = T["w_out"].rearrange("(kc p) n -> p kc n", p=128)
    for kc in range(8):
        P.dma("pool", w_out[:, kc, :], wov[:, kc, :], writes=[w_out])
    gta_p = sb("gta_p", [128, D]); gta_s = sb("gta_s", [128, D])
    m_d = X.m_d
    P.dma("sp", gta_p[:], T["m_d"][0, 2 * D:3 * D].partition_broadcast(128), reads=[m_d], writes=[gta_p])
    for s in range(SB):
        P.dma("sp", gta_s[4 * s:4 * s + 4, :], T["m_d"][1 + s, 2 * D:3 * D].partition_broadcast(4), reads=[m_d], writes=[gta_s])
    mixs = Rot([sb("mix%d" % i, [128, D]) for i in range(2)])
    xs = Rot([sb("x%d" % i, [128, D]) for i in range(2)])
    mixb = sb("mixb", [128, D], BF16)
    mixT = sb("mixT", [128, 8, 128], BF16)
    x1 = Rot([sb("x1_%d" % i, [128, D]) for i in range(2)])
    pTr = ps("pTr", [128, 8, 128], BF16)
    pY = Rot([ps("pY%d" % i, [128, 512]) for i in range(2)])
    for (row, n, xsrc, gta) in tiles(gta_p, gta_s):
        mix = mixs.get(); x = xs.get()
        P.dma("sp", mix[0:n, :], T["mix_d"][row:row + n, :], reads=[X.mix_d], writes=[mix])
        P.dma("sp", x[0:n, :], xsrc, writes=[x])
        cp(P, "pool", mixb, mixb[0:n, :], mix[0:n, :], rd=[mix])
        for kc in range(8):
            tr(P, pTr, pTr[:, kc, 0:n], mixb[0:n, kc * 128:(kc + 1) * 128], identb[0:n, 0:n], [mixb, identb])
        cp(P, "act", mixT, mixT[:, :, 0:n], pTr[:, :, 0:n], rd=[pTr])
        xo = x1.get()
        for g in range(2):
            pp = pY.get()
            for kc in range(8):
                mm(P, pp, pp[0:n, :], mixT[:, kc, 0:n], w_out[:, kc, g * 512:(g + 1) * 512], [mixT, w_out], start=(kc == 0), stop=(kc == 7))
            tt(P, "dve", xo, xo[0:n, g * 512:(g + 1) * 512], pp[0:n, :], gta[0:n, g * 512:(g + 1) * 512], ALU.mult, rd=[pp, gta])
        tt(P, "pool", xo, xo[0:n, :], xo[0:n, :], x[0:n, :], ALU.add, rd=[xo, x])
        P.dma("sp", T["x1_d"][row:row + n, :], xo[0:n, :], reads=[xo], writes=[X.x1_d])


def phase_b(P, nc, es, T, X, passes, ne=NE):
    sb, ps = allocators(nc, es, "b")
    MAXS = max(len(p) for p in passes)
    cst = sb("cst", [128, NCST])
    P.dma("sp", cst[:], T["cst"], writes=[cst])
    ident = cst[:, C_IDENT:C_IDENT + 128]
    m_d = X.m_d
    wmod = sb("wmod", [128, D]); shf = sb("shf", [128, D]); gtf = sb("gtf", [128, D]); nwf = sb("nwf", [128, D])
    h2 = sb("h2", [128, D])
    tmp = h2
    P.dma("sp", nwf[:], T["nw"][2, :].partition_broadcast(128), writes=[nwf])

    def load_mods(sample):
        P.dma("sp", tmp[:], T["nw"][1, :].partition_broadcast(128), writes=[tmp])
        if not sample:
            P.dma("sp", shf[:], T["m_d"][0, 3 * D:4 * D].partition_broadcast(128), reads=[m_d], writes=[shf])
            P.dma("sp", wmod[:], T["m_d"][0, 4 * D:5 * D].partition_broadcast(128), reads=[m_d], writes=[wmod])
            P.dma("sp", gtf[:], T["m_d"][0, 5 * D:6 * D].partition_broadcast(128), reads=[m_d], writes=[gtf])
        else:
            for s in range(SB):
                P.dma("sp", shf[4 * s:4 * s + 4, :], T["m_d"][1 + s, 3 * D:4 * D].partition_broadcast(4), reads=[m_d], writes=[shf])
                P.dma("sp", wmod[4 * s:4 * s + 4, :], T["m_d"][1 + s, 4 * D:5 * D].partition_broadcast(4), reads=[m_d], writes=[wmod])
                P.dma("sp", gtf[4 * s:4 * s + 4, :], T["m_d"][1 + s, 5 * D:6 * D].partition_broadcast(4), reads=[m_d], writes=[gtf])
        nr = ST if sample else 128
        stt(P, "dve", wmod, wmod[0:nr, :], wmod[0:nr, :], 1.0, tmp[0:nr, :], ALU.add, ALU.mult, rd=[wmod, tmp])

    wr = sb("wr", [128, 8, NE])
    P.dma("sp", wr[:], T["w_router"].rearrange("(kc p) e -> p kc e", p=128), writes=[wr])
    brb = sb("brb", [128, NE])
    P.dma("sp", brb[:], T["b_router"][0, :].partition_broadcast(128), writes=[brb])
    bdn = sb("bdn", [NE, D])
    P.dma("sp", bdn[:], T["b_down"], writes=[bdn])
    bup = sb("bup", [NE, 2 * D])
    P.dma("sp", bup[:], T["b_up"], writes=[bup])
    bupT = sb("bupT", [128, 16, NE])
    pB = Rot([ps("pB%d" % i, [128, 512]) for i in range(2)])
    for c in range(16):
        pp = pB.get()
        tr32(P, pp, pp[:, 0:NE], bup[:, c * 128:(c + 1) * 128], ident[0:NE, 0:NE], [bup, cst])
        cp(P, "dve", bupT, bupT[:, c, :], pp[:, 0:NE], rd=[pp])
    H2T = sb("H2T", [128, 8, MAXS * 128], BF16)
    acc = [sb("acc%d" % i, [128, D]) for i in range(MAXS)]
    G = sb("G", [128, MAXS, NE])
    wup = Rot([sb("wup%d" % i, [128, 8, 2 * D], BF16) for i in range(2)])
    wdn = Rot([sb("wdn%d" % i, [128, 8, D], BF16) for i in range(2)])
    actT = sb("actT", [128, 8, 512], BF16)
    gsb = Rot([sb("gsb%d" % i, [128, 512]) for i in range(2)])
    sgs = Rot([sb("sgs%d" % i, [128, 512]) for i in range(1)])
    lsb = Rot([sb("lsb%d" % i, [128, 512]) for i in range(1)])
    xt = Rot([sb("x%d" % i, [128, D]) for i in range(2)])
    h2Tf = sb("h2Tf", [128, 8, 128])
    ss = sb("ss", [128, 8]); lg = sb("lg", [128, NE]); mx8 = sb("mx8", [128, 8]); msk = sb("msk", [128, NE]); ex = sb("ex", [128, NE])
    gT = sb("gT", [NE, 128])
    pG = Rot([ps("pG%d" % i, [128, 512]) for i in range(2)])
    pL = Rot([ps("pL%d" % i, [128, 512]) for i in range(2)])
    pY = Rot([ps("pY%d" % i, [128, 512]) for i in range(2)])
    x1_d = X.x1_d
    wupv = T["w_up"].rearrange("e (kc p) n -> e p kc n", p=128)
    wdnv = T["w_down"].rearrange("e (kc p) n -> e p kc n", p=128)
    cur_mod = [None]

    for tiles in passes:
        for si, (row, n, is_s, out_ap) in enumerate(tiles):
            if cur_mod[0] != is_s:
                load_mods(is_s)
                cur_mod[0] = is_s
            x = xt.get()
            P.dma("sp", x[0:n, :], T["x1_d"][row:row + n, :], reads=[x1_d], writes=[x])
            actf(P, h2, h2[0:n, :], x[0:n, :], AF.Square, rd=[x], accum=ss[0:n, 0:1], wr_extra=[ss])
            rsqrt_col(P, ss, ss[0:n, 2:3], ss[0:n, 0:1], 1.0 / D, EPS, [ss])
            stt(P, "dve", h2, h2[0:n, :], x[0:n, :], ss[0:n, 2:3], wmod[0:n, :], ALU.mult, ALU.mult, rd=[x, ss, wmod])
            tt(P, "pool", h2, h2[0:n, :], h2[0:n, :], shf[0:n, :], ALU.add, rd=[h2, shf])
            for half in range(2):
                pp = pB.get()
                for k4 in range(4):
                    kc = half * 4 + k4
                    tr32(P, pp, pp[:, k4 * 128:k4 * 128 + n], h2[0:n, kc * 128:(kc + 1) * 128], ident[0:n, 0:n], [h2, cst])
                cp(P, "dve", h2Tf, h2Tf[:, half * 4:(half + 1) * 4, 0:n], pp[:, :].rearrange("p (k t) -> p k t", k=4)[:, :, 0:n], rd=[pp])
            cp(P, "pool", H2T, H2T[:, :, si * 128:si * 128 + n], h2Tf[:, :, 0:n], rd=[h2Tf])
            pp = pB.get()
            for kc in range(8):
                mm(P, pp, pp[0:n, 0:NE], h2Tf[:, kc, 0:n], wr[:, kc, :], [h2Tf, wr], start=(kc == 0), stop=(kc == 7))
            tt(P, "dve", lg, lg[0:n, :], pp[0:n, 0:NE], brb[0:n, :], ALU.add, rd=[pp, brb])
            P.op("dve", lambda e, n=n: e.max(mx8[0:n, :], lg[0:n, :]), reads=[lg], writes=[mx8])
            ts(P, "dve", msk, msk[0:n, :], lg[0:n, :], mx8[0:n, 3:4], None, ALU.is_ge, rd=[lg, mx8])
            ts(P, "dve", mx8, mx8[0:n, 7:8], mx8[0:n, 0:1], -1.0, None, ALU.mult, rd=[mx8])
            actf(P, ex, ex[0:n, :], lg[0:n, :], AF.Exp, rd=[lg, mx8], bias=mx8[0:n, 7:8])
            tt(P, "dve", ex, ex[0:n, :], ex[0:n, :], msk[0:n, :], ALU.mult, rd=[ex, msk])
            P.op("dve", lambda e, n=n: e.reduce_sum(ss[0:n, 4:5], ex[0:n, :], AX.X), reads=[ex], writes=[ss])
            P.op("dve", lambda e, n=n: e.reciprocal(ss[0:n, 5:6], ss[0:n, 4:5]), reads=[ss], writes=[ss])
            ts(P, "dve", G, G[0:n, si, :], ex[0:n, :], ss[0:n, 5:6], None, ALU.mult, rd=[ex, ss])
            pp = pB.get()
            tr32(P, pp, pp[0:NE, 0:n], G[0:n, si, :], ident[0:n, 0:n], [G, cst])
            cp(P, "dve", gT, gT[:, 0:n], pp[0:NE, 0:n], rd=[pp])
            for half in range(2):
                pp = pB.get()
                mm(P, pp, pp[0:n, :], gT[:, 0:n], bdn[:, half * 512:(half + 1) * 512], [gT, bdn])
                cp(P, "act" if half == 0 else "dve", acc[si], acc[si][0:n, half * 512:(half + 1) * 512], pp[0:n, :], rd=[pp])
        groups = []
        si = 0
        while si < len(tiles):
            g = list(range(si, min(si + 4, len(tiles))))
            groups.append(g)
            si += 4
        for e_ in range(ne):
            wu = wup.get(); wd = wdn.get()
            for kc in range(8):
                P.dma("pool", wu[:, kc, :], wupv[e_, :, kc, :], writes=[wu])
            for kc in range(8):
                P.dma("pool", wd[:, kc, :], wdnv[e_, :, kc, :], writes=[wd])
            for g in groups:
                c0 = g[0] * 128
                ncol = (g[-1] - g[0]) * 128 + tiles[g[-1]][1]
                for fc in range(8):
                    pg = pG.get(); pl = pL.get()
                    for kc in range(8):
                        mm(P, pg, pg[:, 0:ncol], wu[:, kc, fc * 128:(fc + 1) * 128], H2T[:, kc, c0:c0 + ncol], [wu, H2T], start=(kc == 0), stop=(kc == 7))
                    for kc in range(8):
                        mm(P, pl, pl[:, 0:ncol], wu[:, kc, D + fc * 128:D + (fc + 1) * 128], H2T[:, kc, c0:c0 + ncol], [wu, H2T], start=(kc == 0), stop=(kc == 7))
                    gs = gsb.get(); sg = sgs.get(); ls = lsb.get()
                    ts(P, "dve", gs, gs[:, 0:ncol], pg[:, 0:ncol], bupT[:, fc, e_:e_ + 1], 7.0, ALU.add, ALU.min, rd=[pg, bupT])
                    actf(P, sg, sg[:, 0:ncol], gs[:, 0:ncol], AF.Sigmoid, rd=[gs], scale=1.702)
                    ts(P, "dve", ls, ls[:, 0:ncol], pl[:, 0:ncol], bupT[:, 8 + fc, e_:e_ + 1], 7.0, ALU.add, ALU.min, rd=[pl, bupT])
                    ts(P, "pool", ls, ls[:, 0:ncol], ls[:, 0:ncol], -7.0, 1.0, ALU.max, ALU.add, rd=[ls])
                    tt(P, "pool", gs, gs[:, 0:ncol], gs[:, 0:ncol], sg[:, 0:ncol], ALU.mult, rd=[gs, sg])
                    tt(P, "dve", actT, actT[:, fc, 0:ncol], gs[:, 0:ncol], ls[:, 0:ncol], ALU.mult, rd=[gs, ls])
                for si in g:
                    n = tiles[si][1]
                    o0 = (si - g[0]) * 128
                    for half in range(2):
                        py = pY.get()
                        for fc in range(8):
                            mm(P, py, py[0:n, :], actT[:, fc, o0:o0 + n], wd[:, fc, half * 512:(half + 1) * 512], [actT, wd], start=(fc == 0), stop=(fc == 7))
                        a = acc[si]
                        stt(P, "dve", a, a[0:n, half * 512:(half + 1) * 512], py[0:n, :], G[0:n, si, e_:e_ + 1], a[0:n, half * 512:(half + 1) * 512],
                            ALU.mult, ALU.add, rd=[py, G, a])
        for si, (row, n, is_s, out_ap) in enumerate(tiles):
            if cur_mod[0] != is_s:
                load_mods(is_s)
                cur_mod[0] = is_s
            x = xt.get()
            P.dma("sp", x[0:n, :], T["x1_d"][row:row + n, :], reads=[x1_d], writes=[x])
            a = acc[si]
            tt(P, "pool", a, a[0:n, :], a[0:n, :], gtf[0:n, :], ALU.mult, rd=[a, gtf])
            tt(P, "dve", a, a[0:n, :], a[0:n, :], x[0:n, :], ALU.add, rd=[a, x])
            actf(P, h2, h2[0:n, :], a[0:n, :], AF.Square, rd=[a], accum=ss[0:n, 0:1], wr_extra=[ss])
            rsqrt_col(P, ss, ss[0:n, 2:3], ss[0:n, 0:1], 1.0 / D, EPS, [ss])
            stt(P, "dve", h2, h2[0:n, :], a[0:n, :], ss[0:n, 2:3], nwf[0:n, :], ALU.mult, ALU.mult, rd=[a, ss, nwf])
            ob = Buf(out_ap)
            P.dma("sp", out_ap, h2[0:n, :], reads=[h2], writes=[ob])
            X.outs.append(ob)


NKS = 8192 + 128


def make_sample_onehots():
    out = np.zeros((4, 33, NKS), np.float32)
    for t in range(4):
        d = np.full(NKS, -1, np.int64)
        d[:8192] = 8192 + t - np.arange(8192)
        for t2 in range(4):
            d[8192 + t2] = t - t2
        out[t] = make_bucket_onehot(d)
    return out.reshape(4 * 33, NKS)


def phase_a2s(P, nc, es, T, X):
    sb, ps = allocators(nc, es, "a2s")
    scale = 64.0 ** -0.5
    nseq = getattr(X, "nseq", SB)
    cst = sb("cst", [128, NCST])
    P.dma("sp", cst[:], T["cst"], writes=[cst])
    identb = sb("identb", [128, 128], BF16)
    cp(P, "dve", identb, identb[:], cst[:, C_IDENT:C_IDENT + 128], rd=[cst])
    relb = sb("relb", [33, 8])
    P.op("dve", lambda e: e.memset(relb[:], NEG), writes=[relb])
    P.dma("sp", relb[0:32, :], T["rel_bias"], writes=[relb])
    lhs_t = sb("lhs_t", [33, 4, 32])
    P.op("dve", lambda e: e.memset(lhs_t[:], 0.0), writes=[lhs_t])
    for t in range(4):
        cp(P, "dve", lhs_t, lhs_t[:, t, t * 8:(t + 1) * 8], relb[:, :], rd=[relb])
    tab = sb("tab", [32, NKS])
    ohs = Rot([sb("ohs%d" % i, [33, 4, 512]) for i in range(2)])
    pS = Rot([ps("pS%d" % i, [128, 512]) for i in range(2)])
    ohv = T["ohs"].rearrange("(t b) n -> b t n", t=4)
    for j in range(0, NKS, 512):
        w = min(512, NKS - j)
        o = ohs.get()
        P.dma("sp", o[:, :, 0:w], ohv[:, :, j:j + w], writes=[o])
        pp = pS.get()
        for t in range(4):
            mm(P, pp, pp[0:32, 0:w], lhs_t[:, t, :], o[:, t, 0:w], [lhs_t, o], start=(t == 0), stop=(t == 3))
        actf(P, tab, tab[:, j:j + w], pp[0:32, 0:w], AF.Exp, rd=[pp])
    pt = sb("pt", [128, SB * 64], I32)
    P.dma("sp", pt[:, :], T["page_table"].rearrange("s j -> (s j)").partition_broadcast(128), writes=[pt])
    ptf = sb("ptf", [128, SB * 64])
    cp(P, "dve", ptf, ptf[:, :], pt[:, :], rd=[pt])
    pcol = sb("pcol", [128, 1])
    P.dma("sp", pcol[:, :], T["pcol"], writes=[pcol])
    ts(P, "dve", ptf, ptf[:, :], ptf[:, :], 128.0, pcol[:, 0:1], ALU.mult, ALU.add, rd=[ptf, pcol])
    pidx = sb("pidx", [128, SB * 64], I32)
    cp(P, "dve", pidx, pidx[:, :], ptf[:, :], rd=[ptf])
    sel = sb("sel", [32, 4])
    P.dma("sp", sel[:, :], T["sel01"], writes=[sel])
    qf = sb("qf", [4, 512]); qb = sb("qb", [4, 512], BF16)
    lq = sb("lq", [128, 4, 32], BF16)
    P.op("dve", lambda e: e.memset(lq[:], 0.0), writes=[lq])
    kpg = Rot([sb("kpg%d" % i, [128, 256], BF16) for i in range(4)])
    vall = sb("vall", [128, 65, 256], BF16)
    vslots = [bv2(vall, j, "v%d" % j) for j in range(65)]
    vslots = [Sub(vall.t, (lambda idx, j=j: (idx[0], j) + tuple(idx[1:])), "v%d" % j) for j in range(65)]
    ktd = Rot([sb("ktd%d" % i, [128, 4, 128], BF16) for i in range(3)])
    knew = sb("knew", [128, 256], BF16)
    kdups = Rot([sb("kdd%d" % i, [128, 4, 2, 64], BF16) for i in range(3)])
    kvn = sb("kvn", [4, 512])
    Sa = sb("Sa", [32, NKS]); Ee = sb("Ee", [32, NKS]); Pb = sb("Pb", [32, NKS], BF16)
    PTs = Rot([sb("PTs%d" % i, [128, 16, 32], BF16) for i in range(2)])
    gt = sb("gt", [32, 32]); mx8 = sb("mx8", [32, 8]); m01 = sb("m01", [32, 32]); rm = sb("rm", [32, 1]); nm = sb("nm", [32, 1])
    rs = sb("rs", [32, 1]); rcp = sb("rcp", [32, 1]); osel = sb("osel", [32, 4, 64]); ao = sb("ao", [32, 64])
    pT = Rot([ps("pT%d" % i, [128, 8, 128], BF16) for i in range(2)])
    pP = Rot([ps("pP%d" % i, [128, 16, 32], BF16) for i in range(2)])
    pO = ps("pO", [128, 512])
    mix_d = X.mix_d
    nk_sd = Buf(T["nk_s"]); nv_sd = Buf(T["nv_s"])
    ck = T["cache_k"]; cv = T["cache_v"]
    P.op("pool", lambda e: e.memset(knew[:], 0.0), writes=[knew])

    for s in range(nseq):
        P.dma("sp", qf[:, :], T["q_s"][4 * s:4 * s + 4, :], reads=[X.q_sd], writes=[qf])
        cp(P, "dve", qb, qb[:, :], qf[:, :], rd=[qf])
        pq = pT.get()
        for hk in range(4):
            tr(P, pq, pq[:, hk, 0:4], qb[:, hk * 128:(hk + 1) * 128], identb[0:4, 0:4], [qb, identb])
        lqv = lq[:, :, :].rearrange("p h (t q) -> p h t q", q=8)
        for hk in range(4):
            cp(P, "dve", lq, lqv[0:64, hk, :, 2 * hk], pq[0:64, hk, 0:4], rd=[pq])
            cp(P, "act", lq, lqv[64:128, hk, :, 2 * hk + 1], pq[64:128, hk, 0:4], rd=[pq])
        P.dma("sp", kvn[:, 0:256], T["nk_s"][4 * s:4 * s + 4, :], reads=[X.nks_buf], writes=[kvn])
        P.dma("sp", kvn[:, 256:512], T["nv_s"][4 * s:4 * s + 4, :], reads=[X.nvs_buf], writes=[kvn])
        cp(P, "pool", knew, knew[0:4, :], kvn[:, 0:256], rd=[kvn])
        vn = vslots[64]
        P.op("pool", lambda e, vn=vn: e.memset(vn[:, :], 0.0), writes=[vn])
        cp(P, "pool", vn, vn[0:4, :], kvn[:, 256:512], rd=[kvn])
        pp = None
        for j in range(65):
            if j < 64:
                kp = kpg.get()
                vj = vslots[j]

                def issue(eng, kp=kp, vj=vj, idx=s * 64 + j):
                    eng.indirect_dma_start(out=kp[:, :], out_offset=None, in_=ck[:, :],
                                           in_offset=bass.IndirectOffsetOnAxis(ap=pidx[:, idx:idx + 1], axis=0)).then_inc(P._k_sem, 16)
                    return eng.indirect_dma_start(out=vj[:, :], out_offset=None, in_=cv[:, :],
                                                  in_offset=bass.IndirectOffsetOnAxis(ap=pidx[:, idx:idx + 1], axis=0))
                P.dma_custom("pool", issue, reads=[pidx], writes=[kp, vj], extra=1)
                ksrc = kp
            else:
                ksrc = knew
            pk = pT.get()
            kdd = kdups.get()
            cp(P, "pool" if j % 2 == 0 else "dve", kdd, kdd[:, :, :, :],
               ksrc[:, :].rearrange("p (h d) -> p h d", h=4).unsqueeze(2).to_broadcast([128, 4, 2, 64]), rd=[ksrc])
            for hk in range(4):
                tr(P, pk, pk[:, hk, :], kdd[:, hk, :, :].rearrange("p r d -> p (r d)"), identb[:, :], [kdd, identb])
            kd = ktd.get()
            cp(P, "act" if j % 2 == 0 else "dve", kd, kd[:, :, :], pk[:, 0:4, :], rd=[pk])
            if j % 4 == 0:
                pp = pS.get()
            for hk in range(4):
                mm(P, pp, pp[0:32, (j % 4) * 128:(j % 4 + 1) * 128], lq[:, hk, :], kd[:, hk, :], [lq, kd], start=(hk == 0), stop=(hk == 3))
            if j % 4 == 3 or j == 64:
                c0 = (j // 4) * 512
                w = (j % 4 + 1) * 128
                cp(P, "dve", Sa, Sa[:, c0:c0 + w], pp[0:32, 0:w], rd=[pp])
        P.op("dve", lambda e: e.tensor_reduce(gt[:, :], Sa[:, 0:8192].rearrange("p (n k) -> p n k", k=256), AX.X, ALU.add), reads=[Sa], writes=[gt])
        P.op("dve", lambda e: e.max(mx8[:], gt[:]), reads=[gt], writes=[mx8])
        ts(P, "dve", m01, m01[:], gt[:], mx8[:, 2:3], None, ALU.is_ge, rd=[gt, mx8])
        P.op("dve", lambda e: e.reduce_max(rm[:], Sa[:, :], AX.X), reads=[Sa], writes=[rm])
        ts(P, "dve", nm, nm[:], rm[:], -scale, None, ALU.mult, rd=[rm])
        actf(P, Ee, Ee[:, :], Sa[:, :], AF.Exp, rd=[Sa, nm], scale=scale, bias=nm[:, 0:1])
        tt(P, "pool", Ee, Ee[:, 0:8192].rearrange("p (n k) -> p n k", k=256), Ee[:, 0:8192].rearrange("p (n k) -> p n k", k=256),
           m01[:, :].unsqueeze(2).to_broadcast([32, 32, 256]), ALU.mult, rd=[Ee, m01])
        tt(P, "dve", Ee, Ee[:, :], Ee[:, :], tab[:, :], ALU.mult, rd=[Ee, tab])
        P.op("dve", lambda e: e.reduce_sum(rs[:, 0:1], Ee[:, :], AX.X), reads=[Ee], writes=[rs])
        cp(P, "pool", Pb, Pb[:, :], Ee[:, :], rd=[Ee])
        for g0 in range(0, 65, 16):
            g1 = min(g0 + 16, 65)
            ptp = pP.get()
            for j in range(g0, g1):
                tr(P, ptp, ptp[:, j - g0, :], Pb[:, j * 128:(j + 1) * 128], identb[0:32, 0:32], [Pb, identb])
            pts = PTs.get()
            cp(P, "act" if (g0 // 16) % 2 == 0 else "dve", pts, pts[:, 0:g1 - g0, :], ptp[:, 0:g1 - g0, :], rd=[ptp])
            for j in range(g0, g1):
                mm(P, pO, pO[0:32, 0:256], pts[:, j - g0, :], vslots[j][:, :], [pts, vslots[j]], start=(j == 0), stop=(j == 64))
        tt(P, "dve", osel, osel[:, :, :], pO[0:32, 0:256].rearrange("p (h d) -> p h d", h=4), sel[:, :].unsqueeze(2).to_broadcast([32, 4, 64]), ALU.mult,
           rd=[pO, sel])
        P.op("dve", lambda e: e.tensor_reduce(ao[:, :], osel[:, :, :].rearrange("p h d -> p d h"), AX.X, ALU.add), reads=[osel], writes=[ao])
        P.op("dve", lambda e: e.reciprocal(rcp[:], rs[:]), reads=[rs], writes=[rcp])
        ts(P, "dve", ao, ao[:, :], ao[:, :], rcp[:, 0:1], None, ALU.mult, rd=[ao, rcp])
        for t in range(4):
            P.dma("sp", T["mix_d"][SEQ + 4 * s + t, 0:512].rearrange("(h d) -> h d", h=8), ao[8 * t:8 * t + 8, :], reads=[ao], writes=[mix_d])


ALL_STAGES = ("p0", "a1", "a2", "smp", "a3", "b")
_NC_CACHE = {}


def kernel(**inputs):
    inp = {k: np.asarray(v) for k, v in inputs.items()}
    if "nc" not in _NC_CACHE:
        _NC_CACHE["nc"] = build(ALL_STAGES)
    nc = _NC_CACHE["nc"]
    in_maps = [shard_inputs(inp, c) for c in range(NCORES)]
    res = run_bass_kernel_spmd(nc, in_maps, core_ids=list(range(NCORES)))
    R = res.results
    cat = lambda name: [np.asarray(R[c][name]) for c in range(NCORES)]
    y_p = np.stack(cat("y_p"), 0).reshape(8, SEQ, D)
    y_s = np.concatenate(cat("y_s"), 0).reshape(128, 4, D)
    nk_p = np.stack(cat("nk_p"), 0).reshape(1, 8, SEQ, 4, 64)
    nv_p = np.stack(cat("nv_p"), 0).reshape(1, 8, SEQ, 4, 64)
    nc_p = np.stack(cat("nc_p"), 0).reshape(1, 8, 3, 1536)
    ns_p = np.stack(cat("ns_p"), 0).reshape(1, 8, 4, 128, 128)
    nk_s = np.concatenate(cat("nk_s"), 0).reshape(1, 128, 4, 4, 64)
    nv_s = np.concatenate(cat("nv_s"), 0).reshape(1, 128, 4, 4, 64)
    nc_s = np.concatenate(cat("nc_s"), 0).reshape(1, 128, 3, 1536)
    ns_s = np.concatenate(cat("ns_s"), 0).reshape(1, 128, 4, 128, 128)
    return tuple(a.astype(np.float32, copy=False) for a in (y_p, y_s, nk_p, nv_p, nc_p, ns_p, nk_s, nv_s, nc_s, ns_s))
```

```python
import numpy as np
import concourse.bass as bass
import concourse.mybir as mybir
from concourse.bass_utils import run_bass_kernel_spmd

F32 = mybir.dt.float32
BF16 = mybir.dt.bfloat16
I32 = mybir.dt.int32
AF = mybir.ActivationFunctionType
ALU = mybir.AluOpType
AX = mybir.AxisListType

NCORES = 8
D = 1024
SEQ = 4096
NT = SEQ // 128
SB = 16
ST = SB * 4
IN_W = 3080
NEG = -30000.0
EPS = 1e-6
NE = 32
SEM_LIMIT = 20000


class Buf:
    __slots__ = ("t", "lw", "rd", "name")

    def __init__(self, t, name=""):
        self.t = t
        self.lw = None
        self.rd = []
        self.name = name

    def __getitem__(self, idx):
        return self.t[idx]


class Sub(Buf):
    __slots__ = ("pre",)

    def __init__(self, t, pre, name=""):
        super().__init__(t, name)
        self.pre = pre

    def __getitem__(self, idx):
        if not isinstance(idx, tuple):
            idx = (idx,)
        return self.t[self.pre(idx) if callable(self.pre) else tuple(self.pre) + idx]


class Prog:
    COMPUTE = ("pe", "act", "dve", "pool")

    def __init__(self, nc):
        self.nc = nc
        self.eng = {"pe": nc.tensor, "act": nc.scalar, "dve": nc.vector, "pool": nc.gpsimd, "sp": nc.sync}
        self.rec = {k: [] for k in self.eng}
        self.sems = {}
        self.cur = {}
        self.seen = {k: {} for k in self.eng}
        self.nsem = 0
        for k in self.eng:
            self._new_eng_sem(k)
        self.dq = {}
        self.dq_next = {}
        for q in ("sp", "pool", "act"):
            lst = []
            for i in range(12):
                key = self._alloc_sem("d%s%d" % (q, i))
                lst.append([key, 0])
            self.dq[q] = lst
            self.dq_next[q] = 0

    def _alloc_sem(self, name):
        key = "%s_%d" % (name, self.nsem)
        self.nsem += 1
        self.sems[key] = self.nc.alloc_semaphore(key)
        return key

    def _new_eng_sem(self, e):
        self.cur[e] = [self._alloc_sem("e" + e), 0]

    def _collect(self, e, reads, writes, is_dma):
        need = {}

        def add(dep, raw):
            if dep is None:
                return
            key, val, prod = dep
            if (not is_dma) and (not raw) and prod == e and e == "pe":
                return
            if self.seen[e].get(key, 0) >= val:
                return
            if need.get(key, 0) < val:
                need[key] = val

        for b in reads:
            add(b.lw, True)
        for b in writes:
            add(b.lw, False)
            for r in b.rd:
                add(r, False)
        return need

    def _emit_waits(self, e, need):
        for key, val in need.items():
            self.seen[e][key] = val
            sem = self.sems[key]
            self.rec[e].append(lambda eng, sem=sem, val=val: eng.wait_ge(sem, val))

    def op(self, e, fn, reads=(), writes=(), inc=True):
        need = self._collect(e, reads, writes, False)
        self._emit_waits(e, need)
        pend = getattr(self, "pending", None)
        if pend is None:
            pend = self.pending = {k: False for k in self.eng}
        if self.cur[e][1] >= SEM_LIMIT and not pend[e]:
            self._new_eng_sem(e)
        cur = self.cur[e]
        if inc:
            cur[1] += 1
            key, val = cur[0], cur[1]
            sem = self.sems[key]
            self.rec[e].append(lambda eng, fn=fn, sem=sem: fn(eng).then_inc(sem, 1))
            pend[e] = False
        else:
            key, val = cur[0], cur[1] + 1
            self.rec[e].append(lambda eng, fn=fn: fn(eng))
            pend[e] = True
        dep = (key, val, e)
        for b in writes:
            b.lw = dep
            b.rd = []
        for b in reads:
            b.rd.append(dep)

    def dma(self, q, out_ap, in_ap, reads=(), writes=(), **kw):
        need = self._collect(q, reads, writes, True)
        lst = self.dq[q]
        i = self.dq_next[q]
        self.dq_next[q] = (i + 1) % len(lst)
        slot = lst[i]
        if slot[1] > 0 and self.seen[q].get(slot[0], 0) < slot[1]:
            if need.get(slot[0], 0) < slot[1]:
                need[slot[0]] = slot[1]
        self._emit_waits(q, need)
        slot[1] += 16
        key, val = slot[0], slot[1]
        sem = self.sems[key]
        self.rec[q].append(lambda eng, o=out_ap, i_=in_ap, sem=sem, kw=kw: eng.dma_start(out=o, in_=i_, **kw).then_inc(sem, 16))
        dep = (key, val, "dma")
        for b in writes:
            b.lw = dep
            b.rd = []
        for b in reads:
            b.rd.append(dep)

    def dma_custom(self, q, issue, reads=(), writes=(), extra=0):
        need = self._collect(q, reads, writes, True)
        lst = self.dq[q]
        slots = []
        for _ in range(1 + extra):
            i = self.dq_next[q]
            self.dq_next[q] = (i + 1) % len(lst)
            slot = lst[i]
            if slot[1] > 0 and self.seen[q].get(slot[0], 0) < slot[1]:
                if need.get(slot[0], 0) < slot[1]:
                    need[slot[0]] = slot[1]
            slots.append(slot)
        self._emit_waits(q, need)
        deps = []
        for slot in slots:
            slot[1] += 16
            deps.append((slot[0], slot[1], "dma"))
        sems = [self.sems[sl[0]] for sl in slots]

        def run(eng, issue=issue, sems=sems):
            self._k_sem = sems[0]
            issue(eng).then_inc(sems[-1], 16)
        self.rec[q].append(run)
        for b, dep in zip(writes, deps):
            b.lw = dep
            b.rd = []
        for b in reads:
            b.rd.extend(deps)

    def wait_all(self, e, bufs):
        need = {}
        for b in bufs:
            if b.lw is not None:
                key, val, _ = b.lw
                if self.seen[e].get(key, 0) < val and need.get(key, 0) < val:
                    need[key] = val
        self._emit_waits(e, need)

    def barrier(self):
        for e in self.eng:
            need = {}
            for e2, (key, val) in self.cur.items():
                if val > 0 and self.seen[e].get(key, 0) < val:
                    need[key] = val
            for q, lst in self.dq.items():
                for key, val in lst:
                    if val > 0 and self.seen[e].get(key, 0) < val:
                        need[key] = val
            self._emit_waits(e, need)

    def emit(self):
        nc = self.nc
        rec = self.rec
        self.rec = {k: [] for k in self.eng}
        self._emit(rec)

    def _emit(self, rec):
        nc = self.nc
        with nc.Block() as block:
            @block.sync
            def _(eng):
                for f in rec["sp"]:
                    f(eng)

            @block.scalar
            def _(eng):
                for f in rec["act"]:
                    f(eng)

            @block.vector
            def _(eng):
                for f in rec["dve"]:
                    f(eng)

            @block.gpsimd
            def _(eng):
                for f in rec["pool"]:
                    f(eng)

            @block.tensor
            def _(eng):
                for f in rec["pe"]:
                    f(eng)


def make_consts():
    c = {}
    c["ident"] = np.eye(128, dtype=np.float32)
    j = np.arange(128)[:, None]
    i = np.arange(128)[None, :]
    c["tri"] = (j <= i).astype(np.float32)
    c["negus"] = np.where(i < j, 0.0, NEG).astype(np.float32)
    c["negut"] = np.where(i >= j, 0.0, NEG).astype(np.float32)
    c["jrev"] = np.eye(128, dtype=np.float32)[::-1].copy()
    return np.concatenate([c["ident"], c["tri"], c["negus"], c["negut"], c["jrev"]], axis=1)


C_IDENT, C_TRI, C_NEGUS, C_NEGUT, C_JREV = 0, 128, 256, 384, 512
NCST = 640
RLEN = 4224


def rel_bucket_np(dist):
    n = np.maximum(dist, 0)
    nf = np.maximum(n, 16).astype(np.float32)
    large = 16 + (np.log(nf / np.float32(16)) / np.float32(np.log(4096 / 16)) * np.float32(16)).astype(np.int32)
    return np.where(n < 16, n, np.minimum(large, 31))


def make_bucket_onehot(dists):
    oh = np.zeros((33, len(dists)), np.float32)
    b = rel_bucket_np(dists)
    for i, d in enumerate(dists):
        if d < 0:
            oh[32, i] = 1.0
        else:
            oh[b[i], i] = 1.0
    return oh


class Ctx:
    pass


def allocators(nc, es, prefix):
    def sb(name, shape, dt=F32):
        return Buf(es.enter_context(nc.sbuf_tensor("%s_%s" % (prefix, name), list(shape), dt)), name)

    def ps(name, shape, dt=F32):
        return Buf(es.enter_context(nc.psum_tensor("%s_%s" % (prefix, name), list(shape), dt)), name)
    return sb, ps


def declare_io(nc, npool=10240, ne_store=NE, dbg=False):
    T = {}

    def inp(name, shape, dt=F32):
        T[name] = nc.dram_tensor(name, list(shape), dt, kind="ExternalInput").ap()

    def outp(name, shape, dt=F32):
        T[name] = nc.dram_tensor(name, list(shape), dt, kind="ExternalOutput").ap()

    def scr(name, shape, dt=F32):
        T[name] = nc.dram_tensor(name, list(shape), dt, kind="Internal").ap()

    inp("xp", [SEQ, D]); inp("xs", [ST, D]); inp("cc", [1 + SB, D])
    inp("w_ada", [D, 6 * D]); inp("b_ada", [1, 6 * D]); inp("nw", [3, D])
    inp("w_in", [D, IN_W]); inp("rel_bias", [32, 8]); inp("conv_w", [4, 1536])
    inp("a_log", [1, 4]); inp("dt_bias", [1, 4]); inp("gnw", [1, 128]); inp("w_out", [D, D])
    inp("w_router", [D, NE]); inp("b_router", [1, NE])
    inp("w_up", [ne_store, D, 2 * D]); inp("b_up", [NE, 2 * D]); inp("w_down", [ne_store, D, D]); inp("b_down", [NE, D])
    inp("cache_k", [npool * 128, 256]); inp("cache_v", [npool * 128, 256])
    inp("state_conv", [SB * 3, 1536]); inp("state_ssm", [SB * 4 * 128, 128])
    inp("page_table", [SB, 64], I32)
    inp("cst", [128, NCST]); inp("ohp", [33, RLEN]); inp("ohs", [4 * 33, NKS]); inp("sel01", [32, 4]); inp("pcol", [128, 1])
    outp("y_p", [SEQ, D]); outp("y_s", [ST, D])
    outp("nk_p", [SEQ, 256]); outp("nv_p", [SEQ, 256]); outp("nc_p", [3, 1536]); outp("ns_p", [4 * 128, 128])
    outp("nk_s", [ST, 256]); outp("nv_s", [ST, 256]); outp("nc_s", [SB * 3, 1536]); outp("ns_s", [SB * 4 * 128, 128])
    if dbg:
        outp("dbg", [128, 8192])
    scr("m_d", [1 + SB, 6 * D])
    scr("rd", [8, RLEN])
    scr("q_s", [ST, 512])
    scr("wupb", [NE, 128, 8 * 2 * D], BF16)
    scr("wdnb", [NE, 128, 8 * D], BF16)
    scr("mix_d", [SEQ + ST, D])
    scr("x1_d", [SEQ + ST, D])
    return T


def phase0_ada(P, nc, es, T):
    R = 1 + SB
    sb, ps = allocators(nc, es, "p0")
    cst = sb("cst0", [128, NCST])
    cc = sb("cc", [R, D])
    ccT = sb("ccT", [128, 8, R])
    ones = sb("ones", [1, R])
    bada = sb("bada", [1, 6 * D])
    wch = [sb("wch%d" % i, [128, 8, 512]) for i in range(2)]
    mo = [sb("mo%d" % i, [R, 512]) for i in range(2)]
    pT = ps("pT", [128, 8, R])
    pm = [ps("pm%d" % i, [R, 512]) for i in range(2)]
    m_d = Buf(T["m_d"], "m_d")
    P.dma("sp", cst[:], T["cst"], writes=[cst])
    P.dma("sp", cc[:], T["cc"], writes=[cc])
    P.dma("sp", bada[:], T["b_ada"], writes=[bada])
    P.op("dve", lambda e: e.memset(ones[:], 1.0), writes=[ones])
    P.op("act", lambda e: e.activation(out=cc[:], in_=cc[:], func=AF.Silu), reads=[cc], writes=[cc])
    for kc in range(8):
        P.op("pe", lambda e, kc=kc: e.transpose(pT[:, kc, :], cc[:, kc * 128:(kc + 1) * 128], cst[0:R, 0:R]),
             reads=[cc, cst], writes=[pT])
    P.op("dve", lambda e: e.tensor_copy(ccT[:], pT[:]), reads=[pT], writes=[ccT])
    wv = T["w_ada"].rearrange("(kc p) n -> p kc n", p=128)
    for j in range(12):
        w = wch[j % 2]
        P.dma("sp", w[:], wv[:, :, j * 512:(j + 1) * 512], writes=[w])
        pp = pm[j % 2]
        for kc in range(8):
            P.op("pe", lambda e, kc=kc, w=w, pp=pp: e.matmul(pp[:], lhsT=ccT[:, kc, :], rhs=w[:, kc, :], start=(kc == 0), stop=False),
                 reads=[ccT, w], writes=[pp])
        P.op("pe", lambda e, pp=pp, j=j: e.matmul(pp[:], lhsT=ones[:], rhs=bada[:, j * 512:(j + 1) * 512], start=False, stop=True),
             reads=[ones, bada], writes=[pp])
        o = mo[j % 2]
        P.op("act", lambda e, o=o, pp=pp: e.copy(out=o[:], in_=pp[:]), reads=[pp], writes=[o])
        P.dma("sp", T["m_d"][:, j * 512:(j + 1) * 512], o[:], reads=[o], writes=[m_d])
    return m_d


def build(stages=("p0",), npool=10240, ne_store=NE, dbg=False, **xkw):
    from contextlib import ExitStack
    nc = bass.Bass("TRN2", target_bir_lowering=False)
    T = declare_io(nc, npool, ne_store, dbg)
    P = Prog(nc)
    X = Ctx()
    X.T = T
    for k_, v_ in xkw.items():
        setattr(X, k_, v_)
    X.outs = []
    X.mix_d = Buf(T["mix_d"], "mix_d")
    X.x1_d = Buf(T["x1_d"], "x1_d")
    X.q_sd = Buf(T["q_s"], "q_s")
    X.ncs = Buf(T["nc_s"], "nc_s")
    if "p0" in stages:
        with ExitStack() as es:
            X.m_d = phase0_ada(P, nc, es, T)
            P.barrier()
            if dbg and stages == ("p0",):
                P.dma("sp", T["dbg"][0:17, 0:6144], T["m_d"], reads=[X.m_d])
                P.barrier()
            P.emit()
    if "a1" in stages:
        with ExitStack() as esA:
            alloc_attn_persist(nc, esA, X)
            with ExitStack() as es:
                alloc_a1_extra(nc, es, X)
                phase_a1(P, nc, es, T, X, sample=False)
                if dbg and "a2" not in stages:
                    P.dma("sp", T["dbg"][:, 0:4096], X.mix_d[0:128, :].rearrange("p (a b) -> p a b", a=1)[:, 0, :] if False else T["mix_d"][0:128, :].rearrange("p d -> p d")[:, :], reads=[X.mix_d]) if False else None
                P.barrier()
                P.emit()
            if "a2" in stages:
                with ExitStack() as es:
                    X.rdb = build_rel_table(P, nc, es, T, X, 8, "ohp", RLEN, "rd", "rt")
                    P.barrier()
                    P.emit()
                with ExitStack() as es:
                    phase_a2(P, nc, es, T, X)
                    P.barrier()
                    P.emit()
    if "smp" in stages:
        with ExitStack() as es:
            X.nks_buf = Buf(T["nk_s"], "nk_s"); X.nvs_buf = Buf(T["nv_s"], "nv_s")
            alloc_a1_extra(nc, es, X, "a1xs")
            phase_a1(P, nc, es, T, X, sample=True)
            P.barrier()
            P.emit()
        with ExitStack() as es:
            phase_a2s(P, nc, es, T, X)
            P.barrier()
            P.emit()
    if "a3" in stages:
        with ExitStack() as es:
            def tiles(gta_p, gta_s):
                for t in range(getattr(X, "ntile", NT)):
                    yield (t * 128, 128, T["xp"][t * 128:(t + 1) * 128, :], gta_p)
                if "smp" in stages:
                    yield (SEQ, 4 * getattr(X, "nseq", SB), T["xs"][0:4 * getattr(X, "nseq", SB), :], gta_s)
            phase_a3(P, nc, es, T, X, tiles)
            P.barrier()
            if dbg:
                nt_ = getattr(X, "ntile", NT)
                for t in range(min(nt_, 16)):
                    P.dma("sp", T["dbg"][:, t * 512:(t + 1) * 512], T["mix_d"][t * 128:(t + 1) * 128, 0:512], reads=[X.mix_d])
            P.barrier()
            P.emit()
    if "b" in stages:
        with ExitStack() as es:
            nt_ = getattr(X, "ntile", NT)
            alltiles = [(t * 128, 128, False, T["y_p"][t * 128:(t + 1) * 128, :]) for t in range(nt_)]
            per = getattr(X, "per_pass", 7)
            passes = [alltiles[i:i + per] for i in range(0, len(alltiles), per)]
            if "smp" in stages:
                ns_ = 4 * getattr(X, "nseq", SB)
                stile = (SEQ, ns_, True, T["y_s"][0:ns_, :])
                if passes and len(passes[-1]) < per:
                    passes[-1].append(stile)
                else:
                    passes.append([stile])
            phase_b(P, nc, es, T, X, passes, ne=getattr(X, "ne", NE))
            P.barrier()
            P.emit()
    return nc


def shard_inputs(inp, c):
    f = lambda a: np.ascontiguousarray(a)
    m = {}
    m["xp"] = f(inp["x_prompt"][c])
    m["xs"] = f(inp["x_sample"][c * SB:(c + 1) * SB].reshape(ST, D))
    m["cc"] = f(np.concatenate([inp["c_prompt"][c:c + 1], inp["c_sample"][c * SB:(c + 1) * SB]], axis=0))
    m["w_ada"] = f(inp["w_ada"][0]); m["b_ada"] = f(inp["b_ada"])
    m["nw"] = f(np.stack([inp["norm_attn_w"][0], inp["norm_ffn_w"][0], inp["norm_final_w"]], axis=0))
    m["w_in"] = f(inp["w_in"][0]); m["rel_bias"] = f(inp["rel_bias"]); m["conv_w"] = f(inp["conv_w"][0])
    m["a_log"] = f(inp["a_log"]); m["dt_bias"] = f(inp["dt_bias"]); m["gnw"] = f(inp["gdn_norm_w"])
    m["w_out"] = f(inp["w_out"][0]); m["w_router"] = f(inp["w_router"][0]); m["b_router"] = f(inp["b_router"])
    m["w_up"] = f(inp["w_up"][0]); m["b_up"] = f(inp["b_up"][0]); m["w_down"] = f(inp["w_down"][0]); m["b_down"] = f(inp["b_down"][0])
    m["cache_k"] = inp["cache_k"].reshape(-1, 256); m["cache_v"] = inp["cache_v"].reshape(-1, 256)
    m["state_conv"] = f(inp["state_conv"][0, c * SB:(c + 1) * SB].reshape(SB * 3, 1536))
    m["state_ssm"] = f(inp["state_ssm"][0, c * SB:(c + 1) * SB].reshape(SB * 4 * 128, 128))
    m["page_table"] = f(inp["page_table"][c * SB:(c + 1) * SB])
    m["cst"] = make_consts()
    m["ohp"] = make_bucket_onehot(4095 - np.arange(RLEN))
    m["ohs"] = make_sample_onehots()
    m["pcol"] = np.arange(128, dtype=np.float32).reshape(128, 1)
    m["sel01"] = (np.arange(4)[None, :] == ((np.arange(32) % 8) // 2)[:, None]).astype(np.float32)
    return m


def mm(P, ob, oap, lhsT, rhs, rd, start=True, stop=True, inc=None):
    P.op("pe", lambda e: e.matmul(oap, lhsT=lhsT, rhs=rhs, start=start, stop=stop), reads=rd, writes=[ob], inc=(stop if inc is None else inc))


def tr(P, ob, oap, in_ap, ident_ap, rd, inc=True):
    P.op("pe", lambda e: e.transpose(oap, in_ap, ident_ap), reads=rd, writes=[ob], inc=inc)


def tr32(P, ob, oap, in_ap, ident_ap, rd):
    P.op("pe", lambda e: e.matmul(oap, lhsT=in_ap, rhs=ident_ap, start=True, stop=True), reads=rd, writes=[ob])


def actf(P, ob, oap, in_ap, func, rd, bias=None, scale=None, accum=None, wr_extra=(), eng="act"):
    kw = {}
    if bias is not None:
        kw["bias"] = bias
    if scale is not None:
        kw["scale"] = scale
    if accum is not None:
        kw["accum_out"] = accum
    P.op(eng, lambda e: e.activation(out=oap, in_=in_ap, func=func, **kw), reads=rd, writes=[ob] + list(wr_extra))


def ts(P, eng, ob, oap, in0, s1, s2, op0, op1=None, rd=(), accum=None, wr_extra=()):
    kw = {}
    if op1 is not None:
        kw["op1"] = op1
    if accum is not None:
        kw["accum_out"] = accum
    P.op(eng, lambda e: e.tensor_scalar(oap, in0, s1, s2, op0, **kw), reads=rd, writes=[ob] + list(wr_extra))


def tt(P, eng, ob, oap, in0, in1, op, rd=()):
    P.op(eng, lambda e: e.tensor_tensor(oap, in0, in1, op), reads=rd, writes=[ob])


def stt(P, eng, ob, oap, in0, scalar, in1, op0, op1, rd=()):
    P.op(eng, lambda e: e.scalar_tensor_tensor(oap, in0, scalar, in1, op0, op1), reads=rd, writes=[ob])


def cp(P, eng, ob, oap, in_ap, rd=()):
    if eng == "act":
        P.op(eng, lambda e: e.copy(out=oap, in_=in_ap), reads=rd, writes=[ob])
    else:
        P.op(eng, lambda e: e.tensor_copy(oap, in_ap), reads=rd, writes=[ob])


def rsqrt_col(P, ob, oap, in_ap, scale, eps, rd, post=1.0):
    actf(P, ob, oap, in_ap, AF.Ln, rd=rd, scale=scale, bias=eps)
    if post != 1.0:
        actf(P, ob, oap, oap, AF.Exp, rd=[ob], scale=-0.5, bias=float(np.log(post)))
    else:
        actf(P, ob, oap, oap, AF.Exp, rd=[ob], scale=-0.5)


class Rot:
    def __init__(self, bufs):
        self.b = bufs
        self.i = 0

    def get(self):
        b = self.b[self.i % len(self.b)]
        self.i += 1
        return b


def phase_a1(P, nc, es, T, X, sample=False):
    pre = "a1s" if sample else "a1"
    sb, ps = allocators(nc, es, pre)
    ntile = getattr(X, 'nseq', SB) if sample else getattr(X, 'ntile', NT)
    TP = 4 if sample else 128
    TS = TP
    x_d = T["xs"] if sample else T["xp"]
    row0 = SEQ if sample else 0
    cst = sb("cst", [128, NCST])
    P.dma("sp", cst[:], T["cst"], writes=[cst])
    ident = cst[:, C_IDENT:C_IDENT + 128]
    tri = cst[:, C_TRI:C_TRI + 128]
    negus = cst[:, C_NEGUS:C_NEGUS + 128]
    negut = cst[:, C_NEGUT:C_NEGUT + 128]
    identb = sb("identb", [128, 128], BF16)
    cp(P, "dve", identb, identb[:], ident, rd=[cst])
    jrevb = sb("jrevb", [128, 128], BF16)
    cp(P, "dve", jrevb, jrevb[:], cst[:, C_JREV:C_JREV + 128], rd=[cst])
    wmod = sb("wmod", [128, D]); shb = sb("shb", [128, D]); tmp = sb("tmp", [128, D]); nwb = tmp; junk = tmp
    m_d = X.m_d
    if not sample:
        P.dma("sp", shb[:], T["m_d"][0, 0:D].partition_broadcast(128), reads=[m_d], writes=[shb])
        P.dma("sp", wmod[:], T["m_d"][0, D:2 * D].partition_broadcast(128), reads=[m_d], writes=[wmod])
    if sample:
        nwb2 = sb("nwb2", [4, D])
        P.dma("sp", nwb2[:], T["nw"][0, :].partition_broadcast(4), writes=[nwb2])
    if not sample:
        P.dma("sp", nwb[:], T["nw"][0, :].partition_broadcast(128), writes=[nwb])
        stt(P, "dve", wmod, wmod[0:TP, :], wmod[0:TP, :], 1.0, nwb[0:TP, :], ALU.add, ALU.mult, rd=[wmod, nwb])
    w_in = sb("w_in", [128, 8, IN_W], BF16)
    wiv = T["w_in"].rearrange("(kc p) n -> p kc n", p=128)
    for kc in range(8):
        for hh in range(2):
            P.dma("pool", w_in[:, kc, hh * 1540:(hh + 1) * 1540], wiv[:, kc, hh * 1540:(hh + 1) * 1540], writes=[w_in])
    cw = sb("cw", [128, 12, 4])
    with nc.allow_non_contiguous_dma(reason="tiny conv weight transpose"):
        pass
    for i in range(4):
        P.dma("sp", cw[:, :, i], T["conv_w"][i, :].rearrange("(j p) -> p j", p=128), writes=[cw], allow_slow_non_contiguous=True)
    dtb = sb("dtb", [128, 4]); negA = sb("negA", [128, 4]); gnw = sb("gnw", [128, 128])
    P.dma("sp", dtb[:], T["dt_bias"][0, :].partition_broadcast(128), writes=[dtb])
    P.dma("sp", negA[:], T["a_log"][0, :].partition_broadcast(128), writes=[negA])
    P.dma("sp", gnw[:], T["gnw"][0, :].partition_broadcast(128), writes=[gnw])
    actf(P, negA, negA[:], negA[:], AF.Exp, rd=[negA])
    ts(P, "dve", negA, negA[:], negA[:], -1.0, None, ALU.mult, rd=[negA])
    if getattr(X, 'stopat', 99) == 1:
        return

    xt = Rot([sb("x%d" % i, [128, D]) for i in range(1)])
    hb = sb("hb", [128, D], BF16); hT = sb("hT", [128, 8, 128], BF16)
    ss = sb("ss", [128, 8])
    q_tm = sb("q_tm", [128, 512]); kv_tm = Rot([sb("kv%d" % i, [128, 512]) for i in range(1)])
    sz = sb("sz", [128, 512]); ba = sb("ba", [128, 8])
    ext = sb("ext", [128, 12, 131]); cacc = sb("cacc", [128, 12, 128]); ctmp = sb("ctmp", [128, 12, 128])
    afm = sb("afm", [128, 12, 128])
    pre_tm = ctmp
    beta = sb("beta", [128, 4]); nbeta = sb("nbeta", [128, 4]); g = sb("g", [128, 4]); gc = sb("gc", [128, 4]); ngc = sb("ngc", [128, 4])
    egc = sb("egc", [128, 4]); bege = sb("bege", [128, 4])
    S = [sb("S%d" % h, [128, 128]) for h in range(4)]
    mixg = sb("mixg", [128, 512])
    pTr = ps("pTr", [128, 8, 128], BF16)
    pA = Rot([ps("pA%d" % i, [128, 512]) for i in range(2)])
    gbanks = [ps("pG%d" % i, [128, 4, 128]) for i in range(4)]
    gps = Rot([bv2(b, j, "g%d" % j) for b in gbanks for j in range(4)])
    names = ["kh", "qh", "vb", "kbe", "kt", "qt", "qtt", "Dm", "DTm", "EG", "Nm", "NTm", "Pa", "PTa", "Pb", "PTb", "XTa", "XTb",
             "wT", "IT", "usb", "vnew", "ktil", "tmpg"]
    hsets = [{n: sb("%s_%d" % (n, i), [128, 128]) for n in names} for i in range(1)] * 2
    cols = [{n: sb("%s_%d" % (n, i), [128, 1]) for n in ["rq", "rk", "glast", "eglast", "ekl", "rso", "ssq", "ssk", "sso"]} for i in range(2)]
    for h in range(4):
        P.op("dve", lambda e, h=h: e.memset(S[h][:], 0.0), writes=[S[h]])
    P.op("dve", lambda e: e.memset(ext[:], 0.0), writes=[ext])
    nk_d = X.nks_buf if sample else Buf(T["nk_p"]); nv_d = X.nvs_buf if sample else Buf(T["nv_p"])
    mix_d = X.mix_d
    n = TP

    if sample:
        stc = sb("stc", [SB * 3, 1536])
        P.dma("sp", stc[:], T["state_conv"], writes=[stc])
        Ssm = Rot([[sb("Ss%d_%d" % (i, h), [128, 128]) for h in range(4)] for i in range(2)])
        ns_s = Buf(T["ns_s"], "ns_s")
        q_sd = X.q_sd
    for t in range(ntile):
        x = xt.get()
        P.dma("sp", x[0:n, :], x_d[t * TS:t * TS + n, :], writes=[x])
        if sample:
            P.dma("sp", shb[0:4, :], T["m_d"][1 + t, 0:D].partition_broadcast(4), reads=[m_d], writes=[shb])
            P.dma("sp", wmod[0:4, :], T["m_d"][1 + t, D:2 * D].partition_broadcast(4), reads=[m_d], writes=[wmod])
            stt(P, "dve", wmod, wmod[0:4, :], wmod[0:4, :], 1.0, nwb2[0:4, :], ALU.add, ALU.mult, rd=[wmod, nwb2])
            for jb in range(3):
                pp = pA.get()
                for jj in range(4):
                    j = jb * 4 + jj
                    P.op("pe", lambda e, pp=pp, jj=jj, j=j, t=t: e.matmul(pp[:, jj * 128:jj * 128 + 3], lhsT=stc[:, j * 128:(j + 1) * 128],
                                                                         rhs=ident[0:48, 3 * t:3 * t + 3], start=True, stop=True),
                         reads=[stc, cst], writes=[pp])
                cp(P, "dve", ext, ext[:, jb * 4:(jb + 1) * 4, 0:3], pp[:, :].rearrange("p (j t) -> p j t", j=4)[:, :, 0:3], rd=[pp])
            Scur = Ssm.get()
            for h in range(4):
                r = (t * 4 + h) * 128
                P.dma("sp", Scur[h][:, :], T["state_ssm"][r:r + 128, :], writes=[Scur[h]])
            X.S_for = lambda h, Scur=Scur: Scur[h]
        actf(P, junk, junk[0:n, :], x[0:n, :], AF.Square, rd=[x], accum=ss[0:n, 0:1], wr_extra=[ss])
        rsqrt_col(P, ss, ss[0:n, 2:3], ss[0:n, 0:1], 1.0 / D, EPS, [ss])
        stt(P, "dve", tmp, tmp[0:n, :], x[0:n, :], ss[0:n, 2:3], wmod[0:n, :], ALU.mult, ALU.mult, rd=[x, ss, wmod])
        tt(P, "pool", hb, hb[0:n, :], tmp[0:n, :], shb[0:n, :], ALU.add, rd=[tmp, shb])
        if getattr(X, 'stopat', 99) == 2:
            return

        for kc in range(8):
            tr(P, pTr, pTr[:, kc, 0:n], hb[0:n, kc * 128:(kc + 1) * 128], identb[0:n, 0:n], rd=[hb, identb], inc=(kc == 7))
        cp(P, "act", hT, hT[:, :, 0:n], pTr[:, :, 0:n], rd=[pTr])
        if getattr(X, 'stopat', 99) == 3:
            return

        for gi, (c0, wdt) in enumerate([(0, 512), (512, 512), (2560, 512), (3072, 8)]):
            pp = pA.get()
            for kc in range(8):
                mm(P, pp, pp[0:n, 0:wdt], hT[:, kc, 0:n], w_in[:, kc, c0:c0 + wdt], [hT, w_in], start=(kc == 0), stop=(kc == 7))
            if gi == 0:
                cp(P, "act", q_tm, q_tm[0:n, :], pp[0:n, :], rd=[pp])
            elif gi == 1:
                kv = kv_tm.get()
                cp(P, "dve", kv, kv[0:n, :], pp[0:n, :], rd=[pp])
                P.dma("sp", nk_d[t * TS:t * TS + n, :], kv[0:n, 0:256], reads=[kv], writes=[nk_d])
                P.dma("sp", nv_d[t * TS:t * TS + n, :], kv[0:n, 256:512], reads=[kv], writes=[nv_d])
                kv_hook(P, t, kv, n, locals(), X)
            elif gi == 2:
                actf(P, sz, sz[0:n, :], pp[0:n, :], AF.Silu, rd=[pp])
            else:
                cp(P, "dve", ba, ba[0:n, :], pp[0:n, 0:8], rd=[pp])
        q_hook(P, t, q_tm, n, locals(), X)
        if getattr(X, 'stopat', 99) == 5:
            return

        for jb in range(3):
            pp = pA.get()
            for jj in range(4):
                j = jb * 4 + jj
                for kc in range(8):
                    mm(P, pp, pp[:, jj * 128:jj * 128 + n], w_in[:, kc, 1024 + j * 128:1024 + (j + 1) * 128], hT[:, kc, 0:n], [hT, w_in],
                       start=(kc == 0), stop=(kc == 7))
            cp(P, "act" if jb % 2 == 0 else "dve", ext, ext[:, jb * 4:(jb + 1) * 4, 3:3 + n],
               pp[:, :].rearrange("p (j t) -> p j t", j=4)[:, :, 0:n], rd=[pp])
        conv_hook(P, t, n, locals(), X)
        for i in range(4):
            src = ext[:, :, i:i + n]
            wb = cw[:, :, i:i + 1].to_broadcast([128, 12, n])
            if i == 0:
                tt(P, "dve", cacc, cacc[:, :, 0:n], src, wb, ALU.mult, rd=[ext, cw])
            else:
                tt(P, "pool", ctmp, ctmp[:, :, 0:n], src, wb, ALU.mult, rd=[ext, cw])
                tt(P, "dve", cacc, cacc[:, :, 0:n], cacc[:, :, 0:n], ctmp[:, :, 0:n], ALU.add, rd=[cacc, ctmp])
        actf(P, afm, afm[:, :, 0:n], cacc[:, :, 0:n], AF.Silu, rd=[cacc])
        if getattr(X, 'stopat', 99) == 6:
            return

        if not sample:
            cp(P, "pool", ext, ext[:, :, 0:3], ext[:, :, 128:131], rd=[ext])
        if getattr(X, 'stopat', 99) == 7:
            return
        actf(P, beta, beta[0:n, :], ba[0:n, 0:4], AF.Sigmoid, rd=[ba])
        if getattr(X, 'stopat', 99) == 8:
            return
        ts(P, "dve", nbeta, nbeta[0:n, :], beta[0:n, :], -1.0, None, ALU.mult, rd=[beta])
        tt(P, "dve", g, g[0:n, :], ba[0:n, 4:8], dtb[0:n, :], ALU.add, rd=[ba, dtb])
        actf(P, g, g[0:n, :], g[0:n, :], AF.Exp, rd=[g])
        if getattr(X, 'stopat', 99) == 9:
            return
        actf(P, g, g[0:n, :], g[0:n, :], AF.Ln, rd=[g], bias=1.0)
        if getattr(X, 'stopat', 99) == 10:
            return
        tt(P, "dve", g, g[0:n, :], g[0:n, :], negA[0:n, :], ALU.mult, rd=[g, negA])
        if not getattr(X, 'nogdn', False):
            gdn_tile(P, t, n, locals(), X)
        if sample:
            for h in range(4):
                r = (t * 4 + h) * 128
                P.dma("sp", ns_s[r:r + 128, :], Scur[h][:, :], reads=[Scur[h]], writes=[ns_s])
    a1_end(P, locals(), X)


def sub2(t, j, name=""):
    return Sub(t, lambda idx, j=j: (idx[0], j) + tuple(idx[1:]), name)


class BV:
    def __init__(self, parent, pre, name=""):
        self.parent = parent
        self.pre = pre
        self.name = name

    @property
    def lw(self):
        return self.parent.lw

    @lw.setter
    def lw(self, v):
        self.parent.lw = v

    @property
    def rd(self):
        return self.parent.rd

    @rd.setter
    def rd(self, v):
        self.parent.rd = v

    def __getitem__(self, idx):
        if not isinstance(idx, tuple):
            idx = (idx,)
        return self.parent.t[self.pre(idx)]


class RV(BV):
    def __init__(self, parent, fn, name=""):
        self.parent = parent
        self.fn = fn
        self.name = name

    def __getitem__(self, idx):
        return self.fn(self.parent.t)[idx]


def bv2(parent, j, name=""):
    return BV(parent, lambda idx, j=j: (idx[0], j) + tuple(idx[1:]), name)


def gdn_tile(P, t, n, L, X):
    cst, tri, ident, negus, negut = L["cst"], L["tri"], L["ident"], L["negus"], L["negut"]
    g, gc, ngc, egc, bege, beta, nbeta = L["g"], L["gc"], L["ngc"], L["egc"], L["bege"], L["beta"], L["nbeta"]
    gps, afm, S, sz, gnw, mixg, hsets, cols = L["gps"], L["afm"], L["S"], L["sz"], L["gnw"], L["mixg"], L["hsets"], L["cols"]
    gp = gps.get()
    mm(P, gp, gp[0:n, 0:4], tri[0:n, 0:n], g[0:n, :], [cst, g])
    cp(P, "dve", gc, gc[0:n, :], gp[0:n, 0:4], rd=[gp])
    ts(P, "dve", ngc, ngc[0:n, :], gc[0:n, :], -1.0, None, ALU.mult, rd=[gc])
    actf(P, egc, egc[0:n, :], gc[0:n, :], AF.Exp, rd=[gc])
    tt(P, "dve", bege, bege[0:n, :], beta[0:n, :], egc[0:n, :], ALU.mult, rd=[beta, egc])
    if getattr(X, 'gstop', 99) == 1:
        return
    for h in range(4):
        hs = hsets[h % 2]
        cl = cols[h % 2]
        kh, qh, vb, kbe, kt, qt, qtt = hs["kh"], hs["qh"], hs["vb"], hs["kbe"], hs["kt"], hs["qt"], hs["qtt"]
        Dm, DTm, EG, Nm, NTm, wT, IT, usb, vnew, ktil, tmpg = (hs[k] for k in
                                                                ["Dm", "DTm", "EG", "Nm", "NTm", "wT", "IT", "usb", "vnew", "ktil", "tmpg"])
        pq = gps.get(); tr32(P, pq, pq[0:n, :], afm[:, h, 0:n], ident, [afm, cst])
        pk = gps.get(); tr32(P, pk, pk[0:n, :], afm[:, 4 + h, 0:n], ident, [afm, cst])
        pv = gps.get(); tr32(P, pv, pv[0:n, :], afm[:, 8 + h, 0:n], ident, [afm, cst])
        if getattr(X, 'gstop', 99) == 11:
            return
        actf(P, tmpg, tmpg[0:n, :], pq[0:n, :], AF.Square, rd=[pq], accum=cl["ssq"][0:n, :], wr_extra=[cl["ssq"]])
        actf(P, tmpg, tmpg[0:n, :], pk[0:n, :], AF.Square, rd=[pk], accum=cl["ssk"][0:n, :], wr_extra=[cl["ssk"]])
        if getattr(X, 'gstop', 99) == 12:
            return
        rsqrt_col(P, cl["rq"], cl["rq"][0:n, :], cl["ssq"][0:n, :], 1.0, EPS, [cl["ssq"]])
        rsqrt_col(P, cl["rk"], cl["rk"][0:n, :], cl["ssk"][0:n, :], 1.0, EPS, [cl["ssk"]])
        if getattr(X, 'gstop', 99) == 13:
            return
        ts(P, "dve", qh, qh[0:n, :], pq[0:n, :], cl["rq"][0:n, :], 128.0 ** -0.5, ALU.mult, ALU.mult, rd=[pq, cl["rq"]])
        ts(P, "dve", kh, kh[0:n, :], pk[0:n, :], cl["rk"][0:n, :], None, ALU.mult, rd=[pk, cl["rk"]])
        ts(P, "dve", vb, vb[0:n, :], pv[0:n, :], beta[0:n, h:h + 1], None, ALU.mult, rd=[pv, beta])
        if getattr(X, 'gstop', 99) == 14:
            return
        ts(P, "pool", kbe, kbe[0:n, :], kh[0:n, :], bege[0:n, h:h + 1], None, ALU.mult, rd=[kh, bege])
        if getattr(X, 'gstop', 99) == 2:
            return
        pg = gps.get()
        mm(P, pg, pg[:, 0:n], g[0:n, h:h + 1].to_broadcast([n, 128]), tri[0:n, 0:n], [g, cst])
        cp(P, "dve", cl["glast"], cl["glast"][:, :], pg[:, n - 1:n], rd=[pg])
        actf(P, cl["eglast"], cl["eglast"][:, :], cl["glast"][:, :], AF.Exp, rd=[cl["glast"]])
        actf(P, cl["ekl"], cl["ekl"][0:n, :], gc[0:n, h:h + 1], AF.Exp, rd=[gc, cl["glast"]], scale=-1.0, bias=cl["glast"][0:n, :])
        ts(P, "pool", ktil, ktil[0:n, :], kh[0:n, :], cl["ekl"][0:n, :], None, ALU.mult, rd=[kh, cl["ekl"]])
        if getattr(X, 'gstop', 99) == 3:
            return
        stt(P, "dve", tmpg, tmpg[0:n, 0:n], pg[0:n, 0:n], -1.0, negus[0:n, 0:n], ALU.mult, ALU.add, rd=[pg, cst])
        actf(P, Dm, Dm[0:n, 0:n], tmpg[0:n, 0:n], AF.Exp, rd=[tmpg, gc], bias=gc[0:n, h:h + 1])
        tt(P, "dve", IT, IT[0:n, 0:n], pg[0:n, 0:n], negut[0:n, 0:n], ALU.add, rd=[pg, cst])
        actf(P, DTm, DTm[0:n, 0:n], IT[0:n, 0:n], AF.Exp, rd=[IT, ngc], bias=ngc[0:n, h:h + 1])
        actf(P, EG, EG[:, 0:n], pg[:, 0:n], AF.Exp, rd=[pg])
        if getattr(X, 'gstop', 99) == 4:
            return
        pkt = gps.get(); tr32(P, pkt, pkt[:, 0:n], kh[0:n, :], ident[0:n, 0:n], [kh, cst])
        cp(P, "act", kt, kt[:, 0:n], pkt[:, 0:n], rd=[pkt])
        pqt = gps.get(); tr32(P, pqt, pqt[:, 0:n], qh[0:n, :], ident[0:n, 0:n], [qh, cst])
        cp(P, "dve", qt, qt[:, 0:n], pqt[:, 0:n], rd=[pqt])
        tt(P, "dve", qtt, qtt[:, 0:n], pqt[:, 0:n], EG[:, 0:n], ALU.mult, rd=[pqt, EG])
        if getattr(X, 'gstop', 99) == 5:
            return
        pkk = gps.get(); mm(P, pkk, pkk[0:n, 0:n], kt[:, 0:n], kt[:, 0:n], [kt])
        stt(P, "dve", Nm, Nm[0:n, 0:n], pkk[0:n, 0:n], nbeta[0:n, h:h + 1], Dm[0:n, 0:n], ALU.mult, ALU.mult, rd=[pkk, nbeta, Dm])
        pnt = gps.get(); tr32(P, pnt, pnt[0:n, 0:n], Nm[0:n, 0:n], ident[0:n, 0:n], [Nm, cst])
        cp(P, "act", NTm, NTm[0:n, 0:n], pnt[0:n, 0:n], rd=[pnt])
        XT = hs["XTa"]
        tt(P, "pool", XT, XT[0:n, 0:n], NTm[0:n, 0:n], ident[0:n, 0:n], ALU.add, rd=[NTm, cst])
        if getattr(X, 'gstop', 99) == 6:
            return
        Pc, PTc = Nm, NTm
        nsteps = 0
        while (1 << (nsteps + 1)) < n:
            nsteps += 1
        for s in range(1, nsteps + 1):
            Pn, PTn = (hs["Pa"], hs["PTa"]) if s % 2 == 1 else (hs["Pb"], hs["PTb"])
            p1 = gps.get(); mm(P, p1, p1[0:n, 0:n], PTc[0:n, 0:n], Pc[0:n, 0:n], [PTc, Pc])
            cp(P, "act", Pn, Pn[0:n, 0:n], p1[0:n, 0:n], rd=[p1])
            if s < nsteps:
                p2 = gps.get(); mm(P, p2, p2[0:n, 0:n], Pc[0:n, 0:n], PTc[0:n, 0:n], [PTc, Pc])
                cp(P, "dve", PTn, PTn[0:n, 0:n], p2[0:n, 0:n], rd=[p2])
            p3 = gps.get(); mm(P, p3, p3[0:n, 0:n], Pn[0:n, 0:n], XT[0:n, 0:n], [Pn, XT])
            XTn = hs["XTb"] if XT is hs["XTa"] else hs["XTa"]
            tt(P, "dve", XTn, XTn[0:n, 0:n], XT[0:n, 0:n], p3[0:n, 0:n], ALU.add, rd=[XT, p3])
            XT = XTn
            Pc, PTc = Pn, PTn
        pu = gps.get(); mm(P, pu, pu[0:n, :], XT[0:n, 0:n], vb[0:n, :], [XT, vb])
        if getattr(X, 'gstop', 99) == 7:
            return
        cp(P, "act", usb, usb[0:n, :], pu[0:n, :], rd=[pu])
        pw = gps.get(); mm(P, pw, pw[:, 0:n], kbe[0:n, :], XT[0:n, 0:n], [XT, kbe])
        cp(P, "dve", wT, wT[:, 0:n], pw[:, 0:n], rd=[pw])
        pit = gps.get(); mm(P, pit, pit[0:n, 0:n], kt[:, 0:n], qt[:, 0:n], [kt, qt])
        tt(P, "dve", IT, IT[0:n, 0:n], pit[0:n, 0:n], DTm[0:n, 0:n], ALU.mult, rd=[pit, DTm])
        if getattr(X, 'gstop', 99) == 8:
            return
        Sh = X.S_for(h) if hasattr(X, "S_for") else S[h]
        pws = gps.get(); mm(P, pws, pws[0:n, :], wT[:, 0:n], Sh[:, :], [wT, Sh])
        tt(P, "dve", vnew, vnew[0:n, :], usb[0:n, :], pws[0:n, :], ALU.subtract, rd=[usb, pws])
        po = gps.get()
        mm(P, po, po[0:n, :], qtt[:, 0:n], Sh[:, :], [qtt, Sh], start=True, stop=False)
        mm(P, po, po[0:n, :], IT[0:n, 0:n], vnew[0:n, :], [IT, vnew], start=False, stop=True)
        psu = gps.get(); mm(P, psu, psu[:, :], ktil[0:n, :], vnew[0:n, :], [ktil, vnew])
        stt(P, "dve", Sh, Sh[:, :], Sh[:, :], cl["eglast"][:, 0:1], psu[:, :], ALU.mult, ALU.add, rd=[Sh, cl["eglast"], psu])
        if getattr(X, 'gstop', 99) == 9:
            return
        osb = hs["usb"]
        cp(P, "dve", osb, osb[0:n, :], po[0:n, :], rd=[po])
        actf(P, tmpg, tmpg[0:n, :], osb[0:n, :], AF.Square, rd=[osb], accum=cl["sso"][0:n, :], wr_extra=[cl["sso"]])
        if getattr(X, 'gstop', 99) == 17:
            return
        rsqrt_col(P, cl["rso"], cl["rso"][0:n, :], cl["sso"][0:n, :], 1.0 / 128, EPS, [cl["sso"]])
        if getattr(X, 'gstop', 99) == 18:
            return
        stt(P, "dve", tmpg, tmpg[0:n, :], osb[0:n, :], cl["rso"][0:n, :], gnw[0:n, :], ALU.mult, ALU.mult, rd=[osb, cl["rso"], gnw])
        if getattr(X, 'gstop', 99) == 19:
            return
        tt(P, "pool", mixg, mixg[0:n, h * 128:(h + 1) * 128], tmpg[0:n, :], sz[0:n, h * 128:(h + 1) * 128], ALU.mult, rd=[tmpg, sz])
        if getattr(X, 'gstop', 99) == 15:
            return
    if getattr(X, 'gstop', 99) == 16:
        return
    r0 = L["row0"] + t * L["TS"]
    P.dma("sp", X.T["mix_d"][r0:r0 + n, 512:1024], mixg[0:n, :], reads=[mixg], writes=[X.mix_d])


def kv_hook(P, t, kv, n, L, X):
    if L["sample"]:
        return
    identb = L["identb"]
    kd = X.kdup.get()
    Vs = X.Vbs[t]
    cp(P, "pool", Vs, Vs[:, :, 0:64], kv[:, 256:512].rearrange("p (h d) -> p h d", h=4), rd=[kv])
    P.op("pool", lambda e: e.memset(Vs[:, :, 64:65], 1.0), writes=[Vs])
    src = kv[:, 0:256].rearrange("p (h d) -> p h d", h=4).unsqueeze(2).to_broadcast([128, 4, 2, 64])
    cp(P, "pool", kd, kd[:, :, :, :], src, rd=[kv])
    pk = X.pKT
    for h in range(4):
        tr(P, pk, pk[:, h, :], kd[:, h, :, :].rearrange("p r d -> p (r d)"), identb[:, :], [kd, identb], inc=(h == 3))
    Ks = X.KTs[t]
    cp(P, "act", Ks, Ks[:, :, :], pk[:, :, :], rd=[pk])


def stcT_src(stc, t, j):
    return stc[:, j * 128:(j + 1) * 128]


def q_hook(P, t, q_tm, n, L, X):
    if L["sample"]:
        P.dma("sp", X.T["q_s"][t * 4:t * 4 + 4, :], q_tm[0:4, :], reads=[q_tm], writes=[X.q_sd])
        return
    identb = L["jrevb"]
    qb = X.qb.get()
    cp(P, "pool", qb, qb[:, :], q_tm[:, :], rd=[q_tm])
    pq = X.pQT
    for p in range(4):
        tr(P, pq, pq[:, p, :], qb[:, p * 128:(p + 1) * 128], identb[:, :], [qb, identb], inc=(p == 3))
    Qs = X.QTs[t]
    cp(P, "dve", Qs, Qs[:, :, :], pq[:, :, :], rd=[pq])


def conv_hook(P, t, n, L, X):
    if (not L["sample"]) and t != NT - 1:
        return
    hT, w_in, pA, pre_tm = L["hT"], L["w_in"], L["pA"], L["pre_tm"]
    for gi in range(3):
        pp = pA.get()
        for kc in range(8):
            mm(P, pp, pp[0:n, :], hT[:, kc, 0:n], w_in[:, kc, 1024 + gi * 512:1024 + (gi + 1) * 512], [hT, w_in], start=(kc == 0), stop=(kc == 7))
        cp(P, "act", pre_tm, pre_tm[0:n, gi * 4:(gi + 1) * 4, :], pp[0:n, :].rearrange("p (j t) -> p j t", j=4), rd=[pp])
    if L["sample"]:
        ncs = X.ncs
        P.dma("sp", ncs[3 * t:3 * t + 3, :].rearrange("r (j t) -> r j t", j=12), pre_tm[1:4, :, :], reads=[pre_tm], writes=[ncs])
        return
    ncp = Buf(X.T["nc_p"])
    P.dma("sp", ncp[:, :].rearrange("r (j t) -> r j t", j=12), pre_tm[125:128, :, :], reads=[pre_tm], writes=[ncp])
    X.outs.append(ncp)


def a1_end(P, L, X):
    if L["sample"] or getattr(X, 'noend', False):
        return
    nsp = Buf(X.T["ns_p"])
    v = getattr(X, 'endvar', 0)
    for h in range(4 if v != 1 else 1):
        src = L["S"][h] if v != 2 else L["tmp"]
        P.dma("sp", nsp[h * 128:(h + 1) * 128, :], src[:, 0:128], reads=[src], writes=[nsp])
    X.outs += [nsp, L["nk_d"], L["nv_d"]]


def alloc_attn_persist(nc, es, X):
    sb, ps = allocators(nc, es, "pers")
    QT = sb("QT", [128, 4, SEQ], BF16)
    KT = sb("KT", [128, 4, SEQ], BF16)
    Vb = sb("Vb", [128, NT, 4, 65], BF16)
    X.QT, X.KT, X.Vb = QT, KT, Vb
    X.QTs = [Sub(QT.t, lambda idx, t=t: (idx[0], idx[1], slice(t * 128, (t + 1) * 128)) if True else None, "QT%d" % t) for t in range(NT)]
    X.KTs = [Sub(KT.t, lambda idx, t=t: (idx[0], idx[1], slice(t * 128, (t + 1) * 128)), "KT%d" % t) for t in range(NT)]
    X.Vbs = [sub2(Vb.t, t, "Vb%d" % t) for t in range(NT)]


def alloc_a1_extra(nc, es, X, pre="a1x"):
    sb, ps = allocators(nc, es, pre)
    X.kdup = Rot([sb("kdup%d" % i, [128, 4, 2, 64], BF16) for i in range(2)])
    X.qb = Rot([sb("qb%d" % i, [128, 512], BF16) for i in range(2)])
    pb = Buf(es.enter_context(nc.psum_tensor(pre + "_pKQ", [128, 8, 128], BF16)), "pKQ")
    X.pKT = BV(pb, lambda idx: (idx[0], (slice(0, 4) if isinstance(idx[1], slice) else idx[1])) + tuple(idx[2:]), "pKT")
    X.pQT = BV(pb, lambda idx: (idx[0], (slice(4, 8) if isinstance(idx[1], slice) else idx[1] + 4)) + tuple(idx[2:]), "pQT")


def build_rel_table(P, nc, es, T, X, relb_rows, oh_name, ncols, rd_name, pre):
    sb, ps = allocators(nc, es, pre)
    relb = sb("relb", [33, 8])
    P.op("dve", lambda e: e.memset(relb[:], NEG), writes=[relb])
    P.dma("sp", relb[0:32, :], T["rel_bias"], writes=[relb])
    oh = sb("oh", [33, ncols])
    P.dma("sp", oh[:], T[oh_name], writes=[oh])
    rsb = sb("rsb", [relb_rows, ncols])
    pp = ps("pp", [relb_rows, 512])
    lhs = X.rel_lhs(relb) if hasattr(X, "rel_lhs") else relb[:, :]
    for j in range(0, ncols, 512):
        w = min(512, ncols - j)
        mm(P, pp, pp[:, 0:w], lhs, oh[:, j:j + w], [relb, oh])
        actf(P, rsb, rsb[:, j:j + w], pp[:, 0:w], AF.Exp, rd=[pp], eng="act")
    rdb = Buf(T[rd_name], rd_name)
    P.dma("sp", rdb[:, :], rsb[:, :], reads=[rsb], writes=[rdb])
    return rdb


def phase_a2(P, nc, es, T, X):
    sb, ps = allocators(nc, es, "a2")
    scale = 64.0 ** -0.5
    nch = getattr(X, "ntile", NT)
    cst = sb("cst", [128, NCST])
    P.dma("sp", cst[:], T["cst"], writes=[cst])
    jrevb = sb("jrevb", [128, 128], BF16)
    cp(P, "dve", jrevb, jrevb[:], cst[:, C_JREV:C_JREV + 128], rd=[cst])
    rdb = X.rdb
    tabs = Rot([sb("tab%d" % i, [128, 4096]) for i in range(2)])
    S_all = Rot([sb("S_all%d" % i, [128, 4096]) for i in range(2)])
    E = sb("E", [128, 4096])
    Pb = Rot([sb("Pb%d" % i, [128, 4096], BF16) for i in range(2)])
    PTs = Rot([sb("PTs%d" % i, [128, 4, 128], BF16) for i in range(3)])
    gt = sb("gt", [128, 16]); mx8 = sb("mx8", [128, 8]); mb = sb("mb", [128, 16]); bcol = sb("bcol", [128, 16])
    nm = sb("nm", [128, 1]); rm = sb("rm", [128, 1]); rcp = sb("rcp", [128, 1])
    ao = Rot([sb("ao%d" % i, [128, 64]) for i in range(2)])
    pS = Rot([ps("pS%d" % i, [128, 512]) for i in range(3)])
    pT = Rot([ps("pT%d" % i, [128, 8, 128], BF16) for i in range(2)])
    pO = Rot([ps("pO%d" % i, [128, 512]) for i in range(2)])
    mix_d = X.mix_d
    for hq in range(8):
        hk = hq // 2
        r0 = (hq % 2) * 64
        tab = tabs.get()
        src = bass.AP(tensor=T["rd"].tensor, offset=hq * RLEN, ap=[[1, 128], [1, 4096]])
        P.dma("sp", tab[:, :], src, reads=[rdb], writes=[tab])
        for c in range(nch):
            nk = 128 * (c + 1)
            ob = c // 2
            q0 = 128 * c
            Sa = S_all.get()
            for pi, k0 in enumerate(range(0, nk, 512)):
                w = min(512, nk - k0)
                pp = pS.get()
                rdq = [X.QTs[c]] + [X.KTs[t] for t in range(k0 // 128, (k0 + w) // 128)]
                mm(P, pp, pp[:, 0:w], X.QT[r0:r0 + 64, hk, q0:q0 + 128], X.KT[r0:r0 + 64, hk, k0:k0 + w], rdq)
                cp(P, "dve" if pi % 2 == 0 else "act", Sa, Sa[:, k0:k0 + w], pp[:, 0:w], rd=[pp])
            P.op("pool", lambda e: e.memset(gt[:], -1e30), writes=[gt])
            if ob > 0:
                P.op("dve", lambda e, Sa=Sa, ob=ob: e.tensor_reduce(gt[:, 0:ob], Sa[:, 0:256 * ob].rearrange("p (n k) -> p n k", k=256), AX.X, ALU.add),
                     reads=[Sa], writes=[gt])
            P.op("dve", lambda e: e.max(mx8[:], gt[:]), reads=[gt], writes=[mx8])
            ts(P, "dve", mb, mb[:], gt[:], mx8[:, 2:3], 1.0, ALU.is_ge, ALU.subtract, rd=[gt, mx8])
            P.op("dve", lambda e, Sa=Sa, nk=nk: e.reduce_max(rm[:], Sa[:, 0:nk], AX.X), reads=[Sa], writes=[rm])
            ts(P, "dve", nm, nm[:], rm[:], -scale, None, ALU.mult, rd=[rm])
            ts(P, "dve", bcol, bcol[:], mb[:], -NEG, nm[:, 0:1], ALU.mult, ALU.add, rd=[mb, nm])
            for n in range(ob):
                actf(P, E, E[:, 256 * n:256 * (n + 1)], Sa[:, 256 * n:256 * (n + 1)], AF.Exp, rd=[Sa, bcol], scale=scale, bias=bcol[:, n:n + 1])
            actf(P, E, E[:, 256 * ob:nk], Sa[:, 256 * ob:nk], AF.Exp, rd=[Sa, nm], scale=scale, bias=nm[:, 0:1])
            Pc = Pb.get()
            tt(P, "dve" if c % 2 == 0 else "pool", Pc, Pc[:, 0:nk], E[:, 0:nk], tab[:, 3968 - q0:4096], ALU.mult, rd=[E, tab])
            po = pO.get()
            for g0 in range(0, c + 1, 4):
                g1 = min(g0 + 4, c + 1)
                pt = pT.get()
                for kt in range(g0, g1):
                    tr(P, pt, pt[:, kt - g0, :], Pc[:, kt * 128:(kt + 1) * 128], jrevb[:, :], [Pc, jrevb], inc=(kt == g1 - 1))
                pts = PTs.get()
                cp(P, "act" if (g0 // 4) % 2 == 0 else "dve", pts, pts[:, 0:g1 - g0, :], pt[:, 0:g1 - g0, :], rd=[pt])
                for kt in range(g0, g1):
                    mm(P, po, po[:, 0:65], pts[:, kt - g0, :], X.Vb[:, kt, hk, :], [pts, X.Vbs[kt]], start=(kt == 0), stop=(kt == c), inc=(kt == g1 - 1))
            P.op("dve", lambda e, po=po: e.reciprocal(rcp[:], po[:, 64:65]), reads=[po], writes=[rcp])
            a = ao.get()
            ts(P, "dve", a, a[:, :], po[:, 0:64], rcp[:, 0:1], None, ALU.mult, rd=[po, rcp])
            P.dma("sp", T["mix_d"][q0:q0 + 128, hq * 64:(hq + 1) * 64], a[:, :], reads=[a], writes=[mix_d])


def phase_a3(P, nc, es, T, X, tiles):
    sb, ps = allocators(nc, es, "a3")
    cst = sb("cst", [128, NCST])
    P.dma("sp", cst[:], T["cst"], writes=[cst])
    identb = sb("identb", [128, 128], BF16)
    cp(P, "dve", identb, identb[:], cst[:, C_IDENT:C_IDENT + 128], rd=[cst])
    w_out = sb("w_out", [128, 8, D], BF16)
    wov = T["w_out"].rearrange("(kc p) n -> p kc n", p=128)
    for kc in range(8):
        P.dma("pool", w_out[:, kc, :], wov[:, kc, :], writes=[w_out])
    gta_p = sb("gta_p", [128, D]); gta_s = sb("gta_s", [128, D])
    m_d = X.m_d
    P.dma("sp", gta_p[:], T["m_d"][0, 2 * D:3 * D].partition_broadcast(128), reads=[m_d], writes=[gta_p])
    for s in range(SB):
        P.dma("sp", gta_s[4 * s:4 * s + 4, :], T["m_d"][1 + s, 2 * D:3 * D].partition_broadcast(4), reads=[m_d], writes=[gta_s])
    mixs = Rot([sb("mix%d" % i, [128, D]) for i in range(2)])
    xs = Rot([sb("x%d" % i, [128, D]) for i in range(2)])
    mixb = sb("mixb", [128, D], BF16)
    mixT = sb("mixT", [128, 8, 128], BF16)
    x1 = Rot([sb("x1_%d" % i, [128, D]) for i in range(2)])
    pTr = ps("pTr", [128, 8, 128], BF16)
    pY = Rot([ps("pY%d" % i, [128, 512]) for i in range(2)])
    for (row, n, xsrc, gta) in tiles(gta_p, gta_s):
        mix = mixs.get(); x = xs.get()
        P.dma("sp", mix[0:n, :], T["mix_d"][row:row + n, :], reads=[X.mix_d], writes=[mix])
        P.dma("sp", x[0:n, :], xsrc, writes=[x])
        cp(P, "pool", mixb, mixb[0:n, :], mix[0:n, :], rd=[mix])
        for kc in range(8):
            tr(P, pTr, pTr[:, kc, 0:n], mixb[0:n, kc * 128:(kc + 1) * 128], identb[0:n, 0:n], [mixb, identb], inc=(kc == 7))
        cp(P, "act", mixT, mixT[:, :, 0:n], pTr[:, :, 0:n], rd=[pTr])
        xo = x1.get()
        for g in range(2):
            pp = pY.get()
            for kc in range(8):
                mm(P, pp, pp[0:n, :], mixT[:, kc, 0:n], w_out[:, kc, g * 512:(g + 1) * 512], [mixT, w_out], start=(kc == 0), stop=(kc == 7))
            tt(P, "dve", xo, xo[0:n, g * 512:(g + 1) * 512], pp[0:n, :], gta[0:n, g * 512:(g + 1) * 512], ALU.mult, rd=[pp, gta])
        tt(P, "pool", xo, xo[0:n, :], xo[0:n, :], x[0:n, :], ALU.add, rd=[xo, x])
        P.dma("sp", T["x1_d"][row:row + n, :], xo[0:n, :], reads=[xo], writes=[X.x1_d])


def phase_b(P, nc, es, T, X, passes, ne=NE):
    sb, ps = allocators(nc, es, "b")
    MAXS = max(len(p) for p in passes)
    cst = sb("cst", [128, NCST])
    P.dma("sp", cst[:], T["cst"], writes=[cst])
    ident = cst[:, C_IDENT:C_IDENT + 128]
    m_d = X.m_d
    wmod = sb("wmod", [128, D]); shf = sb("shf", [128, D]); gtf = sb("gtf", [128, D]); nwf = sb("nwf", [128, D])
    h2 = sb("h2", [128, D])
    tmp = h2
    P.dma("sp", nwf[:], T["nw"][2, :].partition_broadcast(128), writes=[nwf])

    def load_mods(sample):
        P.dma("sp", tmp[:], T["nw"][1, :].partition_broadcast(128), writes=[tmp])
        if not sample:
            P.dma("sp", shf[:], T["m_d"][0, 3 * D:4 * D].partition_broadcast(128), reads=[m_d], writes=[shf])
            P.dma("sp", wmod[:], T["m_d"][0, 4 * D:5 * D].partition_broadcast(128), reads=[m_d], writes=[wmod])
            P.dma("sp", gtf[:], T["m_d"][0, 5 * D:6 * D].partition_broadcast(128), reads=[m_d], writes=[gtf])
        else:
            for s in range(SB):
                P.dma("sp", shf[4 * s:4 * s + 4, :], T["m_d"][1 + s, 3 * D:4 * D].partition_broadcast(4), reads=[m_d], writes=[shf])
                P.dma("sp", wmod[4 * s:4 * s + 4, :], T["m_d"][1 + s, 4 * D:5 * D].partition_broadcast(4), reads=[m_d], writes=[wmod])
                P.dma("sp", gtf[4 * s:4 * s + 4, :], T["m_d"][1 + s, 5 * D:6 * D].partition_broadcast(4), reads=[m_d], writes=[gtf])
        nr = ST if sample else 128
        stt(P, "dve", wmod, wmod[0:nr, :], wmod[0:nr, :], 1.0, tmp[0:nr, :], ALU.add, ALU.mult, rd=[wmod, tmp])

    wr = sb("wr", [128, 8, NE])
    P.dma("sp", wr[:], T["w_router"].rearrange("(kc p) e -> p kc e", p=128), writes=[wr])
    brb = sb("brb", [128, NE])
    P.dma("sp", brb[:], T["b_router"][0, :].partition_broadcast(128), writes=[brb])
    bdn = sb("bdn", [NE, D])
    P.dma("sp", bdn[:], T["b_down"], writes=[bdn])
    xt = Rot([sb("x%d" % i, [128, D]) for i in range(1)])
    bupT = sb("bupT", [128, 16, NE])
    pB = Rot([ps("pB%d" % i, [128, 512]) for i in range(2)])
    for hf in range(2):
        bup = xt.get()
        P.dma("sp", bup[0:NE, :], T["b_up"][:, hf * D:(hf + 1) * D], writes=[bup])
        for c8 in range(8):
            c = hf * 8 + c8
            pp = pB.get()
            tr32(P, pp, pp[:, 0:NE], bup[0:NE, c8 * 128:(c8 + 1) * 128], ident[0:NE, 0:NE], [bup, cst])
            cp(P, "dve", bupT, bupT[:, c, :], pp[:, 0:NE], rd=[pp])
    wst = Rot([sb("wst%d" % i, [128, D]) for i in range(3)])
    wub_d = [Buf(T["wupb"][e], "wupb%d" % e) for e in range(NE)]
    wdb_d = [Buf(T["wdnb"][e], "wdnb%d" % e) for e in range(NE)]
    H2T = sb("H2T", [128, 8, MAXS * 128], BF16)
    acc = [sb("acc%d" % i, [128, D]) for i in range(MAXS)]
    G = sb("G", [128, MAXS, NE])
    wup = Rot([sb("wup%d" % i, [128, 8, 2 * D], BF16) for i in range(2)])
    wdn = Rot([sb("wdn%d" % i, [128, 8, D], BF16) for i in range(2)])
    actT = sb("actT", [128, 8, 512], BF16)
    gsb = Rot([sb("gsb%d" % i, [128, 512]) for i in range(2)])
    sgs = Rot([sb("sgs%d" % i, [128, 512]) for i in range(2)])
    lsb = Rot([sb("lsb%d" % i, [128, 512]) for i in range(2)])
    h2Tf = RV(wst.b[0], lambda t: t[:, :].rearrange("p (k t) -> p k t", k=8), "h2Tf")
    ss = sb("ss", [128, 8]); lg = sb("lg", [128, NE]); mx8 = sb("mx8", [128, 8]); msk = sb("msk", [128, NE]); ex = sb("ex", [128, NE])
    gT = sb("gT", [NE, 128])
    pG = Rot([ps("pG%d" % i, [128, 512]) for i in range(2)])
    pL = Rot([ps("pL%d" % i, [128, 512]) for i in range(2)])
    pY = Rot([ps("pY%d" % i, [128, 512]) for i in range(2)])
    x1_d = X.x1_d
    wupv = T["w_up"].rearrange("e (kc p) n -> e p kc n", p=128)
    wdnv = T["w_down"].rearrange("e (kc p) n -> e p kc n", p=128)
    cur_mod = [None]

    for tiles in passes:
        for si, (row, n, is_s, out_ap) in enumerate(tiles):
            if cur_mod[0] != is_s:
                load_mods(is_s)
                cur_mod[0] = is_s
            x = xt.get()
            P.dma("sp", x[0:n, :], T["x1_d"][row:row + n, :], reads=[x1_d], writes=[x])
            actf(P, h2, h2[0:n, :], x[0:n, :], AF.Square, rd=[x], accum=ss[0:n, 0:1], wr_extra=[ss])
            rsqrt_col(P, ss, ss[0:n, 2:3], ss[0:n, 0:1], 1.0 / D, EPS, [ss])
            stt(P, "dve", h2, h2[0:n, :], x[0:n, :], ss[0:n, 2:3], wmod[0:n, :], ALU.mult, ALU.mult, rd=[x, ss, wmod])
            tt(P, "pool", h2, h2[0:n, :], h2[0:n, :], shf[0:n, :], ALU.add, rd=[h2, shf])
            for half in range(2):
                pp = pB.get()
                for k4 in range(4):
                    kc = half * 4 + k4
                    tr32(P, pp, pp[:, k4 * 128:k4 * 128 + n], h2[0:n, kc * 128:(kc + 1) * 128], ident[0:n, 0:n], [h2, cst])
                cp(P, "dve", h2Tf, h2Tf[:, half * 4:(half + 1) * 4, 0:n], pp[:, :].rearrange("p (k t) -> p k t", k=4)[:, :, 0:n], rd=[pp])
            cp(P, "pool", H2T, H2T[:, :, si * 128:si * 128 + n], h2Tf[:, :, 0:n], rd=[h2Tf])
            pp = pB.get()
            for kc in range(8):
                mm(P, pp, pp[0:n, 0:NE], h2Tf[:, kc, 0:n], wr[:, kc, :], [h2Tf, wr], start=(kc == 0), stop=(kc == 7))
            tt(P, "dve", lg, lg[0:n, :], pp[0:n, 0:NE], brb[0:n, :], ALU.add, rd=[pp, brb])
            P.op("dve", lambda e, n=n: e.max(mx8[0:n, :], lg[0:n, :]), reads=[lg], writes=[mx8])
            ts(P, "dve", msk, msk[0:n, :], lg[0:n, :], mx8[0:n, 3:4], None, ALU.is_ge, rd=[lg, mx8])
            ts(P, "dve", mx8, mx8[0:n, 7:8], mx8[0:n, 0:1], -1.0, None, ALU.mult, rd=[mx8])
            actf(P, ex, ex[0:n, :], lg[0:n, :], AF.Exp, rd=[lg, mx8], bias=mx8[0:n, 7:8])
            tt(P, "dve", ex, ex[0:n, :], ex[0:n, :], msk[0:n, :], ALU.mult, rd=[ex, msk])
            P.op("dve", lambda e, n=n: e.reduce_sum(ss[0:n, 4:5], ex[0:n, :], AX.X), reads=[ex], writes=[ss])
            P.op("dve", lambda e, n=n: e.reciprocal(ss[0:n, 5:6], ss[0:n, 4:5]), reads=[ss], writes=[ss])
            ts(P, "dve", G, G[0:n, si, :], ex[0:n, :], ss[0:n, 5:6], None, ALU.mult, rd=[ex, ss])
            pp = pB.get()
            tr32(P, pp, pp[0:NE, 0:n], G[0:n, si, :], ident[0:n, 0:n], [G, cst])
            cp(P, "dve", gT, gT[:, 0:n], pp[0:NE, 0:n], rd=[pp])
            for half in range(2):
                pp = pB.get()
                mm(P, pp, pp[0:n, :], gT[:, 0:n], bdn[:, half * 512:(half + 1) * 512], [gT, bdn])
                cp(P, "act" if half == 0 else "dve", acc[si], acc[si][0:n, half * 512:(half + 1) * 512], pp[0:n, :], rd=[pp])
        groups = []
        si = 0
        while si < len(tiles):
            g = list(range(si, min(si + 4, len(tiles))))
            groups.append(g)
            si += 4
        def load_w(e_, first_pass):
            wu = wup.get(); wd = wdn.get()
            if first_pass:
                k = 0
                for kc in range(8):
                    for hf in range(2):
                        st = wst.get()
                        P.dma("sp", st[:, :], T["w_up"][e_, kc * 128:(kc + 1) * 128, hf * D:(hf + 1) * D], writes=[st])
                        cp(P, "act", wu, wu[:, kc, hf * D:(hf + 1) * D], st[:, :], rd=[st])
                for kc in range(8):
                    st = wst.get()
                    P.dma("sp", st[:, :], T["w_down"][e_, kc * 128:(kc + 1) * 128, :], writes=[st])
                    cp(P, "act", wd, wd[:, kc, :], st[:, :], rd=[st])
                if len(passes) > 1:
                    P.dma("sp", T["wupb"][e_], wu[:, :, :].rearrange("p k n -> p (k n)"), reads=[wu], writes=[wub_d[e_]])
                    P.dma("sp", T["wdnb"][e_], wd[:, :, :].rearrange("p k n -> p (k n)"), reads=[wd], writes=[wdb_d[e_]])
            else:
                P.dma("sp", wu[:, :, :].rearrange("p k n -> p (k n)"), T["wupb"][e_], reads=[wub_d[e_]], writes=[wu])
                P.dma("sp", wd[:, :, :].rearrange("p k n -> p (k n)"), T["wdnb"][e_], reads=[wdb_d[e_]], writes=[wd])
            return wu, wd

        first_pass = (tiles is passes[0])
        nxt = load_w(0, first_pass)
        for e_ in range(ne):
            wu, wd = nxt
            if e_ + 1 < ne:
                nxt = load_w(e_ + 1, first_pass)
            for g in groups:
                c0 = g[0] * 128
                ncol = (g[-1] - g[0]) * 128 + tiles[g[-1]][1]
                for fc in range(8):
                    pg = pG.get(); pl = pL.get()
                    for kc in range(8):
                        mm(P, pg, pg[:, 0:ncol], wu[:, kc, fc * 128:(fc + 1) * 128], H2T[:, kc, c0:c0 + ncol], [wu, H2T], start=(kc == 0), stop=(kc == 7))
                    for kc in range(8):
                        mm(P, pl, pl[:, 0:ncol], wu[:, kc, D + fc * 128:D + (fc + 1) * 128], H2T[:, kc, c0:c0 + ncol], [wu, H2T], start=(kc == 0), stop=(kc == 7))
                    gs = gsb.get(); sg = sgs.get(); ls = lsb.get()
                    actf(P, gs, gs[:, 0:ncol], pg[:, 0:ncol], AF.Identity, rd=[pg, bupT], bias=bupT[:, fc, e_:e_ + 1])
                    ts(P, "pool", gs, gs[:, 0:ncol], gs[:, 0:ncol], 7.0, None, ALU.min, rd=[gs])
                    actf(P, sg, sg[:, 0:ncol], gs[:, 0:ncol], AF.Sigmoid, rd=[gs], scale=1.702)
                    actf(P, ls, ls[:, 0:ncol], pl[:, 0:ncol], AF.Identity, rd=[pl, bupT], bias=bupT[:, 8 + fc, e_:e_ + 1])
                    ts(P, "pool", ls, ls[:, 0:ncol], ls[:, 0:ncol], 7.0, -7.0, ALU.min, ALU.max, rd=[ls])
                    tt(P, "dve", gs, gs[:, 0:ncol], gs[:, 0:ncol], sg[:, 0:ncol], ALU.mult, rd=[gs, sg])
                    stt(P, "dve", actT, actT[:, fc, 0:ncol], ls[:, 0:ncol], 1.0, gs[:, 0:ncol], ALU.add, ALU.mult, rd=[gs, ls])
                for si in g:
                    n = tiles[si][1]
                    o0 = (si - g[0]) * 128
                    for half in range(2):
                        py = pY.get()
                        for fc in range(8):
                            mm(P, py, py[0:n, :], actT[:, fc, o0:o0 + n], wd[:, fc, half * 512:(half + 1) * 512], [actT, wd], start=(fc == 0), stop=(fc == 7))
                        a = acc[si]
                        stt(P, "dve", a, a[0:n, half * 512:(half + 1) * 512], py[0:n, :], G[0:n, si, e_:e_ + 1], a[0:n, half * 512:(half + 1) * 512],
                            ALU.mult, ALU.add, rd=[py, G, a])
        for si, (row, n, is_s, out_ap) in enumerate(tiles):
            if cur_mod[0] != is_s:
                load_mods(is_s)
                cur_mod[0] = is_s
            x = xt.get()
            P.dma("sp", x[0:n, :], T["x1_d"][row:row + n, :], reads=[x1_d], writes=[x])
            a = acc[si]
            tt(P, "pool", a, a[0:n, :], a[0:n, :], gtf[0:n, :], ALU.mult, rd=[a, gtf])
            tt(P, "dve", a, a[0:n, :], a[0:n, :], x[0:n, :], ALU.add, rd=[a, x])
            actf(P, h2, h2[0:n, :], a[0:n, :], AF.Square, rd=[a], accum=ss[0:n, 0:1], wr_extra=[ss])
            rsqrt_col(P, ss, ss[0:n, 2:3], ss[0:n, 0:1], 1.0 / D, EPS, [ss])
            stt(P, "dve", h2, h2[0:n, :], a[0:n, :], ss[0:n, 2:3], nwf[0:n, :], ALU.mult, ALU.mult, rd=[a, ss, nwf])
            ob = Buf(out_ap)
            P.dma("sp", out_ap, h2[0:n, :], reads=[h2], writes=[ob])
            X.outs.append(ob)


NKS = 8192 + 128


def make_sample_onehots():
    out = np.zeros((4, 33, NKS), np.float32)
    for t in range(4):
        d = np.full(NKS, -1, np.int64)
        d[:8192] = 8192 + t - np.arange(8192)
        for t2 in range(4):
            d[8192 + t2] = t - t2
        out[t] = make_bucket_onehot(d)
    return out.reshape(4 * 33, NKS)


def phase_a2s(P, nc, es, T, X):
    sb, ps = allocators(nc, es, "a2s")
    scale = 64.0 ** -0.5
    nseq = getattr(X, "nseq", SB)
    cst = sb("cst", [128, NCST])
    P.dma("sp", cst[:], T["cst"], writes=[cst])
    identb = sb("identb", [128, 128], BF16)
    cp(P, "dve", identb, identb[:], cst[:, C_IDENT:C_IDENT + 128], rd=[cst])
    relb = sb("relb", [33, 8])
    P.op("dve", lambda e: e.memset(relb[:], NEG), writes=[relb])
    P.dma("sp", relb[0:32, :], T["rel_bias"], writes=[relb])
    lhs_t = sb("lhs_t", [33, 4, 32])
    P.op("dve", lambda e: e.memset(lhs_t[:], 0.0), writes=[lhs_t])
    for t in range(4):
        cp(P, "dve", lhs_t, lhs_t[:, t, t * 8:(t + 1) * 8], relb[:, :], rd=[relb])
    tab = sb("tab", [32, NKS])
    ohs = Rot([sb("ohs%d" % i, [33, 4, 512]) for i in range(2)])
    pS = Rot([ps("pS%d" % i, [128, 512]) for i in range(2)])
    ohv = T["ohs"].rearrange("(t b) n -> b t n", t=4)
    for j in range(0, NKS, 512):
        w = min(512, NKS - j)
        o = ohs.get()
        P.dma("sp", o[:, :, 0:w], ohv[:, :, j:j + w], writes=[o])
        pp = pS.get()
        for t in range(4):
            mm(P, pp, pp[0:32, 0:w], lhs_t[:, t, :], o[:, t, 0:w], [lhs_t, o], start=(t == 0), stop=(t == 3))
        actf(P, tab, tab[:, j:j + w], pp[0:32, 0:w], AF.Exp, rd=[pp])
    pt = sb("pt", [128, SB * 64], I32)
    P.dma("sp", pt[:, :], T["page_table"].rearrange("s j -> (s j)").partition_broadcast(128), writes=[pt])
    ptf = sb("ptf", [128, SB * 64])
    cp(P, "dve", ptf, ptf[:, :], pt[:, :], rd=[pt])
    pcol = sb("pcol", [128, 1])
    P.dma("sp", pcol[:, :], T["pcol"], writes=[pcol])
    ts(P, "dve", ptf, ptf[:, :], ptf[:, :], 128.0, pcol[:, 0:1], ALU.mult, ALU.add, rd=[ptf, pcol])
    pidx = sb("pidx", [128, SB * 64], I32)
    cp(P, "dve", pidx, pidx[:, :], ptf[:, :], rd=[ptf])
    sel = sb("sel", [32, 4])
    P.dma("sp", sel[:, :], T["sel01"], writes=[sel])
    qf = sb("qf", [4, 512]); qb = sb("qb", [4, 512], BF16)
    lq = sb("lq", [128, 4, 32], BF16)
    P.op("dve", lambda e: e.memset(lq[:], 0.0), writes=[lq])
    kpg = Rot([sb("kpg%d" % i, [128, 256], BF16) for i in range(4)])
    vall = sb("vall", [128, 65, 256], BF16)
    vslots = [bv2(vall, j, "v%d" % j) for j in range(65)]
    vslots = [Sub(vall.t, (lambda idx, j=j: (idx[0], j) + tuple(idx[1:])), "v%d" % j) for j in range(65)]
    ktd = Rot([sb("ktd%d" % i, [128, 4, 128], BF16) for i in range(3)])
    knew = sb("knew", [128, 256], BF16)
    kdups = Rot([sb("kdd%d" % i, [128, 4, 2, 64], BF16) for i in range(3)])
    kvn = sb("kvn", [4, 512])
    Sa = sb("Sa", [32, NKS]); Ee = sb("Ee", [32, NKS]); Pb = sb("Pb", [32, NKS], BF16)
    PTs = Rot([sb("PTs%d" % i, [128, 16, 32], BF16) for i in range(2)])
    gt = sb("gt", [32, 32]); mx8 = sb("mx8", [32, 8]); m01 = sb("m01", [32, 32]); rm = sb("rm", [32, 1]); nm = sb("nm", [32, 1])
    rs = sb("rs", [32, 1]); rcp = sb("rcp", [32, 1]); osel = sb("osel", [32, 4, 64]); ao = sb("ao", [32, 64])
    pT = Rot([ps("pT%d" % i, [128, 8, 128], BF16) for i in range(2)])
    pP = Rot([ps("pP%d" % i, [128, 16, 32], BF16) for i in range(2)])
    pO = ps("pO", [128, 512])
    mix_d = X.mix_d
    nk_sd = Buf(T["nk_s"]); nv_sd = Buf(T["nv_s"])
    ck = T["cache_k"]; cv = T["cache_v"]
    P.op("pool", lambda e: e.memset(knew[:], 0.0), writes=[knew])

    for s in range(nseq):
        P.dma("sp", qf[:, :], T["q_s"][4 * s:4 * s + 4, :], reads=[X.q_sd], writes=[qf])
        cp(P, "dve", qb, qb[:, :], qf[:, :], rd=[qf])
        pq = pT.get()
        for hk in range(4):
            tr(P, pq, pq[:, hk, 0:4], qb[:, hk * 128:(hk + 1) * 128], identb[0:4, 0:4], [qb, identb])
        lqv = lq[:, :, :].rearrange("p h (t q) -> p h t q", q=8)
        for hk in range(4):
            cp(P, "dve", lq, lqv[0:64, hk, :, 2 * hk], pq[0:64, hk, 0:4], rd=[pq])
            cp(P, "act", lq, lqv[64:128, hk, :, 2 * hk + 1], pq[64:128, hk, 0:4], rd=[pq])
        P.dma("sp", kvn[:, 0:256], T["nk_s"][4 * s:4 * s + 4, :], reads=[X.nks_buf], writes=[kvn])
        P.dma("sp", kvn[:, 256:512], T["nv_s"][4 * s:4 * s + 4, :], reads=[X.nvs_buf], writes=[kvn])
        cp(P, "pool", knew, knew[0:4, :], kvn[:, 0:256], rd=[kvn])
        vn = vslots[64]
        P.op("pool", lambda e, vn=vn: e.memset(vn[:, :], 0.0), writes=[vn])
        cp(P, "pool", vn, vn[0:4, :], kvn[:, 256:512], rd=[kvn])
        pp = None
        for j in range(65):
            if j < 64:
                kp = kpg.get()
                vj = vslots[j]

                def issue(eng, kp=kp, vj=vj, idx=s * 64 + j):
                    eng.indirect_dma_start(out=kp[:, :], out_offset=None, in_=ck[:, :],
                                           in_offset=bass.IndirectOffsetOnAxis(ap=pidx[:, idx:idx + 1], axis=0)).then_inc(P._k_sem, 16)
                    return eng.indirect_dma_start(out=vj[:, :], out_offset=None, in_=cv[:, :],
                                                  in_offset=bass.IndirectOffsetOnAxis(ap=pidx[:, idx:idx + 1], axis=0))
                P.dma_custom("pool", issue, reads=[pidx], writes=[kp, vj], extra=1)
                ksrc = kp
            else:
                ksrc = knew
            pk = pT.get()
            kdd = kdups.get()
            cp(P, "pool" if j % 2 == 0 else "dve", kdd, kdd[:, :, :, :],
               ksrc[:, :].rearrange("p (h d) -> p h d", h=4).unsqueeze(2).to_broadcast([128, 4, 2, 64]), rd=[ksrc])
            for hk in range(4):
                tr(P, pk, pk[:, hk, :], kdd[:, hk, :, :].rearrange("p r d -> p (r d)"), identb[:, :], [kdd, identb], inc=(hk == 3))
            kd = ktd.get()
            cp(P, "act" if j % 2 == 0 else "dve", kd, kd[:, :, :], pk[:, 0:4, :], rd=[pk])
            if j % 4 == 0:
                pp = pS.get()
            for hk in range(4):
                mm(P, pp, pp[0:32, (j % 4) * 128:(j % 4 + 1) * 128], lq[:, hk, :], kd[:, hk, :], [lq, kd], start=(hk == 0), stop=(hk == 3))
            if j % 4 == 3 or j == 64:
                c0 = (j // 4) * 512
                w = (j % 4 + 1) * 128
                cp(P, "dve", Sa, Sa[:, c0:c0 + w], pp[0:32, 0:w], rd=[pp])
        P.op("dve", lambda e: e.tensor_reduce(gt[:, :], Sa[:, 0:8192].rearrange("p (n k) -> p n k", k=256), AX.X, ALU.add), reads=[Sa], writes=[gt])
        P.op("dve", lambda e: e.max(mx8[:], gt[:]), reads=[gt], writes=[mx8])
        ts(P, "dve", m01, m01[:], gt[:], mx8[:, 2:3], None, ALU.is_ge, rd=[gt, mx8])
        P.op("dve", lambda e: e.reduce_max(rm[:], Sa[:, :], AX.X), reads=[Sa], writes=[rm])
        ts(P, "dve", nm, nm[:], rm[:], -scale, None, ALU.mult, rd=[rm])
        actf(P, Ee, Ee[:, :], Sa[:, :], AF.Exp, rd=[Sa, nm], scale=scale, bias=nm[:, 0:1])
        tt(P, "pool", Ee, Ee[:, 0:8192].rearrange("p (n k) -> p n k", k=256), Ee[:, 0:8192].rearrange("p (n k) -> p n k", k=256),
           m01[:, :].unsqueeze(2).to_broadcast([32, 32, 256]), ALU.mult, rd=[Ee, m01])
        tt(P, "dve", Ee, Ee[:, :], Ee[:, :], tab[:, :], ALU.mult, rd=[Ee, tab])
        P.op("dve", lambda e: e.reduce_sum(rs[:, 0:1], Ee[:, :], AX.X), reads=[Ee], writes=[rs])
        cp(P, "pool", Pb, Pb[:, :], Ee[:, :], rd=[Ee])
        for g0 in range(0, 65, 16):
            g1 = min(g0 + 16, 65)
            ptp = pP.get()
            for j in range(g0, g1):
                tr(P, ptp, ptp[:, j - g0, :], Pb[:, j * 128:(j + 1) * 128], identb[0:32, 0:32], [Pb, identb], inc=(j == g1 - 1))
            pts = PTs.get()
            cp(P, "act" if (g0 // 16) % 2 == 0 else "dve", pts, pts[:, 0:g1 - g0, :], ptp[:, 0:g1 - g0, :], rd=[ptp])
            for j in range(g0, g1):
                mm(P, pO, pO[0:32, 0:256], pts[:, j - g0, :], vslots[j][:, :], [pts, vslots[j]], start=(j == 0), stop=(j == 64), inc=(j == g1 - 1))
        tt(P, "dve", osel, osel[:, :, :], pO[0:32, 0:256].rearrange("p (h d) -> p h d", h=4), sel[:, :].unsqueeze(2).to_broadcast([32, 4, 64]), ALU.mult,
           rd=[pO, sel])
        P.op("dve", lambda e: e.tensor_reduce(ao[:, :], osel[:, :, :].rearrange("p h d -> p d h"), AX.X, ALU.add), reads=[osel], writes=[ao])
        P.op("dve", lambda e: e.reciprocal(rcp[:], rs[:]), reads=[rs], writes=[rcp])
        ts(P, "dve", ao, ao[:, :], ao[:, :], rcp[:, 0:1], None, ALU.mult, rd=[ao, rcp])
        for t in range(4):
            P.dma("sp", T["mix_d"][SEQ + 4 * s + t, 0:512].rearrange("(h d) -> h d", h=8), ao[8 * t:8 * t + 8, :], reads=[ao], writes=[mix_d])


ALL_STAGES = ("p0", "a1", "a2", "smp", "a3", "b")
_NC_CACHE = {}


def kernel(**inputs):
    inp = {k: np.asarray(v) for k, v in inputs.items()}
    if "nc" not in _NC_CACHE:
        _NC_CACHE["nc"] = build(ALL_STAGES)
    nc = _NC_CACHE["nc"]
    in_maps = [shard_inputs(inp, c) for c in range(NCORES)]
    res = run_bass_kernel_spmd(nc, in_maps, core_ids=list(range(NCORES)))
    R = res.results
    cat = lambda name: [np.asarray(R[c][name]) for c in range(NCORES)]
    y_p = np.stack(cat("y_p"), 0).reshape(8, SEQ, D)
    y_s = np.concatenate(cat("y_s"), 0).reshape(128, 4, D)
    nk_p = np.stack(cat("nk_p"), 0).reshape(1, 8, SEQ, 4, 64)
    nv_p = np.stack(cat("nv_p"), 0).reshape(1, 8, SEQ, 4, 64)
    nc_p = np.stack(cat("nc_p"), 0).reshape(1, 8, 3, 1536)
    ns_p = np.stack(cat("ns_p"), 0).reshape(1, 8, 4, 128, 128)
    nk_s = np.concatenate(cat("nk_s"), 0).reshape(1, 128, 4, 4, 64)
    nv_s = np.concatenate(cat("nv_s"), 0).reshape(1, 128, 4, 4, 64)
    nc_s = np.concatenate(cat("nc_s"), 0).reshape(1, 128, 3, 1536)
    ns_s = np.concatenate(cat("ns_s"), 0).reshape(1, 128, 4, 128, 128)
    return tuple(a.astype(np.float32, copy=False) for a in (y_p, y_s, nk_p, nv_p, nc_p, ns_p, nk_s, nv_s, nc_s, ns_s))
```

```python
import numpy as np
import concourse.bass as bass
import concourse.mybir as mybir
from concourse.bass_utils import run_bass_kernel_spmd

F32 = mybir.dt.float32
BF16 = mybir.dt.bfloat16
I32 = mybir.dt.int32
AF = mybir.ActivationFunctionType
ALU = mybir.AluOpType
AX = mybir.AxisListType

NCORES = 8
D = 1024
SEQ = 4096
NT = SEQ // 128
SB = 16
ST = SB * 4
IN_W = 3080
NEG = -30000.0
EPS = 1e-6
NE = 32
SEM_LIMIT = 20000


class Buf:
    __slots__ = ("t", "lw", "rd", "name")

    def __init__(self, t, name=""):
        self.t = t
        self.lw = None
        self.rd = []
        self.name = name

    def __getitem__(self, idx):
        return self.t[idx]


class Sub(Buf):
    __slots__ = ("pre",)

    def __init__(self, t, pre, name=""):
        super().__init__(t, name)
        self.pre = pre

    def __getitem__(self, idx):
        if not isinstance(idx, tuple):
            idx = (idx,)
        return self.t[self.pre(idx) if callable(self.pre) else tuple(self.pre) + idx]


class Prog:
    COMPUTE = ("pe", "act", "dve", "pool")

    def __init__(self, nc):
        self.nc = nc
        self.eng = {"pe": nc.tensor, "act": nc.scalar, "dve": nc.vector, "pool": nc.gpsimd, "sp": nc.sync}
        self.rec = {k: [] for k in self.eng}
        self.sems = {}
        self.cur = {}
        self.seen = {k: {} for k in self.eng}
        self.nsem = 0
        for k in self.eng:
            self._new_eng_sem(k)
        self.dq = {}
        self.dq_next = {}
        for q in ("sp", "pool", "act"):
            lst = []
            for i in range(12):
                key = self._alloc_sem("d%s%d" % (q, i))
                lst.append([key, 0])
            self.dq[q] = lst
            self.dq_next[q] = 0

    def _alloc_sem(self, name):
        key = "%s_%d" % (name, self.nsem)
        self.nsem += 1
        self.sems[key] = self.nc.alloc_semaphore(key)
        return key

    def _new_eng_sem(self, e):
        self.cur[e] = [self._alloc_sem("e" + e), 0]

    def _collect(self, e, reads, writes, is_dma):
        need = {}

        def add(dep, raw):
            if dep is None:
                return
            key, val, prod = dep
            if (not is_dma) and (not raw) and prod == e and e == "pe":
                return
            if self.seen[e].get(key, 0) >= val:
                return
            if need.get(key, 0) < val:
                need[key] = val

        for b in reads:
            add(b.lw, True)
        for b in writes:
            add(b.lw, False)
            for r in b.rd:
                add(r, False)
        return need

    def _emit_waits(self, e, need):
        for key, val in need.items():
            self.seen[e][key] = val
            sem = self.sems[key]
            self.rec[e].append(lambda eng, sem=sem, val=val: eng.wait_ge(sem, val))

    def op(self, e, fn, reads=(), writes=(), inc=True):
        need = self._collect(e, reads, writes, False)
        self._emit_waits(e, need)
        pend = getattr(self, "pending", None)
        if pend is None:
            pend = self.pending = {k: False for k in self.eng}
        if self.cur[e][1] >= SEM_LIMIT and not pend[e]:
            self._new_eng_sem(e)
        cur = self.cur[e]
        if inc:
            cur[1] += 1
            key, val = cur[0], cur[1]
            sem = self.sems[key]
            self.rec[e].append(lambda eng, fn=fn, sem=sem: fn(eng).then_inc(sem, 1))
            pend[e] = False
        else:
            key, val = cur[0], cur[1] + 1
            self.rec[e].append(lambda eng, fn=fn: fn(eng))
            pend[e] = True
        dep = (key, val, e)
        for b in writes:
            b.lw = dep
            b.rd = []
        for b in reads:
            b.rd.append(dep)

    def dma(self, q, out_ap, in_ap, reads=(), writes=(), **kw):
        need = self._collect(q, reads, writes, True)
        lst = self.dq[q]
        i = self.dq_next[q]
        self.dq_next[q] = (i + 1) % len(lst)
        slot = lst[i]
        if slot[1] > 0 and self.seen[q].get(slot[0], 0) < slot[1]:
            if need.get(slot[0], 0) < slot[1]:
                need[slot[0]] = slot[1]
        self._emit_waits(q, need)
        slot[1] += 16
        key, val = slot[0], slot[1]
        sem = self.sems[key]
        self.rec[q].append(lambda eng, o=out_ap, i_=in_ap, sem=sem, kw=kw: eng.dma_start(out=o, in_=i_, **kw).then_inc(sem, 16))
        dep = (key, val, "dma")
        for b in writes:
            b.lw = dep
            b.rd = []
        for b in reads:
            b.rd.append(dep)

    def dma_custom(self, q, issue, reads=(), writes=(), extra=0):
        need = self._collect(q, reads, writes, True)
        lst = self.dq[q]
        slots = []
        for _ in range(1 + extra):
            i = self.dq_next[q]
            self.dq_next[q] = (i + 1) % len(lst)
            slot = lst[i]
            if slot[1] > 0 and self.seen[q].get(slot[0], 0) < slot[1]:
                if need.get(slot[0], 0) < slot[1]:
                    need[slot[0]] = slot[1]
            slots.append(slot)
        self._emit_waits(q, need)
        deps = []
        for slot in slots:
            slot[1] += 16
            deps.append((slot[0], slot[1], "dma"))
        sems = [self.sems[sl[0]] for sl in slots]

        def run(eng, issue=issue, sems=sems):
            self._k_sem = sems[0]
            issue(eng).then_inc(sems[-1], 16)
        self.rec[q].append(run)
        for b, dep in zip(writes, deps):
            b.lw = dep
            b.rd = []
        for b in reads:
            b.rd.extend(deps)

    def wait_all(self, e, bufs):
        need = {}
        for b in bufs:
            if b.lw is not None:
                key, val, _ = b.lw
                if self.seen[e].get(key, 0) < val and need.get(key, 0) < val:
                    need[key] = val
        self._emit_waits(e, need)

    def barrier(self):
        for e in self.eng:
            need = {}
            for e2, (key, val) in self.cur.items():
                if val > 0 and self.seen[e].get(key, 0) < val:
                    need[key] = val
            for q, lst in self.dq.items():
                for key, val in lst:
                    if val > 0 and self.seen[e].get(key, 0) < val:
                        need[key] = val
            self._emit_waits(e, need)

    def emit(self):
        nc = self.nc
        rec = self.rec
        self.rec = {k: [] for k in self.eng}
        self._emit(rec)

    def _emit(self, rec):
        nc = self.nc
        with nc.Block() as block:
            @block.sync
            def _(eng):
                for f in rec["sp"]:
                    f(eng)

            @block.scalar
            def _(eng):
                for f in rec["act"]:
                    f(eng)

            @block.vector
            def _(eng):
                for f in rec["dve"]:
                    f(eng)

            @block.gpsimd
            def _(eng):
                for f in rec["pool"]:
                    f(eng)

            @block.tensor
            def _(eng):
                for f in rec["pe"]:
                    f(eng)


def make_consts():
    c = {}
    c["ident"] = np.eye(128, dtype=np.float32)
    j = np.arange(128)[:, None]
    i = np.arange(128)[None, :]
    c["tri"] = (j <= i).astype(np.float32)
    c["negus"] = np.where(i < j, 0.0, NEG).astype(np.float32)
    c["negut"] = np.where(i >= j, 0.0, NEG).astype(np.float32)
    c["jrev"] = np.eye(128, dtype=np.float32)[::-1].copy()
    return np.concatenate([c["ident"], c["tri"], c["negus"], c["negut"], c["jrev"]], axis=1)


C_IDENT, C_TRI, C_NEGUS, C_NEGUT, C_JREV = 0, 128, 256, 384, 512
NCST = 640
RLEN = 4224


def rel_bucket_np(dist):
    n = np.maximum(dist, 0)
    nf = np.maximum(n, 16).astype(np.float32)
    large = 16 + (np.log(nf / np.float32(16)) / np.float32(np.log(4096 / 16)) * np.float32(16)).astype(np.int32)
    return np.where(n < 16, n, np.minimum(large, 31))


def make_bucket_onehot(dists):
    oh = np.zeros((33, len(dists)), np.float32)
    b = rel_bucket_np(dists)
    for i, d in enumerate(dists):
        if d < 0:
            oh[32, i] = 1.0
        else:
            oh[b[i], i] = 1.0
    return oh


class Ctx:
    pass


def allocators(nc, es, prefix):
    def sb(name, shape, dt=F32):
        return Buf(es.enter_context(nc.sbuf_tensor("%s_%s" % (prefix, name), list(shape), dt)), name)

    def ps(name, shape, dt=F32):
        return Buf(es.enter_context(nc.psum_tensor("%s_%s" % (prefix, name), list(shape), dt)), name)
    return sb, ps


def declare_io(nc, npool=10240, ne_store=NE, dbg=False):
    T = {}

    def inp(name, shape, dt=F32):
        T[name] = nc.dram_tensor(name, list(shape), dt, kind="ExternalInput").ap()

    def outp(name, shape, dt=F32):
        T[name] = nc.dram_tensor(name, list(shape), dt, kind="ExternalOutput").ap()

    def scr(name, shape, dt=F32):
        T[name] = nc.dram_tensor(name, list(shape), dt, kind="Internal").ap()

    inp("xp", [SEQ, D]); inp("xs", [ST, D]); inp("cc", [1 + SB, D])
    inp("w_ada", [D, 6 * D]); inp("b_ada", [1, 6 * D]); inp("nw", [3, D])
    inp("w_in", [D, IN_W]); inp("rel_bias", [32, 8]); inp("conv_w", [4, 1536])
    inp("a_log", [1, 4]); inp("dt_bias", [1, 4]); inp("gnw", [1, 128]); inp("w_out", [D, D])
    inp("w_router", [D, NE]); inp("b_router", [1, NE])
    inp("w_up", [ne_store, D, 2 * D]); inp("b_up", [NE, 2 * D]); inp("w_down", [ne_store, D, D]); inp("b_down", [NE, D])
    inp("cache_k", [npool * 128, 256]); inp("cache_v", [npool * 128, 256])
    inp("state_conv", [SB * 3, 1536]); inp("state_ssm", [SB * 4 * 128, 128])
    inp("page_table", [SB, 64], I32)
    inp("cst", [128, NCST]); inp("ohp", [33, RLEN]); inp("ohs", [4 * 33, NKS]); inp("sel01", [32, 4]); inp("pcol", [128, 1])
    outp("y_p", [SEQ, D]); outp("y_s", [ST, D])
    outp("nk_p", [SEQ, 256]); outp("nv_p", [SEQ, 256]); outp("nc_p", [3, 1536]); outp("ns_p", [4 * 128, 128])
    outp("nk_s", [ST, 256]); outp("nv_s", [ST, 256]); outp("nc_s", [SB * 3, 1536]); outp("ns_s", [SB * 4 * 128, 128])
    if dbg:
        outp("dbg", [128, 8192])
    scr("m_d", [1 + SB, 6 * D])
    scr("rd", [8, RLEN])
    scr("q_s", [ST, 512])
    scr("wupb", [NE, 128, 8 * 2 * D], BF16)
    scr("wdnb", [NE, 128, 8 * D], BF16)
    scr("mix_d", [SEQ + ST, D])
    scr("x1_d", [SEQ + ST, D])
    return T


def phase0_ada(P, nc, es, T):
    R = 1 + SB
    sb, ps = allocators(nc, es, "p0")
    cst = sb("cst0", [128, NCST])
    cc = sb("cc", [R, D])
    ccT = sb("ccT", [128, 8, R])
    ones = sb("ones", [1, R])
    bada = sb("bada", [1, 6 * D])
    wch = [sb("wch%d" % i, [128, 8, 512]) for i in range(2)]
    mo = [sb("mo%d" % i, [R, 512]) for i in range(2)]
    pT = ps("pT", [128, 8, R])
    pm = [ps("pm%d" % i, [R, 512]) for i in range(2)]
    m_d = Buf(T["m_d"], "m_d")
    P.dma("sp", cst[:], T["cst"], writes=[cst])
    P.dma("sp", cc[:], T["cc"], writes=[cc])
    P.dma("sp", bada[:], T["b_ada"], writes=[bada])
    P.op("dve", lambda e: e.memset(ones[:], 1.0), writes=[ones])
    P.op("act", lambda e: e.activation(out=cc[:], in_=cc[:], func=AF.Silu), reads=[cc], writes=[cc])
    for kc in range(8):
        P.op("pe", lambda e, kc=kc: e.transpose(pT[:, kc, :], cc[:, kc * 128:(kc + 1) * 128], cst[0:R, 0:R]),
             reads=[cc, cst], writes=[pT])
    P.op("dve", lambda e: e.tensor_copy(ccT[:], pT[:]), reads=[pT], writes=[ccT])
    wv = T["w_ada"].rearrange("(kc p) n -> p kc n", p=128)
    for j in range(12):
        w = wch[j % 2]
        P.dma("sp", w[:], wv[:, :, j * 512:(j + 1) * 512], writes=[w])
        pp = pm[j % 2]
        for kc in range(8):
            P.op("pe", lambda e, kc=kc, w=w, pp=pp: e.matmul(pp[:], lhsT=ccT[:, kc, :], rhs=w[:, kc, :], start=(kc == 0), stop=False),
                 reads=[ccT, w], writes=[pp])
        P.op("pe", lambda e, pp=pp, j=j: e.matmul(pp[:], lhsT=ones[:], rhs=bada[:, j * 512:(j + 1) * 512], start=False, stop=True),
             reads=[ones, bada], writes=[pp])
        o = mo[j % 2]
        P.op("act", lambda e, o=o, pp=pp: e.copy(out=o[:], in_=pp[:]), reads=[pp], writes=[o])
        P.dma("sp", T["m_d"][:, j * 512:(j + 1) * 512], o[:], reads=[o], writes=[m_d])
    return m_d


def build(stages=("p0",), npool=10240, ne_store=NE, dbg=False, **xkw):
    from contextlib import ExitStack
    nc = bass.Bass("TRN2", target_bir_lowering=False)
    T = declare_io(nc, npool, ne_store, dbg)
    P = Prog(nc)
    X = Ctx()
    X.T = T
    for k_, v_ in xkw.items():
        setattr(X, k_, v_)
    X.outs = []
    X.mix_d = Buf(T["mix_d"], "mix_d")
    X.x1_d = Buf(T["x1_d"], "x1_d")
    X.q_sd = Buf(T["q_s"], "q_s")
    X.ncs = Buf(T["nc_s"], "nc_s")
    if "p0" in stages:
        with ExitStack() as es:
            X.m_d = phase0_ada(P, nc, es, T)
            P.barrier()
            if dbg and stages == ("p0",):
                P.dma("sp", T["dbg"][0:17, 0:6144], T["m_d"], reads=[X.m_d])
                P.barrier()
            P.emit()
    if "a1" in stages:
        with ExitStack() as esA:
            alloc_attn_persist(nc, esA, X)
            with ExitStack() as es:
                alloc_a1_extra(nc, es, X)
                phase_a1(P, nc, es, T, X, sample=False)
                if dbg and "a2" not in stages:
                    P.dma("sp", T["dbg"][:, 0:4096], X.mix_d[0:128, :].rearrange("p (a b) -> p a b", a=1)[:, 0, :] if False else T["mix_d"][0:128, :].rearrange("p d -> p d")[:, :], reads=[X.mix_d]) if False else None
                P.barrier()
                P.emit()
            if "a2" in stages:
                with ExitStack() as es:
                    X.rdb = build_rel_table(P, nc, es, T, X, 8, "ohp", RLEN, "rd", "rt")
                    P.barrier()
                    P.emit()
                with ExitStack() as es:
                    phase_a2(P, nc, es, T, X)
                    P.barrier()
                    P.emit()
    if "smp" in stages:
        with ExitStack() as es:
            X.nks_buf = Buf(T["nk_s"], "nk_s"); X.nvs_buf = Buf(T["nv_s"], "nv_s")
            alloc_a1_extra(nc, es, X, "a1xs")
            phase_a1(P, nc, es, T, X, sample=True)
            P.barrier()
            P.emit()
        with ExitStack() as es:
            phase_a2s(P, nc, es, T, X)
            P.barrier()
            P.emit()
    if "a3" in stages:
        with ExitStack() as es:
            def tiles(gta_p, gta_s):
                for t in range(getattr(X, "ntile", NT)):
                    yield (t * 128, 128, T["xp"][t * 128:(t + 1) * 128, :], gta_p)
                if "smp" in stages:
                    yield (SEQ, 4 * getattr(X, "nseq", SB), T["xs"][0:4 * getattr(X, "nseq", SB), :], gta_s)
            phase_a3(P, nc, es, T, X, tiles)
            P.barrier()
            if dbg:
                nt_ = getattr(X, "ntile", NT)
                for t in range(min(nt_, 16)):
                    P.dma("sp", T["dbg"][:, t * 512:(t + 1) * 512], T["mix_d"][t * 128:(t + 1) * 128, 0:512], reads=[X.mix_d])
            P.barrier()
            P.emit()
    if "b" in stages:
        with ExitStack() as es:
            nt_ = getattr(X, "ntile", NT)
            alltiles = [(t * 128, 128, False, T["y_p"][t * 128:(t + 1) * 128, :]) for t in range(nt_)]
            per = getattr(X, "per_pass", 6)
            passes = [alltiles[i:i + per] for i in range(0, len(alltiles), per)]
            if "smp" in stages:
                ns_ = 4 * getattr(X, "nseq", SB)
                stile = (SEQ, ns_, True, T["y_s"][0:ns_, :])
                if passes and len(passes[-1]) < per:
                    passes[-1].append(stile)
                else:
                    passes.append([stile])
            phase_b(P, nc, es, T, X, passes, ne=getattr(X, "ne", NE))
            P.barrier()
            P.emit()
    return nc


def shard_inputs(inp, c):
    f = lambda a: np.ascontiguousarray(a)
    m = {}
    m["xp"] = f(inp["x_prompt"][c])
    m["xs"] = f(inp["x_sample"][c * SB:(c + 1) * SB].reshape(ST, D))
    m["cc"] = f(np.concatenate([inp["c_prompt"][c:c + 1], inp["c_sample"][c * SB:(c + 1) * SB]], axis=0))
    m["w_ada"] = f(inp["w_ada"][0]); m["b_ada"] = f(inp["b_ada"])
    m["nw"] = f(np.stack([inp["norm_attn_w"][0], inp["norm_ffn_w"][0], inp["norm_final_w"]], axis=0))
    m["w_in"] = f(inp["w_in"][0]); m["rel_bias"] = f(inp["rel_bias"]); m["conv_w"] = f(inp["conv_w"][0])
    m["a_log"] = f(inp["a_log"]); m["dt_bias"] = f(inp["dt_bias"]); m["gnw"] = f(inp["gdn_norm_w"])
    m["w_out"] = f(inp["w_out"][0]); m["w_router"] = f(inp["w_router"][0]); m["b_router"] = f(inp["b_router"])
    m["w_up"] = f(inp["w_up"][0]); m["b_up"] = f(inp["b_up"][0]); m["w_down"] = f(inp["w_down"][0]); m["b_down"] = f(inp["b_down"][0])
    m["cache_k"] = inp["cache_k"].reshape(-1, 256); m["cache_v"] = inp["cache_v"].reshape(-1, 256)
    m["state_conv"] = f(inp["state_conv"][0, c * SB:(c + 1) * SB].reshape(SB * 3, 1536))
    m["state_ssm"] = f(inp["state_ssm"][0, c * SB:(c + 1) * SB].reshape(SB * 4 * 128, 128))
    m["page_table"] = f(inp["page_table"][c * SB:(c + 1) * SB])
    m["cst"] = make_consts()
    m["ohp"] = make_bucket_onehot(4095 - np.arange(RLEN))
    m["ohs"] = make_sample_onehots()
    m["pcol"] = np.arange(128, dtype=np.float32).reshape(128, 1)
    m["sel01"] = (np.arange(4)[None, :] == ((np.arange(32) % 8) // 2)[:, None]).astype(np.float32)
    return m


def mm(P, ob, oap, lhsT, rhs, rd, start=True, stop=True, inc=None):
    P.op("pe", lambda e: e.matmul(oap, lhsT=lhsT, rhs=rhs, start=start, stop=stop), reads=rd, writes=[ob], inc=(stop if inc is None else inc))


def tr(P, ob, oap, in_ap, ident_ap, rd, inc=True):
    P.op("pe", lambda e: e.transpose(oap, in_ap, ident_ap), reads=rd, writes=[ob], inc=inc)


def tr32(P, ob, oap, in_ap, ident_ap, rd):
    P.op("pe", lambda e: e.matmul(oap, lhsT=in_ap, rhs=ident_ap, start=True, stop=True), reads=rd, writes=[ob])


def actf(P, ob, oap, in_ap, func, rd, bias=None, scale=None, accum=None, wr_extra=(), eng="act"):
    kw = {}
    if bias is not None:
        kw["bias"] = bias
    if scale is not None:
        kw["scale"] = scale
    if accum is not None:
        kw["accum_out"] = accum
    P.op(eng, lambda e: e.activation(out=oap, in_=in_ap, func=func, **kw), reads=rd, writes=[ob] + list(wr_extra))


def ts(P, eng, ob, oap, in0, s1, s2, op0, op1=None, rd=(), accum=None, wr_extra=()):
    kw = {}
    if op1 is not None:
        kw["op1"] = op1
    if accum is not None:
        kw["accum_out"] = accum
    P.op(eng, lambda e: e.tensor_scalar(oap, in0, s1, s2, op0, **kw), reads=rd, writes=[ob] + list(wr_extra))


def tt(P, eng, ob, oap, in0, in1, op, rd=()):
    P.op(eng, lambda e: e.tensor_tensor(oap, in0, in1, op), reads=rd, writes=[ob])


def stt(P, eng, ob, oap, in0, scalar, in1, op0, op1, rd=()):
    P.op(eng, lambda e: e.scalar_tensor_tensor(oap, in0, scalar, in1, op0, op1), reads=rd, writes=[ob])


def cp(P, eng, ob, oap, in_ap, rd=()):
    if eng == "act":
        P.op(eng, lambda e: e.copy(out=oap, in_=in_ap), reads=rd, writes=[ob])
    else:
        P.op(eng, lambda e: e.tensor_copy(oap, in_ap), reads=rd, writes=[ob])


def rsqrt_col(P, ob, oap, in_ap, scale, eps, rd, post=1.0):
    actf(P, ob, oap, in_ap, AF.Ln, rd=rd, scale=scale, bias=eps)
    if post != 1.0:
        actf(P, ob, oap, oap, AF.Exp, rd=[ob], scale=-0.5, bias=float(np.log(post)))
    else:
        actf(P, ob, oap, oap, AF.Exp, rd=[ob], scale=-0.5)


class Rot:
    def __init__(self, bufs):
        self.b = bufs
        self.i = 0

    def get(self):
        b = self.b[self.i % len(self.b)]
        self.i += 1
        return b


def phase_a1(P, nc, es, T, X, sample=False):
    pre = "a1s" if sample else "a1"
    sb, ps = allocators(nc, es, pre)
    ntile = getattr(X, 'nseq', SB) if sample else getattr(X, 'ntile', NT)
    TP = 4 if sample else 128
    TS = TP
    x_d = T["xs"] if sample else T["xp"]
    row0 = SEQ if sample else 0
    cst = sb("cst", [128, NCST])
    P.dma("sp", cst[:], T["cst"], writes=[cst])
    ident = cst[:, C_IDENT:C_IDENT + 128]
    tri = cst[:, C_TRI:C_TRI + 128]
    negus = cst[:, C_NEGUS:C_NEGUS + 128]
    negut = cst[:, C_NEGUT:C_NEGUT + 128]
    identb = sb("identb", [128, 128], BF16)
    cp(P, "dve", identb, identb[:], ident, rd=[cst])
    jrevb = sb("jrevb", [128, 128], BF16)
    cp(P, "dve", jrevb, jrevb[:], cst[:, C_JREV:C_JREV + 128], rd=[cst])
    wmod = sb("wmod", [128, D]); shb = sb("shb", [128, D]); tmp = sb("tmp", [128, D]); nwb = tmp; junk = tmp
    m_d = X.m_d
    if not sample:
        P.dma("sp", shb[:], T["m_d"][0, 0:D].partition_broadcast(128), reads=[m_d], writes=[shb])
        P.dma("sp", wmod[:], T["m_d"][0, D:2 * D].partition_broadcast(128), reads=[m_d], writes=[wmod])
    if sample:
        nwb2 = sb("nwb2", [4, D])
        P.dma("sp", nwb2[:], T["nw"][0, :].partition_broadcast(4), writes=[nwb2])
    if not sample:
        P.dma("sp", nwb[:], T["nw"][0, :].partition_broadcast(128), writes=[nwb])
        stt(P, "dve", wmod, wmod[0:TP, :], wmod[0:TP, :], 1.0, nwb[0:TP, :], ALU.add, ALU.mult, rd=[wmod, nwb])
    w_in = sb("w_in", [128, 8, IN_W], BF16)
    wiv = T["w_in"].rearrange("(kc p) n -> p kc n", p=128)
    for kc in range(8):
        for hh in range(2):
            P.dma("pool", w_in[:, kc, hh * 1540:(hh + 1) * 1540], wiv[:, kc, hh * 1540:(hh + 1) * 1540], writes=[w_in])
    cw = sb("cw", [128, 12, 4])
    with nc.allow_non_contiguous_dma(reason="tiny conv weight transpose"):
        pass
    for i in range(4):
        P.dma("sp", cw[:, :, i], T["conv_w"][i, :].rearrange("(j p) -> p j", p=128), writes=[cw], allow_slow_non_contiguous=True)
    dtb = sb("dtb", [128, 4]); negA = sb("negA", [128, 4]); gnw = sb("gnw", [128, 128])
    P.dma("sp", dtb[:], T["dt_bias"][0, :].partition_broadcast(128), writes=[dtb])
    P.dma("sp", negA[:], T["a_log"][0, :].partition_broadcast(128), writes=[negA])
    P.dma("sp", gnw[:], T["gnw"][0, :].partition_broadcast(128), writes=[gnw])
    actf(P, negA, negA[:], negA[:], AF.Exp, rd=[negA])
    ts(P, "dve", negA, negA[:], negA[:], -1.0, None, ALU.mult, rd=[negA])
    if getattr(X, 'stopat', 99) == 1:
        return

    xt = Rot([sb("x%d" % i, [128, D]) for i in range(1)])
    hb = sb("hb", [128, D], BF16); hT = sb("hT", [128, 8, 128], BF16)
    ss = sb("ss", [128, 8])
    q_tm = sb("q_tm", [128, 512]); kv_tm = Rot([sb("kv%d" % i, [128, 512]) for i in range(1)])
    sz = sb("sz", [128, 512]); ba = sb("ba", [128, 8])
    ext = sb("ext", [128, 12, 131]); cacc = sb("cacc", [128, 12, 128]); ctmp = sb("ctmp", [128, 12, 128])
    afm = sb("afm", [128, 12, 128])
    pre_tm = ctmp
    beta = sb("beta", [128, 4]); nbeta = sb("nbeta", [128, 4]); g = sb("g", [128, 4]); gc = sb("gc", [128, 4]); ngc = sb("ngc", [128, 4])
    egc = sb("egc", [128, 4]); bege = sb("bege", [128, 4])
    S = [sb("S%d" % h, [128, 128]) for h in range(4)]
    mixg = sb("mixg", [128, 512])
    pTr = ps("pTr", [128, 8, 128], BF16)
    pA = Rot([ps("pA%d" % i, [128, 512]) for i in range(2)])
    gbanks = [ps("pG%d" % i, [128, 4, 128]) for i in range(4)]
    gps = Rot([bv2(b, j, "g%d" % j) for b in gbanks for j in range(4)])
    names = ["kh", "qh", "vb", "kbe", "kt", "qt", "qtt", "Dm", "DTm", "EG", "Nm", "NTm", "Pa", "PTa", "Pb", "PTb", "XTa", "XTb",
             "wT", "IT", "usb", "vnew", "ktil", "tmpg"]
    hsets = [{n: sb("%s_%d" % (n, i), [128, 128]) for n in names} for i in range(1)] * 2
    cols = [{n: sb("%s_%d" % (n, i), [128, 1]) for n in ["rq", "rk", "glast", "eglast", "ekl", "rso", "ssq", "ssk", "sso"]} for i in range(2)]
    for h in range(4):
        P.op("dve", lambda e, h=h: e.memset(S[h][:], 0.0), writes=[S[h]])
    P.op("dve", lambda e: e.memset(ext[:], 0.0), writes=[ext])
    nk_d = X.nks_buf if sample else Buf(T["nk_p"]); nv_d = X.nvs_buf if sample else Buf(T["nv_p"])
    mix_d = X.mix_d
    n = TP

    if sample:
        stc = sb("stc", [SB * 3, 1536])
        P.dma("sp", stc[:], T["state_conv"], writes=[stc])
        Ssm = Rot([[sb("Ss%d_%d" % (i, h), [128, 128]) for h in range(4)] for i in range(2)])
        ns_s = Buf(T["ns_s"], "ns_s")
        q_sd = X.q_sd
    for t in range(ntile):
        x = xt.get()
        P.dma("sp", x[0:n, :], x_d[t * TS:t * TS + n, :], writes=[x])
        if sample:
            P.dma("sp", shb[0:4, :], T["m_d"][1 + t, 0:D].partition_broadcast(4), reads=[m_d], writes=[shb])
            P.dma("sp", wmod[0:4, :], T["m_d"][1 + t, D:2 * D].partition_broadcast(4), reads=[m_d], writes=[wmod])
            stt(P, "dve", wmod, wmod[0:4, :], wmod[0:4, :], 1.0, nwb2[0:4, :], ALU.add, ALU.mult, rd=[wmod, nwb2])
            for jb in range(3):
                pp = pA.get()
                for jj in range(4):
                    j = jb * 4 + jj
                    P.op("pe", lambda e, pp=pp, jj=jj, j=j, t=t: e.matmul(pp[:, jj * 128:jj * 128 + 3], lhsT=stc[:, j * 128:(j + 1) * 128],
                                                                         rhs=ident[0:48, 3 * t:3 * t + 3], start=True, stop=True),
                         reads=[stc, cst], writes=[pp])
                cp(P, "dve", ext, ext[:, jb * 4:(jb + 1) * 4, 0:3], pp[:, :].rearrange("p (j t) -> p j t", j=4)[:, :, 0:3], rd=[pp])
            Scur = Ssm.get()
            for h in range(4):
                r = (t * 4 + h) * 128
                P.dma("sp", Scur[h][:, :], T["state_ssm"][r:r + 128, :], writes=[Scur[h]])
            X.S_for = lambda h, Scur=Scur: Scur[h]
        actf(P, junk, junk[0:n, :], x[0:n, :], AF.Square, rd=[x], accum=ss[0:n, 0:1], wr_extra=[ss])
        rsqrt_col(P, ss, ss[0:n, 2:3], ss[0:n, 0:1], 1.0 / D, EPS, [ss])
        stt(P, "dve", tmp, tmp[0:n, :], x[0:n, :], ss[0:n, 2:3], wmod[0:n, :], ALU.mult, ALU.mult, rd=[x, ss, wmod])
        tt(P, "pool", hb, hb[0:n, :], tmp[0:n, :], shb[0:n, :], ALU.add, rd=[tmp, shb])
        if getattr(X, 'stopat', 99) == 2:
            return

        for kc in range(8):
            tr(P, pTr, pTr[:, kc, 0:n], hb[0:n, kc * 128:(kc + 1) * 128], identb[0:n, 0:n], rd=[hb, identb], inc=(kc == 7))
        cp(P, "act", hT, hT[:, :, 0:n], pTr[:, :, 0:n], rd=[pTr])
        if getattr(X, 'stopat', 99) == 3:
            return

        for gi, (c0, wdt) in enumerate([(0, 512), (512, 512), (2560, 512), (3072, 8)]):
            pp = pA.get()
            for kc in range(8):
                mm(P, pp, pp[0:n, 0:wdt], hT[:, kc, 0:n], w_in[:, kc, c0:c0 + wdt], [hT, w_in], start=(kc == 0), stop=(kc == 7))
            if gi == 0:
                cp(P, "act", q_tm, q_tm[0:n, :], pp[0:n, :], rd=[pp])
            elif gi == 1:
                kv = kv_tm.get()
                cp(P, "dve", kv, kv[0:n, :], pp[0:n, :], rd=[pp])
                P.dma("sp", nk_d[t * TS:t * TS + n, :], kv[0:n, 0:256], reads=[kv], writes=[nk_d])
                P.dma("sp", nv_d[t * TS:t * TS + n, :], kv[0:n, 256:512], reads=[kv], writes=[nv_d])
                kv_hook(P, t, kv, n, locals(), X)
            elif gi == 2:
                actf(P, sz, sz[0:n, :], pp[0:n, :], AF.Silu, rd=[pp])
            else:
                cp(P, "dve", ba, ba[0:n, :], pp[0:n, 0:8], rd=[pp])
        q_hook(P, t, q_tm, n, locals(), X)
        if getattr(X, 'stopat', 99) == 5:
            return

        for jb in range(3):
            pp = pA.get()
            for jj in range(4):
                j = jb * 4 + jj
                for kc in range(8):
                    mm(P, pp, pp[:, jj * 128:jj * 128 + n], w_in[:, kc, 1024 + j * 128:1024 + (j + 1) * 128], hT[:, kc, 0:n], [hT, w_in],
                       start=(kc == 0), stop=(kc == 7))
            cp(P, "act" if jb % 2 == 0 else "dve", ext, ext[:, jb * 4:(jb + 1) * 4, 3:3 + n],
               pp[:, :].rearrange("p (j t) -> p j t", j=4)[:, :, 0:n], rd=[pp])
        conv_hook(P, t, n, locals(), X)
        for i in range(4):
            src = ext[:, :, i:i + n]
            wb = cw[:, :, i:i + 1].to_broadcast([128, 12, n])
            if i == 0:
                tt(P, "dve", cacc, cacc[:, :, 0:n], src, wb, ALU.mult, rd=[ext, cw])
            else:
                tt(P, "pool", ctmp, ctmp[:, :, 0:n], src, wb, ALU.mult, rd=[ext, cw])
                tt(P, "dve", cacc, cacc[:, :, 0:n], cacc[:, :, 0:n], ctmp[:, :, 0:n], ALU.add, rd=[cacc, ctmp])
        actf(P, afm, afm[:, :, 0:n], cacc[:, :, 0:n], AF.Silu, rd=[cacc])
        if getattr(X, 'stopat', 99) == 6:
            return

        if not sample:
            cp(P, "pool", ext, ext[:, :, 0:3], ext[:, :, 128:131], rd=[ext])
        if getattr(X, 'stopat', 99) == 7:
            return
        actf(P, beta, beta[0:n, :], ba[0:n, 0:4], AF.Sigmoid, rd=[ba])
        if getattr(X, 'stopat', 99) == 8:
            return
        ts(P, "dve", nbeta, nbeta[0:n, :], beta[0:n, :], -1.0, None, ALU.mult, rd=[beta])
        tt(P, "dve", g, g[0:n, :], ba[0:n, 4:8], dtb[0:n, :], ALU.add, rd=[ba, dtb])
        actf(P, g, g[0:n, :], g[0:n, :], AF.Exp, rd=[g])
        if getattr(X, 'stopat', 99) == 9:
            return
        actf(P, g, g[0:n, :], g[0:n, :], AF.Ln, rd=[g], bias=1.0)
        if getattr(X, 'stopat', 99) == 10:
            return
        tt(P, "dve", g, g[0:n, :], g[0:n, :], negA[0:n, :], ALU.mult, rd=[g, negA])
        if not getattr(X, 'nogdn', False):
            gdn_tile(P, t, n, locals(), X)
        if sample:
            for h in range(4):
                r = (t * 4 + h) * 128
                P.dma("sp", ns_s[r:r + 128, :], Scur[h][:, :], reads=[Scur[h]], writes=[ns_s])
    a1_end(P, locals(), X)


def sub2(t, j, name=""):
    return Sub(t, lambda idx, j=j: (idx[0], j) + tuple(idx[1:]), name)


class BV:
    def __init__(self, parent, pre, name=""):
        self.parent = parent
        self.pre = pre
        self.name = name

    @property
    def lw(self):
        return self.parent.lw

    @lw.setter
    def lw(self, v):
        self.parent.lw = v

    @property
    def rd(self):
        return self.parent.rd

    @rd.setter
    def rd(self, v):
        self.parent.rd = v

    def __getitem__(self, idx):
        if not isinstance(idx, tuple):
            idx = (idx,)
        return self.parent.t[self.pre(idx)]


class RV(BV):
    def __init__(self, parent, fn, name=""):
        self.parent = parent
        self.fn = fn
        self.name = name

    def __getitem__(self, idx):
        return self.fn(self.parent.t)[idx]


def bv2(parent, j, name=""):
    return BV(parent, lambda idx, j=j: (idx[0], j) + tuple(idx[1:]), name)


def gdn_tile(P, t, n, L, X):
    cst, tri, ident, negus, negut = L["cst"], L["tri"], L["ident"], L["negus"], L["negut"]
    g, gc, ngc, egc, bege, beta, nbeta = L["g"], L["gc"], L["ngc"], L["egc"], L["bege"], L["beta"], L["nbeta"]
    gps, afm, S, sz, gnw, mixg, hsets, cols = L["gps"], L["afm"], L["S"], L["sz"], L["gnw"], L["mixg"], L["hsets"], L["cols"]
    gp = gps.get()
    mm(P, gp, gp[0:n, 0:4], tri[0:n, 0:n], g[0:n, :], [cst, g])
    cp(P, "dve", gc, gc[0:n, :], gp[0:n, 0:4], rd=[gp])
    ts(P, "dve", ngc, ngc[0:n, :], gc[0:n, :], -1.0, None, ALU.mult, rd=[gc])
    actf(P, egc, egc[0:n, :], gc[0:n, :], AF.Exp, rd=[gc])
    tt(P, "dve", bege, bege[0:n, :], beta[0:n, :], egc[0:n, :], ALU.mult, rd=[beta, egc])
    if getattr(X, 'gstop', 99) == 1:
        return
    for h in range(4):
        hs = hsets[h % 2]
        cl = cols[h % 2]
        kh, qh, vb, kbe, kt, qt, qtt = hs["kh"], hs["qh"], hs["vb"], hs["kbe"], hs["kt"], hs["qt"], hs["qtt"]
        Dm, DTm, EG, Nm, NTm, wT, IT, usb, vnew, ktil, tmpg = (hs[k] for k in
                                                                ["Dm", "DTm", "EG", "Nm", "NTm", "wT", "IT", "usb", "vnew", "ktil", "tmpg"])
        pq = gps.get(); tr32(P, pq, pq[0:n, :], afm[:, h, 0:n], ident, [afm, cst])
        pk = gps.get(); tr32(P, pk, pk[0:n, :], afm[:, 4 + h, 0:n], ident, [afm, cst])
        pv = gps.get(); tr32(P, pv, pv[0:n, :], afm[:, 8 + h, 0:n], ident, [afm, cst])
        if getattr(X, 'gstop', 99) == 11:
            return
        actf(P, tmpg, tmpg[0:n, :], pq[0:n, :], AF.Square, rd=[pq], accum=cl["ssq"][0:n, :], wr_extra=[cl["ssq"]])
        actf(P, tmpg, tmpg[0:n, :], pk[0:n, :], AF.Square, rd=[pk], accum=cl["ssk"][0:n, :], wr_extra=[cl["ssk"]])
        if getattr(X, 'gstop', 99) == 12:
            return
        rsqrt_col(P, cl["rq"], cl["rq"][0:n, :], cl["ssq"][0:n, :], 1.0, EPS, [cl["ssq"]])
        rsqrt_col(P, cl["rk"], cl["rk"][0:n, :], cl["ssk"][0:n, :], 1.0, EPS, [cl["ssk"]])
        if getattr(X, 'gstop', 99) == 13:
            return
        ts(P, "dve", qh, qh[0:n, :], pq[0:n, :], cl["rq"][0:n, :], 128.0 ** -0.5, ALU.mult, ALU.mult, rd=[pq, cl["rq"]])
        ts(P, "dve", kh, kh[0:n, :], pk[0:n, :], cl["rk"][0:n, :], None, ALU.mult, rd=[pk, cl["rk"]])
        ts(P, "dve", vb, vb[0:n, :], pv[0:n, :], beta[0:n, h:h + 1], None, ALU.mult, rd=[pv, beta])
        if getattr(X, 'gstop', 99) == 14:
            return
        ts(P, "pool", kbe, kbe[0:n, :], kh[0:n, :], bege[0:n, h:h + 1], None, ALU.mult, rd=[kh, bege])
        if getattr(X, 'gstop', 99) == 2:
            return
        pg = gps.get()
        mm(P, pg, pg[:, 0:n], g[0:n, h:h + 1].to_broadcast([n, 128]), tri[0:n, 0:n], [g, cst])
        cp(P, "dve", cl["glast"], cl["glast"][:, :], pg[:, n - 1:n], rd=[pg])
        actf(P, cl["eglast"], cl["eglast"][:, :], cl["glast"][:, :], AF.Exp, rd=[cl["glast"]])
        actf(P, cl["ekl"], cl["ekl"][0:n, :], gc[0:n, h:h + 1], AF.Exp, rd=[gc, cl["glast"]], scale=-1.0, bias=cl["glast"][0:n, :])
        ts(P, "pool", ktil, ktil[0:n, :], kh[0:n, :], cl["ekl"][0:n, :], None, ALU.mult, rd=[kh, cl["ekl"]])
        if getattr(X, 'gstop', 99) == 3:
            return
        stt(P, "dve", tmpg, tmpg[0:n, 0:n], pg[0:n, 0:n], -1.0, negus[0:n, 0:n], ALU.mult, ALU.add, rd=[pg, cst])
        actf(P, Dm, Dm[0:n, 0:n], tmpg[0:n, 0:n], AF.Exp, rd=[tmpg, gc], bias=gc[0:n, h:h + 1])
        tt(P, "dve", IT, IT[0:n, 0:n], pg[0:n, 0:n], negut[0:n, 0:n], ALU.add, rd=[pg, cst])
        actf(P, DTm, DTm[0:n, 0:n], IT[0:n, 0:n], AF.Exp, rd=[IT, ngc], bias=ngc[0:n, h:h + 1])
        actf(P, EG, EG[:, 0:n], pg[:, 0:n], AF.Exp, rd=[pg])
        if getattr(X, 'gstop', 99) == 4:
            return
        pkt = gps.get(); tr32(P, pkt, pkt[:, 0:n], kh[0:n, :], ident[0:n, 0:n], [kh, cst])
        cp(P, "act", kt, kt[:, 0:n], pkt[:, 0:n], rd=[pkt])
        pqt = gps.get(); tr32(P, pqt, pqt[:, 0:n], qh[0:n, :], ident[0:n, 0:n], [qh, cst])
        cp(P, "dve", qt, qt[:, 0:n], pqt[:, 0:n], rd=[pqt])
        tt(P, "dve", qtt, qtt[:, 0:n], pqt[:, 0:n], EG[:, 0:n], ALU.mult, rd=[pqt, EG])
        if getattr(X, 'gstop', 99) == 5:
            return
        pkk = gps.get(); mm(P, pkk, pkk[0:n, 0:n], kt[:, 0:n], kt[:, 0:n], [kt])
        stt(P, "dve", Nm, Nm[0:n, 0:n], pkk[0:n, 0:n], nbeta[0:n, h:h + 1], Dm[0:n, 0:n], ALU.mult, ALU.mult, rd=[pkk, nbeta, Dm])
        pnt = gps.get(); tr32(P, pnt, pnt[0:n, 0:n], Nm[0:n, 0:n], ident[0:n, 0:n], [Nm, cst])
        cp(P, "act", NTm, NTm[0:n, 0:n], pnt[0:n, 0:n], rd=[pnt])
        XT = hs["XTa"]
        tt(P, "pool", XT, XT[0:n, 0:n], NTm[0:n, 0:n], ident[0:n, 0:n], ALU.add, rd=[NTm, cst])
        if getattr(X, 'gstop', 99) == 6:
            return
        Pc, PTc = Nm, NTm
        nsteps = 0
        while (1 << (nsteps + 1)) < n:
            nsteps += 1
        for s in range(1, nsteps + 1):
            Pn, PTn = (hs["Pa"], hs["PTa"]) if s % 2 == 1 else (hs["Pb"], hs["PTb"])
            p1 = gps.get(); mm(P, p1, p1[0:n, 0:n], PTc[0:n, 0:n], Pc[0:n, 0:n], [PTc, Pc])
            cp(P, "act", Pn, Pn[0:n, 0:n], p1[0:n, 0:n], rd=[p1])
            if s < nsteps:
                p2 = gps.get(); mm(P, p2, p2[0:n, 0:n], Pc[0:n, 0:n], PTc[0:n, 0:n], [PTc, Pc])
                cp(P, "dve", PTn, PTn[0:n, 0:n], p2[0:n, 0:n], rd=[p2])
            p3 = gps.get(); mm(P, p3, p3[0:n, 0:n], Pn[0:n, 0:n], XT[0:n, 0:n], [Pn, XT])
            XTn = hs["XTb"] if XT is hs["XTa"] else hs["XTa"]
            tt(P, "dve", XTn, XTn[0:n, 0:n], XT[0:n, 0:n], p3[0:n, 0:n], ALU.add, rd=[XT, p3])
            XT = XTn
            Pc, PTc = Pn, PTn
        pu = gps.get(); mm(P, pu, pu[0:n, :], XT[0:n, 0:n], vb[0:n, :], [XT, vb])
        if getattr(X, 'gstop', 99) == 7:
            return
        cp(P, "act", usb, usb[0:n, :], pu[0:n, :], rd=[pu])
        pw = gps.get(); mm(P, pw, pw[:, 0:n], kbe[0:n, :], XT[0:n, 0:n], [XT, kbe])
        cp(P, "dve", wT, wT[:, 0:n], pw[:, 0:n], rd=[pw])
        pit = gps.get(); mm(P, pit, pit[0:n, 0:n], kt[:, 0:n], qt[:, 0:n], [kt, qt])
        tt(P, "dve", IT, IT[0:n, 0:n], pit[0:n, 0:n], DTm[0:n, 0:n], ALU.mult, rd=[pit, DTm])
        if getattr(X, 'gstop', 99) == 8:
            return
        Sh = X.S_for(h) if hasattr(X, "S_for") else S[h]
        pws = gps.get(); mm(P, pws, pws[0:n, :], wT[:, 0:n], Sh[:, :], [wT, Sh])
        tt(P, "dve", vnew, vnew[0:n, :], usb[0:n, :], pws[0:n, :], ALU.subtract, rd=[usb, pws])
        po = gps.get()
        mm(P, po, po[0:n, :], qtt[:, 0:n], Sh[:, :], [qtt, Sh], start=True, stop=False)
        mm(P, po, po[0:n, :], IT[0:n, 0:n], vnew[0:n, :], [IT, vnew], start=False, stop=True)
        psu = gps.get(); mm(P, psu, psu[:, :], ktil[0:n, :], vnew[0:n, :], [ktil, vnew])
        stt(P, "dve", Sh, Sh[:, :], Sh[:, :], cl["eglast"][:, 0:1], psu[:, :], ALU.mult, ALU.add, rd=[Sh, cl["eglast"], psu])
        if getattr(X, 'gstop', 99) == 9:
            return
        osb = hs["usb"]
        cp(P, "dve", osb, osb[0:n, :], po[0:n, :], rd=[po])
        actf(P, tmpg, tmpg[0:n, :], osb[0:n, :], AF.Square, rd=[osb], accum=cl["sso"][0:n, :], wr_extra=[cl["sso"]])
        if getattr(X, 'gstop', 99) == 17:
            return
        rsqrt_col(P, cl["rso"], cl["rso"][0:n, :], cl["sso"][0:n, :], 1.0 / 128, EPS, [cl["sso"]])
        if getattr(X, 'gstop', 99) == 18:
            return
        stt(P, "dve", tmpg, tmpg[0:n, :], osb[0:n, :], cl["rso"][0:n, :], gnw[0:n, :], ALU.mult, ALU.mult, rd=[osb, cl["rso"], gnw])
        if getattr(X, 'gstop', 99) == 19:
            return
        tt(P, "pool", mixg, mixg[0:n, h * 128:(h + 1) * 128], tmpg[0:n, :], sz[0:n, h * 128:(h + 1) * 128], ALU.mult, rd=[tmpg, sz])
        if getattr(X, 'gstop', 99) == 15:
            return
    if getattr(X, 'gstop', 99) == 16:
        return
    r0 = L["row0"] + t * L["TS"]
    P.dma("sp", X.T["mix_d"][r0:r0 + n, 512:1024], mixg[0:n, :], reads=[mixg], writes=[X.mix_d])


def kv_hook(P, t, kv, n, L, X):
    if L["sample"]:
        return
    identb = L["identb"]
    kd = X.kdup.get()
    Vs = X.Vbs[t]
    cp(P, "pool", Vs, Vs[:, :, 0:64], kv[:, 256:512].rearrange("p (h d) -> p h d", h=4), rd=[kv])
    P.op("pool", lambda e: e.memset(Vs[:, :, 64:65], 1.0), writes=[Vs])
    src = kv[:, 0:256].rearrange("p (h d) -> p h d", h=4).unsqueeze(2).to_broadcast([128, 4, 2, 64])
    cp(P, "pool", kd, kd[:, :, :, :], src, rd=[kv])
    pk = X.pKT
    for h in range(4):
        tr(P, pk, pk[:, h, :], kd[:, h, :, :].rearrange("p r d -> p (r d)"), identb[:, :], [kd, identb], inc=(h == 3))
    Ks = X.KTs[t]
    cp(P, "act", Ks, Ks[:, :, :], pk[:, :, :], rd=[pk])


def stcT_src(stc, t, j):
    return stc[:, j * 128:(j + 1) * 128]


def q_hook(P, t, q_tm, n, L, X):
    if L["sample"]:
        P.dma("sp", X.T["q_s"][t * 4:t * 4 + 4, :], q_tm[0:4, :], reads=[q_tm], writes=[X.q_sd])
        return
    identb = L["jrevb"]
    qb = X.qb.get()
    cp(P, "pool", qb, qb[:, :], q_tm[:, :], rd=[q_tm])
    pq = X.pQT
    for p in range(4):
        tr(P, pq, pq[:, p, :], qb[:, p * 128:(p + 1) * 128], identb[:, :], [qb, identb], inc=(p == 3))
    Qs = X.QTs[t]
    cp(P, "dve", Qs, Qs[:, :, :], pq[:, :, :], rd=[pq])


def conv_hook(P, t, n, L, X):
    if (not L["sample"]) and t != NT - 1:
        return
    hT, w_in, pA, pre_tm = L["hT"], L["w_in"], L["pA"], L["pre_tm"]
    for gi in range(3):
        pp = pA.get()
        for kc in range(8):
            mm(P, pp, pp[0:n, :], hT[:, kc, 0:n], w_in[:, kc, 1024 + gi * 512:1024 + (gi + 1) * 512], [hT, w_in], start=(kc == 0), stop=(kc == 7))
        cp(P, "act", pre_tm, pre_tm[0:n, gi * 4:(gi + 1) * 4, :], pp[0:n, :].rearrange("p (j t) -> p j t", j=4), rd=[pp])
    if L["sample"]:
        ncs = X.ncs
        P.dma("sp", ncs[3 * t:3 * t + 3, :].rearrange("r (j t) -> r j t", j=12), pre_tm[1:4, :, :], reads=[pre_tm], writes=[ncs])
        return
    ncp = Buf(X.T["nc_p"])
    P.dma("sp", ncp[:, :].rearrange("r (j t) -> r j t", j=12), pre_tm[125:128, :, :], reads=[pre_tm], writes=[ncp])
    X.outs.append(ncp)


def a1_end(P, L, X):
    if L["sample"] or getattr(X, 'noend', False):
        return
    nsp = Buf(X.T["ns_p"])
    v = getattr(X, 'endvar', 0)
    for h in range(4 if v != 1 else 1):
        src = L["S"][h] if v != 2 else L["tmp"]
        P.dma("sp", nsp[h * 128:(h + 1) * 128, :], src[:, 0:128], reads=[src], writes=[nsp])
    X.outs += [nsp, L["nk_d"], L["nv_d"]]


def alloc_attn_persist(nc, es, X):
    sb, ps = allocators(nc, es, "pers")
    QT = sb("QT", [128, 4, SEQ], BF16)
    KT = sb("KT", [128, 4, SEQ], BF16)
    Vb = sb("Vb", [128, NT, 4, 65], BF16)
    X.QT, X.KT, X.Vb = QT, KT, Vb
    X.QTs = [Sub(QT.t, lambda idx, t=t: (idx[0], idx[1], slice(t * 128, (t + 1) * 128)) if True else None, "QT%d" % t) for t in range(NT)]
    X.KTs = [Sub(KT.t, lambda idx, t=t: (idx[0], idx[1], slice(t * 128, (t + 1) * 128)), "KT%d" % t) for t in range(NT)]
    X.Vbs = [sub2(Vb.t, t, "Vb%d" % t) for t in range(NT)]


def alloc_a1_extra(nc, es, X, pre="a1x"):
    sb, ps = allocators(nc, es, pre)
    X.kdup = Rot([sb("kdup%d" % i, [128, 4, 2, 64], BF16) for i in range(2)])
    X.qb = Rot([sb("qb%d" % i, [128, 512], BF16) for i in range(2)])
    pb = Buf(es.enter_context(nc.psum_tensor(pre + "_pKQ", [128, 8, 128], BF16)), "pKQ")
    X.pKT = BV(pb, lambda idx: (idx[0], (slice(0, 4) if isinstance(idx[1], slice) else idx[1])) + tuple(idx[2:]), "pKT")
    X.pQT = BV(pb, lambda idx: (idx[0], (slice(4, 8) if isinstance(idx[1], slice) else idx[1] + 4)) + tuple(idx[2:]), "pQT")


def build_rel_table(P, nc, es, T, X, relb_rows, oh_name, ncols, rd_name, pre):
    sb, ps = allocators(nc, es, pre)
    relb = sb("relb", [33, 8])
    P.op("dve", lambda e: e.memset(relb[:], NEG), writes=[relb])
    P.dma("sp", relb[0:32, :], T["rel_bias"], writes=[relb])
    oh = sb("oh", [33, ncols])
    P.dma("sp", oh[:], T[oh_name], writes=[oh])
    rsb = sb("rsb", [relb_rows, ncols])
    pp = ps("pp", [relb_rows, 512])
    lhs = X.rel_lhs(relb) if hasattr(X, "rel_lhs") else relb[:, :]
    for j in range(0, ncols, 512):
        w = min(512, ncols - j)
        mm(P, pp, pp[:, 0:w], lhs, oh[:, j:j + w], [relb, oh])
        actf(P, rsb, rsb[:, j:j + w], pp[:, 0:w], AF.Exp, rd=[pp], eng="act")
    rdb = Buf(T[rd_name], rd_name)
    P.dma("sp", rdb[:, :], rsb[:, :], reads=[rsb], writes=[rdb])
    return rdb


def phase_a2(P, nc, es, T, X):
    sb, ps = allocators(nc, es, "a2")
    scale = 64.0 ** -0.5
    nch = getattr(X, "ntile", NT)
    cst = sb("cst", [128, NCST])
    P.dma("sp", cst[:], T["cst"], writes=[cst])
    jrevb = sb("jrevb", [128, 128], BF16)
    cp(P, "dve", jrevb, jrevb[:], cst[:, C_JREV:C_JREV + 128], rd=[cst])
    rdb = X.rdb
    tabs = Rot([sb("tab%d" % i, [128, 4096]) for i in range(2)])
    S_all = Rot([sb("S_all%d" % i, [128, 4096]) for i in range(2)])
    E = sb("E", [128, 4096])
    Pb = Rot([sb("Pb%d" % i, [128, 4096], BF16) for i in range(2)])
    PTs = Rot([sb("PTs%d" % i, [128, 4, 128], BF16) for i in range(3)])
    gt = sb("gt", [128, 16]); mx8 = sb("mx8", [128, 8]); mb = sb("mb", [128, 16]); bcol = sb("bcol", [128, 16])
    nm = sb("nm", [128, 1]); rm = sb("rm", [128, 1]); rcp = sb("rcp", [128, 1])
    ao = Rot([sb("ao%d" % i, [128, 64]) for i in range(2)])
    pS = Rot([ps("pS%d" % i, [128, 512]) for i in range(3)])
    pT = Rot([ps("pT%d" % i, [128, 8, 128], BF16) for i in range(2)])
    pO = Rot([ps("pO%d" % i, [128, 512]) for i in range(2)])
    mix_d = X.mix_d
    for hq in range(8):
        hk = hq // 2
        r0 = (hq % 2) * 64
        tab = tabs.get()
        src = bass.AP(tensor=T["rd"].tensor, offset=hq * RLEN, ap=[[1, 128], [1, 4096]])
        P.dma("sp", tab[:, :], src, reads=[rdb], writes=[tab])
        for c in range(nch):
            nk = 128 * (c + 1)
            ob = c // 2
            q0 = 128 * c
            Sa = S_all.get()
            for pi, k0 in enumerate(range(0, nk, 512)):
                w = min(512, nk - k0)
                pp = pS.get()
                rdq = [X.QTs[c]] + [X.KTs[t] for t in range(k0 // 128, (k0 + w) // 128)]
                mm(P, pp, pp[:, 0:w], X.QT[r0:r0 + 64, hk, q0:q0 + 128], X.KT[r0:r0 + 64, hk, k0:k0 + w], rdq)
                cp(P, "dve" if pi % 2 == 0 else "act", Sa, Sa[:, k0:k0 + w], pp[:, 0:w], rd=[pp])
            P.op("pool", lambda e: e.memset(gt[:], -1e30), writes=[gt])
            if ob > 0:
                P.op("dve", lambda e, Sa=Sa, ob=ob: e.tensor_reduce(gt[:, 0:ob], Sa[:, 0:256 * ob].rearrange("p (n k) -> p n k", k=256), AX.X, ALU.add),
                     reads=[Sa], writes=[gt])
            P.op("dve", lambda e: e.max(mx8[:], gt[:]), reads=[gt], writes=[mx8])
            ts(P, "dve", mb, mb[:], gt[:], mx8[:, 2:3], 1.0, ALU.is_ge, ALU.subtract, rd=[gt, mx8])
            P.op("dve", lambda e, Sa=Sa, nk=nk: e.reduce_max(rm[:], Sa[:, 0:nk], AX.X), reads=[Sa], writes=[rm])
            ts(P, "dve", nm, nm[:], rm[:], -scale, None, ALU.mult, rd=[rm])
            ts(P, "dve", bcol, bcol[:], mb[:], -NEG, nm[:, 0:1], ALU.mult, ALU.add, rd=[mb, nm])
            for n in range(ob):
                actf(P, E, E[:, 256 * n:256 * (n + 1)], Sa[:, 256 * n:256 * (n + 1)], AF.Exp, rd=[Sa, bcol], scale=scale, bias=bcol[:, n:n + 1])
            actf(P, E, E[:, 256 * ob:nk], Sa[:, 256 * ob:nk], AF.Exp, rd=[Sa, nm], scale=scale, bias=nm[:, 0:1])
            Pc = Pb.get()
            tt(P, "dve" if c % 2 == 0 else "pool", Pc, Pc[:, 0:nk], E[:, 0:nk], tab[:, 3968 - q0:4096], ALU.mult, rd=[E, tab])
            po = pO.get()
            for g0 in range(0, c + 1, 4):
                g1 = min(g0 + 4, c + 1)
                pt = pT.get()
                for kt in range(g0, g1):
                    tr(P, pt, pt[:, kt - g0, :], Pc[:, kt * 128:(kt + 1) * 128], jrevb[:, :], [Pc, jrevb], inc=(kt == g1 - 1))
                pts = PTs.get()
                cp(P, "act" if (g0 // 4) % 2 == 0 else "dve", pts, pts[:, 0:g1 - g0, :], pt[:, 0:g1 - g0, :], rd=[pt])
                for kt in range(g0, g1):
                    mm(P, po, po[:, 0:65], pts[:, kt - g0, :], X.Vb[:, kt, hk, :], [pts, X.Vbs[kt]], start=(kt == 0), stop=(kt == c), inc=(kt == g1 - 1))
            P.op("dve", lambda e, po=po: e.reciprocal(rcp[:], po[:, 64:65]), reads=[po], writes=[rcp])
            a = ao.get()
            ts(P, "dve", a, a[:, :], po[:, 0:64], rcp[:, 0:1], None, ALU.mult, rd=[po, rcp])
            P.dma("sp", T["mix_d"][q0:q0 + 128, hq * 64:(hq + 1) * 64], a[:, :], reads=[a], writes=[mix_d])


def phase_a3(P, nc, es, T, X, tiles):
    sb, ps = allocators(nc, es, "a3")
    cst = sb("cst", [128, NCST])
    P.dma("sp", cst[:], T["cst"], writes=[cst])
    identb = sb("identb", [128, 128], BF16)
    cp(P, "dve", identb, identb[:], cst[:, C_IDENT:C_IDENT + 128], rd=[cst])
    w_out = sb("w_out", [128, 8, D], BF16)
    wov = T["w_out"].rearrange("(kc p) n -> p kc n", p=128)
    for kc in range(8):
        P.dma("pool", w_out[:, kc, :], wov[:, kc, :], writes=[w_out])
    gta_p = sb("gta_p", [128, D]); gta_s = sb("gta_s", [128, D])
    m_d = X.m_d
    P.dma("sp", gta_p[:], T["m_d"][0, 2 * D:3 * D].partition_broadcast(128), reads=[m_d], writes=[gta_p])
    for s in range(SB):
        P.dma("sp", gta_s[4 * s:4 * s + 4, :], T["m_d"][1 + s, 2 * D:3 * D].partition_broadcast(4), reads=[m_d], writes=[gta_s])
    mixs = Rot([sb("mix%d" % i, [128, D]) for i in range(2)])
    xs = Rot([sb("x%d" % i, [128, D]) for i in range(2)])
    mixb = sb("mixb", [128, D], BF16)
    mixT = sb("mixT", [128, 8, 128], BF16)
    x1 = Rot([sb("x1_%d" % i, [128, D]) for i in range(2)])
    pTr = ps("pTr", [128, 8, 128], BF16)
    pY = Rot([ps("pY%d" % i, [128, 512]) for i in range(2)])
    for (row, n, xsrc, gta) in tiles(gta_p, gta_s):
        mix = mixs.get(); x = xs.get()
        P.dma("sp", mix[0:n, :], T["mix_d"][row:row + n, :], reads=[X.mix_d], writes=[mix])
        P.dma("sp", x[0:n, :], xsrc, writes=[x])
        cp(P, "pool", mixb, mixb[0:n, :], mix[0:n, :], rd=[mix])
        for kc in range(8):
            tr(P, pTr, pTr[:, kc, 0:n], mixb[0:n, kc * 128:(kc + 1) * 128], identb[0:n, 0:n], [mixb, identb], inc=(kc == 7))
        cp(P, "act", mixT, mixT[:, :, 0:n], pTr[:, :, 0:n], rd=[pTr])
        xo = x1.get()
        for g in range(2):
            pp = pY.get()
            for kc in range(8):
                mm(P, pp, pp[0:n, :], mixT[:, kc, 0:n], w_out[:, kc, g * 512:(g + 1) * 512], [mixT, w_out], start=(kc == 0), stop=(kc == 7))
            tt(P, "dve", xo, xo[0:n, g * 512:(g + 1) * 512], pp[0:n, :], gta[0:n, g * 512:(g + 1) * 512], ALU.mult, rd=[pp, gta])
        tt(P, "pool", xo, xo[0:n, :], xo[0:n, :], x[0:n, :], ALU.add, rd=[xo, x])
        P.dma("sp", T["x1_d"][row:row + n, :], xo[0:n, :], reads=[xo], writes=[X.x1_d])


def phase_b(P, nc, es, T, X, passes, ne=NE):
    sb, ps = allocators(nc, es, "b")
    MAXS = max(len(p) for p in passes)
    cst = sb("cst", [128, NCST])
    P.dma("sp", cst[:], T["cst"], writes=[cst])
    ident = cst[:, C_IDENT:C_IDENT + 128]
    m_d = X.m_d
    wmod = sb("wmod", [128, D]); shf = sb("shf", [128, D]); gtf = sb("gtf", [128, D]); nwf = sb("nwf", [128, D])
    h2 = sb("h2", [128, D])
    tmp = h2
    P.dma("sp", nwf[:], T["nw"][2, :].partition_broadcast(128), writes=[nwf])

    def load_mods(sample):
        P.dma("sp", tmp[:], T["nw"][1, :].partition_broadcast(128), writes=[tmp])
        if not sample:
            P.dma("sp", shf[:], T["m_d"][0, 3 * D:4 * D].partition_broadcast(128), reads=[m_d], writes=[shf])
            P.dma("sp", wmod[:], T["m_d"][0, 4 * D:5 * D].partition_broadcast(128), reads=[m_d], writes=[wmod])
            P.dma("sp", gtf[:], T["m_d"][0, 5 * D:6 * D].partition_broadcast(128), reads=[m_d], writes=[gtf])
        else:
            for s in range(SB):
                P.dma("sp", shf[4 * s:4 * s + 4, :], T["m_d"][1 + s, 3 * D:4 * D].partition_broadcast(4), reads=[m_d], writes=[shf])
                P.dma("sp", wmod[4 * s:4 * s + 4, :], T["m_d"][1 + s, 4 * D:5 * D].partition_broadcast(4), reads=[m_d], writes=[wmod])
                P.dma("sp", gtf[4 * s:4 * s + 4, :], T["m_d"][1 + s, 5 * D:6 * D].partition_broadcast(4), reads=[m_d], writes=[gtf])
        nr = ST if sample else 128
        stt(P, "dve", wmod, wmod[0:nr, :], wmod[0:nr, :], 1.0, tmp[0:nr, :], ALU.add, ALU.mult, rd=[wmod, tmp])

    wr = sb("wr", [128, 8, NE])
    P.dma("sp", wr[:], T["w_router"].rearrange("(kc p) e -> p kc e", p=128), writes=[wr])
    brb = sb("brb", [128, NE])
    P.dma("sp", brb[:], T["b_router"][0, :].partition_broadcast(128), writes=[brb])
    bdn = sb("bdn", [NE, D])
    P.dma("sp", bdn[:], T["b_down"], writes=[bdn])
    xt = Rot([sb("x%d" % i, [128, D]) for i in range(1)])
    bupT = sb("bupT", [128, 16, NE])
    pB = Rot([ps("pB%d" % i, [128, 512]) for i in range(2)])
    for hf in range(2):
        bup = xt.get()
        P.dma("sp", bup[0:NE, :], T["b_up"][:, hf * D:(hf + 1) * D], writes=[bup])
        for c8 in range(8):
            c = hf * 8 + c8
            pp = pB.get()
            tr32(P, pp, pp[:, 0:NE], bup[0:NE, c8 * 128:(c8 + 1) * 128], ident[0:NE, 0:NE], [bup, cst])
            cp(P, "dve", bupT, bupT[:, c, :], pp[:, 0:NE], rd=[pp])
    wst = Rot([sb("wst%d" % i, [128, D]) for i in range(3)])
    wub_d = [Buf(T["wupb"][e], "wupb%d" % e) for e in range(NE)]
    wdb_d = [Buf(T["wdnb"][e], "wdnb%d" % e) for e in range(NE)]
    H2T = sb("H2T", [128, 8, MAXS * 128], BF16)
    acc = [sb("acc%d" % i, [128, D]) for i in range(MAXS)]
    G = sb("G", [128, MAXS, NE])
    wup = Rot([sb("wup%d" % i, [128, 8, 2 * D], BF16) for i in range(2)])
    wdn = Rot([sb("wdn%d" % i, [128, 8, D], BF16) for i in range(2)])
    actT = sb("actT", [128, 8, 512], BF16)
    actT2 = sb("actT2", [128, 8, 512], BF16)
    gsb = Rot([sb("gsb%d" % i, [128, 512]) for i in range(2)])
    sgs = Rot([sb("sgs%d" % i, [128, 512]) for i in range(2)])
    lsb = Rot([sb("lsb%d" % i, [128, 512]) for i in range(2)])
    h2Tf = RV(wst.b[0], lambda t: t[:, :].rearrange("p (k t) -> p k t", k=8), "h2Tf")
    ss = sb("ss", [128, 8]); lg = sb("lg", [128, NE]); mx8 = sb("mx8", [128, 8]); msk = sb("msk", [128, NE]); ex = sb("ex", [128, NE])
    gT = sb("gT", [NE, 128])
    pG = Rot([ps("pG%d" % i, [128, 512]) for i in range(2)])
    pL = Rot([ps("pL%d" % i, [128, 512]) for i in range(2)])
    pY = Rot([ps("pY%d" % i, [128, 512]) for i in range(2)])
    x1_d = X.x1_d
    wupv = T["w_up"].rearrange("e (kc p) n -> e p kc n", p=128)
    wdnv = T["w_down"].rearrange("e (kc p) n -> e p kc n", p=128)
    cur_mod = [None]

    for tiles in passes:
        for si, (row, n, is_s, out_ap) in enumerate(tiles):
            if cur_mod[0] != is_s:
                load_mods(is_s)
                cur_mod[0] = is_s
            x = xt.get()
            P.dma("sp", x[0:n, :], T["x1_d"][row:row + n, :], reads=[x1_d], writes=[x])
            actf(P, h2, h2[0:n, :], x[0:n, :], AF.Square, rd=[x], accum=ss[0:n, 0:1], wr_extra=[ss])
            rsqrt_col(P, ss, ss[0:n, 2:3], ss[0:n, 0:1], 1.0 / D, EPS, [ss])
            stt(P, "dve", h2, h2[0:n, :], x[0:n, :], ss[0:n, 2:3], wmod[0:n, :], ALU.mult, ALU.mult, rd=[x, ss, wmod])
            tt(P, "pool", h2, h2[0:n, :], h2[0:n, :], shf[0:n, :], ALU.add, rd=[h2, shf])
            for half in range(2):
                pp = pB.get()
                for k4 in range(4):
                    kc = half * 4 + k4
                    tr32(P, pp, pp[:, k4 * 128:k4 * 128 + n], h2[0:n, kc * 128:(kc + 1) * 128], ident[0:n, 0:n], [h2, cst])
                cp(P, "dve", h2Tf, h2Tf[:, half * 4:(half + 1) * 4, 0:n], pp[:, :].rearrange("p (k t) -> p k t", k=4)[:, :, 0:n], rd=[pp])
            cp(P, "pool", H2T, H2T[:, :, si * 128:si * 128 + n], h2Tf[:, :, 0:n], rd=[h2Tf])
            pp = pB.get()
            for kc in range(8):
                mm(P, pp, pp[0:n, 0:NE], h2Tf[:, kc, 0:n], wr[:, kc, :], [h2Tf, wr], start=(kc == 0), stop=(kc == 7))
            tt(P, "dve", lg, lg[0:n, :], pp[0:n, 0:NE], brb[0:n, :], ALU.add, rd=[pp, brb])
            P.op("dve", lambda e, n=n: e.max(mx8[0:n, :], lg[0:n, :]), reads=[lg], writes=[mx8])
            ts(P, "dve", msk, msk[0:n, :], lg[0:n, :], mx8[0:n, 3:4], None, ALU.is_ge, rd=[lg, mx8])
            ts(P, "dve", mx8, mx8[0:n, 7:8], mx8[0:n, 0:1], -1.0, None, ALU.mult, rd=[mx8])
            actf(P, ex, ex[0:n, :], lg[0:n, :], AF.Exp, rd=[lg, mx8], bias=mx8[0:n, 7:8])
            tt(P, "dve", ex, ex[0:n, :], ex[0:n, :], msk[0:n, :], ALU.mult, rd=[ex, msk])
            P.op("dve", lambda e, n=n: e.reduce_sum(ss[0:n, 4:5], ex[0:n, :], AX.X), reads=[ex], writes=[ss])
            P.op("dve", lambda e, n=n: e.reciprocal(ss[0:n, 5:6], ss[0:n, 4:5]), reads=[ss], writes=[ss])
            ts(P, "dve", G, G[0:n, si, :], ex[0:n, :], ss[0:n, 5:6], None, ALU.mult, rd=[ex, ss])
            pp = pB.get()
            tr32(P, pp, pp[0:NE, 0:n], G[0:n, si, :], ident[0:n, 0:n], [G, cst])
            cp(P, "dve", gT, gT[:, 0:n], pp[0:NE, 0:n], rd=[pp])
            for half in range(2):
                pp = pB.get()
                mm(P, pp, pp[0:n, :], gT[:, 0:n], bdn[:, half * 512:(half + 1) * 512], [gT, bdn])
                cp(P, "act" if half == 0 else "dve", acc[si], acc[si][0:n, half * 512:(half + 1) * 512], pp[0:n, :], rd=[pp])
        groups = []
        si = 0
        while si < len(tiles):
            g = list(range(si, min(si + 4, len(tiles))))
            groups.append(g)
            si += 4
        def load_w(e_, first_pass):
            wu = wup.get(); wd = wdn.get()
            if getattr(X, "noload", False) and (e_ > 1 or not first_pass):
                return wu, wd
            if first_pass:
                k = 0
                for kc in range(8):
                    for hf in range(2):
                        st = wst.get()
                        P.dma("sp", st[:, :], T["w_up"][e_, kc * 128:(kc + 1) * 128, hf * D:(hf + 1) * D], writes=[st])
                        cp(P, "act", wu, wu[:, kc, hf * D:(hf + 1) * D], st[:, :], rd=[st])
                for kc in range(8):
                    st = wst.get()
                    P.dma("sp", st[:, :], T["w_down"][e_, kc * 128:(kc + 1) * 128, :], writes=[st])
                    cp(P, "act", wd, wd[:, kc, :], st[:, :], rd=[st])
                if len(passes) > 1:
                    P.dma("sp", T["wupb"][e_], wu[:, :, :].rearrange("p k n -> p (k n)"), reads=[wu], writes=[wub_d[e_]])
                    P.dma("sp", T["wdnb"][e_], wd[:, :, :].rearrange("p k n -> p (k n)"), reads=[wd], writes=[wdb_d[e_]])
            else:
                P.dma("sp", wu[:, :, :].rearrange("p k n -> p (k n)"), T["wupb"][e_], reads=[wub_d[e_]], writes=[wu])
                P.dma("sp", wd[:, :, :].rearrange("p k n -> p (k n)"), T["wdnb"][e_], reads=[wdb_d[e_]], writes=[wd])
            return wu, wd

        first_pass = (tiles is passes[0])
        wts = {0: load_w(0, first_pass)}
        if ne > 1:
            wts[1] = load_w(1, first_pass)
        units = [(e_, gi) for e_ in range(ne) for gi in range(len(groups))]

        def up_unit(u, aT):
            e_, gi = units[u]
            wu, wd = wts[e_]
            g = groups[gi]
            c0 = g[0] * 128
            ncol = (g[-1] - g[0]) * 128 + tiles[g[-1]][1]
            for fc in range(8):
                pg = pG.get(); pl = pL.get()
                for kc in range(8):
                    mm(P, pg, pg[:, 0:ncol], wu[:, kc, fc * 128:(fc + 1) * 128], H2T[:, kc, c0:c0 + ncol], [wu, H2T], start=(kc == 0), stop=(kc == 7))
                for kc in range(8):
                    mm(P, pl, pl[:, 0:ncol], wu[:, kc, D + fc * 128:D + (fc + 1) * 128], H2T[:, kc, c0:c0 + ncol], [wu, H2T], start=(kc == 0), stop=(kc == 7))
                gs = gsb.get(); sg = sgs.get(); ls = lsb.get()
                actf(P, gs, gs[:, 0:ncol], pg[:, 0:ncol], AF.Identity, rd=[pg, bupT], bias=bupT[:, fc, e_:e_ + 1])
                actf(P, ls, ls[:, 0:ncol], pl[:, 0:ncol], AF.Identity, rd=[pl, bupT], bias=bupT[:, 8 + fc, e_:e_ + 1])
                ts(P, "pool", gs, gs[:, 0:ncol], gs[:, 0:ncol], 7.0, None, ALU.min, rd=[gs])
                ts(P, "pool", ls, ls[:, 0:ncol], ls[:, 0:ncol], 7.0, -7.0, ALU.min, ALU.max, rd=[ls])
                actf(P, sg, sg[:, 0:ncol], gs[:, 0:ncol], AF.Sigmoid, rd=[gs], scale=1.702)
                tt(P, "dve", gs, gs[:, 0:ncol], gs[:, 0:ncol], sg[:, 0:ncol], ALU.mult, rd=[gs, sg])
                stt(P, "dve", aT, aT[:, fc, 0:ncol], ls[:, 0:ncol], 1.0, gs[:, 0:ncol], ALU.add, ALU.mult, rd=[gs, ls])

        def down_unit(u, aT):
            e_, gi = units[u]
            wu, wd = wts[e_]
            g = groups[gi]
            for si in g:
                n = tiles[si][1]
                o0 = (si - g[0]) * 128
                for half in range(2):
                    py = pY.get()
                    for fc in range(8):
                        mm(P, py, py[0:n, :], aT[:, fc, o0:o0 + n], wd[:, fc, half * 512:(half + 1) * 512], [aT, wd], start=(fc == 0), stop=(fc == 7))
                    a = acc[si]
                    stt(P, "dve", a, a[0:n, half * 512:(half + 1) * 512], py[0:n, :], G[0:n, si, e_:e_ + 1], a[0:n, half * 512:(half + 1) * 512],
                        ALU.mult, ALU.add, rd=[py, G, a])
            if gi == len(groups) - 1 and e_ + 2 < ne:
                wts[e_ + 2] = load_w(e_ + 2, first_pass)

        aTs = [actT, actT2]
        up_unit(0, aTs[0])
        for u in range(len(units)):
            if u + 1 < len(units):
                up_unit(u + 1, aTs[(u + 1) % 2])
            down_unit(u, aTs[u % 2])
        for si, (row, n, is_s, out_ap) in enumerate(tiles):
            if cur_mod[0] != is_s:
                load_mods(is_s)
                cur_mod[0] = is_s
            x = xt.get()
            P.dma("sp", x[0:n, :], T["x1_d"][row:row + n, :], reads=[x1_d], writes=[x])
            a = acc[si]
            tt(P, "pool", a, a[0:n, :], a[0:n, :], gtf[0:n, :], ALU.mult, rd=[a, gtf])
            tt(P, "dve", a, a[0:n, :], a[0:n, :], x[0:n, :], ALU.add, rd=[a, x])
            actf(P, h2, h2[0:n, :], a[0:n, :], AF.Square, rd=[a], accum=ss[0:n, 0:1], wr_extra=[ss])
            rsqrt_col(P, ss, ss[0:n, 2:3], ss[0:n, 0:1], 1.0 / D, EPS, [ss])
            stt(P, "dve", h2, h2[0:n, :], a[0:n, :], ss[0:n, 2:3], nwf[0:n, :], ALU.mult, ALU.mult, rd=[a, ss, nwf])
            ob = Buf(out_ap)
            P.dma("sp", out_ap, h2[0:n, :], reads=[h2], writes=[ob])
            X.outs.append(ob)


NKS = 8192 + 128


def make_sample_onehots():
    out = np.zeros((4, 33, NKS), np.float32)
    for t in range(4):
        d = np.full(NKS, -1, np.int64)
        d[:8192] = 8192 + t - np.arange(8192)
        for t2 in range(4):
            d[8192 + t2] = t - t2
        out[t] = make_bucket_onehot(d)
    return out.reshape(4 * 33, NKS)


def phase_a2s(P, nc, es, T, X):
    sb, ps = allocators(nc, es, "a2s")
    scale = 64.0 ** -0.5
    nseq = getattr(X, "nseq", SB)
    cst = sb("cst", [128, NCST])
    P.dma("sp", cst[:], T["cst"], writes=[cst])
    identb = sb("identb", [128, 128], BF16)
    cp(P, "dve", identb, identb[:], cst[:, C_IDENT:C_IDENT + 128], rd=[cst])
    relb = sb("relb", [33, 8])
    P.op("dve", lambda e: e.memset(relb[:], NEG), writes=[relb])
    P.dma("sp", relb[0:32, :], T["rel_bias"], writes=[relb])
    lhs_t = sb("lhs_t", [33, 4, 32])
    P.op("dve", lambda e: e.memset(lhs_t[:], 0.0), writes=[lhs_t])
    for t in range(4):
        cp(P, "dve", lhs_t, lhs_t[:, t, t * 8:(t + 1) * 8], relb[:, :], rd=[relb])
    tab = sb("tab", [32, NKS])
    ohs = Rot([sb("ohs%d" % i, [33, 4, 512]) for i in range(2)])
    pS = Rot([ps("pS%d" % i, [128, 512]) for i in range(2)])
    ohv = T["ohs"].rearrange("(t b) n -> b t n", t=4)
    for j in range(0, NKS, 512):
        w = min(512, NKS - j)
        o = ohs.get()
        P.dma("sp", o[:, :, 0:w], ohv[:, :, j:j + w], writes=[o])
        pp = pS.get()
        for t in range(4):
            mm(P, pp, pp[0:32, 0:w], lhs_t[:, t, :], o[:, t, 0:w], [lhs_t, o], start=(t == 0), stop=(t == 3))
        actf(P, tab, tab[:, j:j + w], pp[0:32, 0:w], AF.Exp, rd=[pp])
    pt = sb("pt", [128, SB * 64], I32)
    P.dma("sp", pt[:, :], T["page_table"].rearrange("s j -> (s j)").partition_broadcast(128), writes=[pt])
    ptf = sb("ptf", [128, SB * 64])
    cp(P, "dve", ptf, ptf[:, :], pt[:, :], rd=[pt])
    pcol = sb("pcol", [128, 1])
    P.dma("sp", pcol[:, :], T["pcol"], writes=[pcol])
    ts(P, "dve", ptf, ptf[:, :], ptf[:, :], 128.0, pcol[:, 0:1], ALU.mult, ALU.add, rd=[ptf, pcol])
    pidx = sb("pidx", [128, SB * 64], I32)
    cp(P, "dve", pidx, pidx[:, :], ptf[:, :], rd=[ptf])
    sel = sb("sel", [32, 4])
    P.dma("sp", sel[:, :], T["sel01"], writes=[sel])
    qf = sb("qf", [4, 512]); qb = sb("qb", [4, 512], BF16)
    lq = sb("lq", [128, 4, 32], BF16)
    P.op("dve", lambda e: e.memset(lq[:], 0.0), writes=[lq])
    kpg = Rot([sb("kpg%d" % i, [128, 256], BF16) for i in range(4)])
    vall = sb("vall", [128, 65, 256], BF16)
    vslots = [bv2(vall, j, "v%d" % j) for j in range(65)]
    vslots = [Sub(vall.t, (lambda idx, j=j: (idx[0], j) + tuple(idx[1:])), "v%d" % j) for j in range(65)]
    ktd = Rot([sb("ktd%d" % i, [128, 4, 128], BF16) for i in range(3)])
    knew = sb("knew", [128, 256], BF16)
    kdups = Rot([sb("kdd%d" % i, [128, 4, 2, 64], BF16) for i in range(3)])
    kvn = sb("kvn", [4, 512])
    Sa = sb("Sa", [32, NKS]); Ee = sb("Ee", [32, NKS]); Pb = sb("Pb", [32, NKS], BF16)
    PTs = Rot([sb("PTs%d" % i, [128, 16, 32], BF16) for i in range(2)])
    gt = sb("gt", [32, 32]); mx8 = sb("mx8", [32, 8]); m01 = sb("m01", [32, 32]); rm = sb("rm", [32, 1]); nm = sb("nm", [32, 1])
    rs = sb("rs", [32, 1]); rcp = sb("rcp", [32, 1]); osel = sb("osel", [32, 4, 64]); ao = sb("ao", [32, 64])
    pT = Rot([ps("pT%d" % i, [128, 8, 128], BF16) for i in range(2)])
    pP = Rot([ps("pP%d" % i, [128, 16, 32], BF16) for i in range(2)])
    pO = ps("pO", [128, 512])
    mix_d = X.mix_d
    nk_sd = Buf(T["nk_s"]); nv_sd = Buf(T["nv_s"])
    ck = T["cache_k"]; cv = T["cache_v"]
    P.op("pool", lambda e: e.memset(knew[:], 0.0), writes=[knew])

    for s in range(nseq):
        P.dma("sp", qf[:, :], T["q_s"][4 * s:4 * s + 4, :], reads=[X.q_sd], writes=[qf])
        cp(P, "dve", qb, qb[:, :], qf[:, :], rd=[qf])
        pq = pT.get()
        for hk in range(4):
            tr(P, pq, pq[:, hk, 0:4], qb[:, hk * 128:(hk + 1) * 128], identb[0:4, 0:4], [qb, identb])
        lqv = lq[:, :, :].rearrange("p h (t q) -> p h t q", q=8)
        for hk in range(4):
            cp(P, "dve", lq, lqv[0:64, hk, :, 2 * hk], pq[0:64, hk, 0:4], rd=[pq])
            cp(P, "act", lq, lqv[64:128, hk, :, 2 * hk + 1], pq[64:128, hk, 0:4], rd=[pq])
        P.dma("sp", kvn[:, 0:256], T["nk_s"][4 * s:4 * s + 4, :], reads=[X.nks_buf], writes=[kvn])
        P.dma("sp", kvn[:, 256:512], T["nv_s"][4 * s:4 * s + 4, :], reads=[X.nvs_buf], writes=[kvn])
        cp(P, "pool", knew, knew[0:4, :], kvn[:, 0:256], rd=[kvn])
        vn = vslots[64]
        P.op("pool", lambda e, vn=vn: e.memset(vn[:, :], 0.0), writes=[vn])
        cp(P, "pool", vn, vn[0:4, :], kvn[:, 256:512], rd=[kvn])
        pp = None
        for j in range(65):
            if j < 64:
                kp = kpg.get()
                vj = vslots[j]

                def issue(eng, kp=kp, vj=vj, idx=s * 64 + j):
                    eng.indirect_dma_start(out=kp[:, :], out_offset=None, in_=ck[:, :],
                                           in_offset=bass.IndirectOffsetOnAxis(ap=pidx[:, idx:idx + 1], axis=0)).then_inc(P._k_sem, 16)
                    return eng.indirect_dma_start(out=vj[:, :], out_offset=None, in_=cv[:, :],
                                                  in_offset=bass.IndirectOffsetOnAxis(ap=pidx[:, idx:idx + 1], axis=0))
                P.dma_custom("pool", issue, reads=[pidx], writes=[kp, vj], extra=1)
                ksrc = kp
            else:
                ksrc = knew
            pk = pT.get()
            kdd = kdups.get()
            cp(P, "pool" if j % 2 == 0 else "dve", kdd, kdd[:, :, :, :],
               ksrc[:, :].rearrange("p (h d) -> p h d", h=4).unsqueeze(2).to_broadcast([128, 4, 2, 64]), rd=[ksrc])
            for hk in range(4):
                tr(P, pk, pk[:, hk, :], kdd[:, hk, :, :].rearrange("p r d -> p (r d)"), identb[:, :], [kdd, identb], inc=(hk == 3))
            kd = ktd.get()
            cp(P, "act" if j % 2 == 0 else "dve", kd, kd[:, :, :], pk[:, 0:4, :], rd=[pk])
            if j % 4 == 0:
                pp = pS.get()
            for hk in range(4):
                mm(P, pp, pp[0:32, (j % 4) * 128:(j % 4 + 1) * 128], lq[:, hk, :], kd[:, hk, :], [lq, kd], start=(hk == 0), stop=(hk == 3))
            if j % 4 == 3 or j == 64:
                c0 = (j // 4) * 512
                w = (j % 4 + 1) * 128
                cp(P, "dve", Sa, Sa[:, c0:c0 + w], pp[0:32, 0:w], rd=[pp])
        P.op("dve", lambda e: e.tensor_reduce(gt[:, :], Sa[:, 0:8192].rearrange("p (n k) -> p n k", k=256), AX.X, ALU.add), reads=[Sa], writes=[gt])
        P.op("dve", lambda e: e.max(mx8[:], gt[:]), reads=[gt], writes=[mx8])
        ts(P, "dve", m01, m01[:], gt[:], mx8[:, 2:3], None, ALU.is_ge, rd=[gt, mx8])
        P.op("dve", lambda e: e.reduce_max(rm[:], Sa[:, :], AX.X), reads=[Sa], writes=[rm])
        ts(P, "dve", nm, nm[:], rm[:], -scale, None, ALU.mult, rd=[rm])
        actf(P, Ee, Ee[:, :], Sa[:, :], AF.Exp, rd=[Sa, nm], scale=scale, bias=nm[:, 0:1])
        tt(P, "pool", Ee, Ee[:, 0:8192].rearrange("p (n k) -> p n k", k=256), Ee[:, 0:8192].rearrange("p (n k) -> p n k", k=256),
           m01[:, :].unsqueeze(2).to_broadcast([32, 32, 256]), ALU.mult, rd=[Ee, m01])
        tt(P, "dve", Ee, Ee[:, :], Ee[:, :], tab[:, :], ALU.mult, rd=[Ee, tab])
        P.op("dve", lambda e: e.reduce_sum(rs[:, 0:1], Ee[:, :], AX.X), reads=[Ee], writes=[rs])
        cp(P, "pool", Pb, Pb[:, :], Ee[:, :], rd=[Ee])
        for g0 in range(0, 65, 16):
            g1 = min(g0 + 16, 65)
            ptp = pP.get()
            for j in range(g0, g1):
                tr(P, ptp, ptp[:, j - g0, :], Pb[:, j * 128:(j + 1) * 128], identb[0:32, 0:32], [Pb, identb], inc=(j == g1 - 1))
            pts = PTs.get()
            cp(P, "act" if (g0 // 16) % 2 == 0 else "dve", pts, pts[:, 0:g1 - g0, :], ptp[:, 0:g1 - g0, :], rd=[ptp])
            for j in range(g0, g1):
                mm(P, pO, pO[0:32, 0:256], pts[:, j - g0, :], vslots[j][:, :], [pts, vslots[j]], start=(j == 0), stop=(j == 64), inc=(j == g1 - 1))
        tt(P, "dve", osel, osel[:, :, :], pO[0:32, 0:256].rearrange("p (h d) -> p h d", h=4), sel[:, :].unsqueeze(2).to_broadcast([32, 4, 64]), ALU.mult,
           rd=[pO, sel])
        P.op("dve", lambda e: e.tensor_reduce(ao[:, :], osel[:, :, :].rearrange("p h d -> p d h"), AX.X, ALU.add), reads=[osel], writes=[ao])
        P.op("dve", lambda e: e.reciprocal(rcp[:], rs[:]), reads=[rs], writes=[rcp])
        ts(P, "dve", ao, ao[:, :], ao[:, :], rcp[:, 0:1], None, ALU.mult, rd=[ao, rcp])
        for t in range(4):
            P.dma("sp", T["mix_d"][SEQ + 4 * s + t, 0:512].rearrange("(h d) -> h d", h=8), ao[8 * t:8 * t + 8, :], reads=[ao], writes=[mix_d])


ALL_STAGES = ("p0", "a1", "a2", "smp", "a3", "b")
_NC_CACHE = {}


def kernel(**inputs):
    inp = {k: np.asarray(v) for k, v in inputs.items()}
    if "nc" not in _NC_CACHE:
        _NC_CACHE["nc"] = build(ALL_STAGES)
    nc = _NC_CACHE["nc"]
    in_maps = [shard_inputs(inp, c) for c in range(NCORES)]
    res = run_bass_kernel_spmd(nc, in_maps, core_ids=list(range(NCORES)))
    R = res.results
    cat = lambda name: [np.asarray(R[c][name]) for c in range(NCORES)]
    y_p = np.stack(cat("y_p"), 0).reshape(8, SEQ, D)
    y_s = np.concatenate(cat("y_s"), 0).reshape(128, 4, D)
    nk_p = np.stack(cat("nk_p"), 0).reshape(1, 8, SEQ, 4, 64)
    nv_p = np.stack(cat("nv_p"), 0).reshape(1, 8, SEQ, 4, 64)
    nc_p = np.stack(cat("nc_p"), 0).reshape(1, 8, 3, 1536)
    ns_p = np.stack(cat("ns_p"), 0).reshape(1, 8, 4, 128, 128)
    nk_s = np.concatenate(cat("nk_s"), 0).reshape(1, 128, 4, 4, 64)
    nv_s = np.concatenate(cat("nv_s"), 0).reshape(1, 128, 4, 4, 64)
    nc_s = np.concatenate(cat("nc_s"), 0).reshape(1, 128, 3, 1536)
    ns_s = np.concatenate(cat("ns_s"), 0).reshape(1, 128, 4, 128, 128)
    return tuple(a.astype(np.float32, copy=False) for a in (y_p, y_s, nk_p, nv_p, nc_p, ns_p, nk_s, nv_s, nc_s, ns_s))
```
